# Optimizing a Trainium2 kernel written in Bass

```python
import functools
import jax, jax.numpy as jnp
from jax import lax
import numpy as np

D_MODEL = 2048
BATCH = 4
SEQ = 4096
DEPTH = 2

N_MIXERS = 2
Q_BLOCK = 128
EPS = 1e-6
MLA_HEADS = 16
MLA_Q_LORA = 512
MLA_KV_LORA = 512
MLA_NOPE = 128
MLA_ROPE = 64
MLA_V = 128
ROPE_THETA = 10000.0
FOX_HEADS = 16
FOX_HEAD_DIM = 128
N_GROUPS = 8
EXPERTS_PER_GROUP = 8
N_EXPERTS = N_GROUPS * EXPERTS_PER_GROUP
TOP_K = 2
D_EXPERT = 512
ROUTE_BLOCK = 128

kernel_name = "hybrid_mla_fox_hier_moe"


def rms_norm(x, gain):
    xf = x.astype(jnp.float32)
    y = xf * lax.rsqrt(jnp.mean(xf * xf, axis=-1, keepdims=True) + EPS)
    return (y * gain.astype(jnp.float32)).astype(x.dtype)


def apply_rope(x, positions):
    half = x.shape[-1] // 2
    freqs = ROPE_THETA ** (-jnp.arange(half, dtype=jnp.float32) / half)
    ang = positions.astype(jnp.float32)[..., None] * freqs
    cos = jnp.cos(ang)[:, :, None, :]
    sin = jnp.sin(ang)[:, :, None, :]
    x1 = x[..., :half].astype(jnp.float32)
    x2 = x[..., half:].astype(jnp.float32)
    out = jnp.concatenate([x1 * cos - x2 * sin, x2 * cos + x1 * sin], axis=-1)
    return out.astype(x.dtype)


def causal_block_attention(q, k, v, scale, log_decay=None):
    B, S, H, Dk = q.shape
    Dv = v.shape[-1]
    nb = S // Q_BLOCK
    q_blocks = q.reshape(B, nb, Q_BLOCK, H, Dk).transpose(1, 0, 2, 3, 4)
    k_pos = jnp.arange(S)
    xs = (jnp.arange(nb), q_blocks)
    if log_decay is not None:
        xs = xs + (log_decay.reshape(B, H, nb, Q_BLOCK).transpose(2, 0, 1, 3),)

    def one_block(blk):
        i, q_i = blk[0], blk[1]
        s = jnp.einsum("bqhd,bkhd->bhqk", q_i, k, preferred_element_type=jnp.float32) * scale
        if log_decay is not None:
            s = s + (blk[2][..., :, None] - log_decay[:, :, None, :])
        q_pos = i * Q_BLOCK + jnp.arange(Q_BLOCK)
        s = jnp.where(q_pos[:, None] >= k_pos[None, :], s, -jnp.inf)
        p = jax.nn.softmax(s, axis=-1)
        return jnp.einsum("bhqk,bkhd->bqhd", p.astype(v.dtype), v)

    out = lax.map(one_block, xs)
    return out.transpose(1, 0, 2, 3, 4).reshape(B, S, H * Dv)


def mla_mixer(h, positions, w_in, q_lat_norm, kv_lat_norm, w_uq, w_ukv, qk_gain, w_o):
    B, S, _ = h.shape
    z = h @ w_in
    c_q = z[..., :MLA_Q_LORA]
    c_kv = z[..., MLA_Q_LORA:MLA_Q_LORA + MLA_KV_LORA]
    k_rope = z[..., MLA_Q_LORA + MLA_KV_LORA:][:, :, None, :]
    q = (rms_norm(c_q, q_lat_norm) @ w_uq).reshape(B, S, MLA_HEADS, MLA_NOPE + MLA_ROPE)
    kv = (rms_norm(c_kv, kv_lat_norm) @ w_ukv).reshape(B, S, MLA_HEADS, MLA_NOPE + MLA_V)
    q_nope = rms_norm(q[..., :MLA_NOPE], qk_gain[0, :MLA_NOPE])
    q_rope = apply_rope(rms_norm(q[..., MLA_NOPE:], qk_gain[0, MLA_NOPE:]), positions)
    k_nope = rms_norm(kv[..., :MLA_NOPE], qk_gain[1, :MLA_NOPE])
    k_rope = apply_rope(rms_norm(k_rope, qk_gain[1, MLA_NOPE:]), positions)
    v = kv[..., MLA_NOPE:]
    q_full = jnp.concatenate([q_nope, q_rope], axis=-1)
    k_full = jnp.concatenate([k_nope, jnp.broadcast_to(k_rope, (B, S, MLA_HEADS, MLA_ROPE))], axis=-1)
    o = causal_block_attention(q_full, k_full, v, 1.0 / math_sqrt(MLA_NOPE + MLA_ROPE))
    return o @ w_o


def math_sqrt(n):
    return float(np.sqrt(n))


def fox_mixer(h, w_in, forget_bias, qk_gain, w_o):
    B, S, _ = h.shape
    HD = FOX_HEADS * FOX_HEAD_DIM
    z = h @ w_in
    q = rms_norm(z[..., :HD].reshape(B, S, FOX_HEADS, FOX_HEAD_DIM), qk_gain[0])
    k = rms_norm(z[..., HD:2 * HD].reshape(B, S, FOX_HEADS, FOX_HEAD_DIM), qk_gain[1])
    v = z[..., 2 * HD:3 * HD].reshape(B, S, FOX_HEADS, FOX_HEAD_DIM)
    f_logit = z[..., 3 * HD:3 * HD + FOX_HEADS]
    gate = z[..., 3 * HD + FOX_HEADS:]
    log_f = jax.nn.log_sigmoid(f_logit.astype(jnp.float32) + forget_bias.astype(jnp.float32))
    c = jnp.cumsum(log_f, axis=1).transpose(0, 2, 1)
    o = causal_block_attention(q, k, v, 1.0 / math_sqrt(FOX_HEAD_DIM), log_decay=c)
    o = o * jax.nn.sigmoid(gate)
    return o @ w_o


def routed_experts(t, e_idx, gate, w_gate_up, w_down):
    T, D = t.shape
    n_assign = T * TOP_K
    e_flat = e_idx.reshape(-1)
    tok_flat = jnp.repeat(jnp.arange(T), TOP_K)
    g_flat = gate.reshape(-1)
    order = jnp.argsort(e_flat)
    e_sorted, tok_sorted, g_sorted = e_flat[order], tok_flat[order], g_flat[order]
    counts = jnp.zeros((N_EXPERTS,), jnp.int32).at[e_flat].add(1)
    start = jnp.cumsum(counts) - counts
    padded = (counts + ROUTE_BLOCK - 1) // ROUTE_BLOCK * ROUTE_BLOCK
    pend = jnp.cumsum(padded)
    pstart = pend - padded
    dest = pstart[e_sorted] + (jnp.arange(n_assign) - start[e_sorted])
    n_blocks = (n_assign + N_EXPERTS * (ROUTE_BLOCK - 1) + ROUTE_BLOCK - 1) // ROUTE_BLOCK
    buf = jnp.zeros((n_blocks * ROUTE_BLOCK, D), t.dtype).at[dest].set(t[tok_sorted])
    block_e = jnp.minimum(
        jnp.searchsorted(pend, jnp.arange(n_blocks) * ROUTE_BLOCK, side="right"), N_EXPERTS - 1)

    def expert_block(blk):
        xb, e = blk
        gu = xb @ w_gate_up[e]
        return (jax.nn.silu(gu[:, :D_EXPERT]) * gu[:, D_EXPERT:]) @ w_down[e]

    y = lax.map(expert_block, (buf.reshape(n_blocks, ROUTE_BLOCK, D), block_e))
    y = y.reshape(n_blocks * ROUTE_BLOCK, D)[dest]
    return jax.ops.segment_sum(y * g_sorted[:, None], tok_sorted, num_segments=T)


def hier_moe(h, w_router_group, b_router_group, w_router_expert, b_router_expert, w_gate_up, w_down):
    B, S, D = h.shape
    T = B * S
    t = h.reshape(T, D)
    g_logits = (t @ w_router_group).astype(jnp.float32) + b_router_group.astype(jnp.float32)
    p_group = jax.nn.softmax(g_logits, axis=-1)
    _, grp = lax.top_k(g_logits, 1)
    p_g = jnp.take_along_axis(p_group, grp, axis=-1)
    e_logits = ((t @ w_router_expert).astype(jnp.float32)
                + b_router_expert.astype(jnp.float32)).reshape(T, N_GROUPS, EXPERTS_PER_GROUP)
    e_in = jnp.take_along_axis(e_logits, grp[:, :, None], axis=1)[:, 0]
    p_e = jax.nn.softmax(e_in, axis=-1)
    top_p, top_i = lax.top_k(p_e, TOP_K)
    gate = p_g * top_p / jnp.sum(top_p, axis=-1, keepdims=True)
    e_idx = grp * EXPERTS_PER_GROUP + top_i
    y = routed_experts(t, e_idx, gate.astype(t.dtype), w_gate_up, w_down)
    return y.reshape(B, S, D)


def setup_inputs(seed: int = 0) -> dict:
    key = jax.random.key(seed)
    ks = iter(jax.random.split(key, 40))
    D = D_MODEL

    def dense(shape, fan_in):
        return jax.random.normal(next(ks), shape, jnp.float32) * fan_in ** -0.5

    def gain(shape):
        return 1.0 + 0.1 * jax.random.normal(next(ks), shape, jnp.float32)

    def small(shape):
        return 0.01 * jax.random.normal(next(ks), shape, jnp.float32)

    def moe_params(prefix):
        return {
            prefix + "norm_ffn": gain((D,)),
            prefix + "router_group": dense((D, N_GROUPS), D),
            prefix + "router_group_bias": small((N_GROUPS,)),
            prefix + "router_expert": dense((D, N_EXPERTS), D),
            prefix + "router_expert_bias": small((N_EXPERTS,)),
            prefix + "w_gate_up": dense((N_EXPERTS, D, 2 * D_EXPERT), D),
            prefix + "w_down": dense((N_EXPERTS, D_EXPERT, D), D_EXPERT),
        }

    x = jax.random.normal(next(ks), (BATCH, SEQ, D), jnp.float32)
    offsets = jax.random.randint(next(ks), (BATCH, 1), 0, 1024, dtype=jnp.int32)
    positions = offsets + jnp.arange(SEQ, dtype=jnp.int32)[None, :]
    inp = {"x": x, "positions": positions}
    inp["l0_norm_mix"] = gain((D,))
    inp["l0_mla_w_in"] = dense((D, MLA_Q_LORA + MLA_KV_LORA + MLA_ROPE), D)
    inp["l0_mla_q_lat_norm"] = gain((MLA_Q_LORA,))
    inp["l0_mla_kv_lat_norm"] = gain((MLA_KV_LORA,))
    inp["l0_mla_w_uq"] = dense((MLA_Q_LORA, MLA_HEADS * (MLA_NOPE + MLA_ROPE)), MLA_Q_LORA)
    inp["l0_mla_w_ukv"] = dense((MLA_KV_LORA, MLA_HEADS * (MLA_NOPE + MLA_V)), MLA_KV_LORA)
    inp["l0_mla_qk_gain"] = gain((2, MLA_NOPE + MLA_ROPE))
    inp["l0_mla_w_o"] = dense((MLA_HEADS * MLA_V, D), MLA_HEADS * MLA_V)
    inp.update(moe_params("l0_"))
    HD = FOX_HEADS * FOX_HEAD_DIM
    inp["l1_norm_mix"] = gain((D,))
    inp["l1_fox_w_in"] = dense((D, 3 * HD + FOX_HEADS + HD), D)
    inp["l1_fox_forget_bias"] = jax.random.uniform(next(ks), (FOX_HEADS,), jnp.float32, 1.0, 5.0)
    inp["l1_fox_qk_gain"] = gain((2, FOX_HEAD_DIM))
    inp["l1_fox_w_o"] = dense((HD, D), HD)
    inp.update(moe_params("l1_"))
    return inp


def reference(x, positions,
              l0_norm_mix, l0_mla_w_in, l0_mla_q_lat_norm, l0_mla_kv_lat_norm, l0_mla_w_uq,
              l0_mla_w_ukv, l0_mla_qk_gain, l0_mla_w_o,
              l0_norm_ffn, l0_router_group, l0_router_group_bias, l0_router_expert,
              l0_router_expert_bias, l0_w_gate_up, l0_w_down,
              l1_norm_mix, l1_fox_w_in, l1_fox_forget_bias, l1_fox_qk_gain, l1_fox_w_o,
              l1_norm_ffn, l1_router_group, l1_router_group_bias, l1_router_expert,
              l1_router_expert_bias, l1_w_gate_up, l1_w_down):
    mixers = [
        functools.partial(mla_mixer, positions=positions, w_in=l0_mla_w_in,
                          q_lat_norm=l0_mla_q_lat_norm, kv_lat_norm=l0_mla_kv_lat_norm,
                          w_uq=l0_mla_w_uq, w_ukv=l0_mla_w_ukv, qk_gain=l0_mla_qk_gain,
                          w_o=l0_mla_w_o),
        functools.partial(fox_mixer, w_in=l1_fox_w_in, forget_bias=l1_fox_forget_bias,
                          qk_gain=l1_fox_qk_gain, w_o=l1_fox_w_o),
    ]
    layers = [
        (l0_norm_mix, l0_norm_ffn, (l0_router_group, l0_router_group_bias, l0_router_expert,
                                     l0_router_expert_bias, l0_w_gate_up, l0_w_down)),
        (l1_norm_mix, l1_norm_ffn, (l1_router_group, l1_router_group_bias, l1_router_expert,
                                     l1_router_expert_bias, l1_w_gate_up, l1_w_down)),
    ]
    h = x
    for i in range(DEPTH):
        norm_mix, norm_ffn, moe_w = layers[i]
        h = h + mixers[i % N_MIXERS](rms_norm(h, norm_mix))
        h = h + hier_moe(rms_norm(h, norm_ffn), *moe_w)
    return h
```

```python
import numpy as np
import ml_dtypes
from contextlib import ExitStack
import concourse.bass as bass
import concourse.mybir as mybir
from concourse.bass_utils import run_bass_kernel_spmd

F32 = mybir.dt.float32
BF16 = mybir.dt.bfloat16
I32 = mybir.dt.int32
U32 = mybir.dt.uint32
AF = mybir.ActivationFunctionType
ALU = mybir.AluOpType
AX = mybir.AxisListType
P = 128
EPS = 1e-6


class Sem:
    def __init__(self, h):
        self.h = h
        self.count = 0


class Buf:
    def __init__(self, t, name, persist=True):
        self.t = t
        self.name = name
        self.lw = None
        self.rd = {}
        self.sem = None
        self.persist = persist

    def __getitem__(self, key):
        return self.t[key]


class Op:
    __slots__ = ("eng", "meth", "args", "kw", "deps", "needed", "isdma", "sem", "val", "barrier")


class K:
    def __init__(self, nc):
        self.nc = nc
        self.es = ExitStack()
        self.eng = {"pe": nc.tensor, "act": nc.scalar, "dve": nc.vector, "pool": nc.gpsimd, "sp": nc.sync}
        self.esem = {}
        for e in self.eng:
            self.esem[e] = Sem(self.es.enter_context(nc.semaphore("sem_" + e)))
        self.allsems = list(self.esem.values())
        self.ops = []
        self.emitted = 0
        self.waited = {e: {} for e in self.eng}
        self.last = {}
        self.nbuf = 0
        self.stage = None
        self.last_barrier = 0
        self.free_sems = []
        self.stage_bufs = []
        self.ps = [Buf(self.es.enter_context(nc.psum_tensor("psb%d" % i, [P, 512], F32)), "psb%d" % i) for i in range(8)]

    def begin_stage(self):
        self.stage = ExitStack()

    def end_stage(self):
        self.barrier()
        self.flush()
        self.stage.close()
        self.stage = None
        for b in self.stage_bufs:
            if b.sem is not None:
                self.free_sems.append(b.sem)
                b.sem = None
        self.stage_bufs = []

    def sb(self, name, shape, dt, persist=False):
        st = self.es if persist else self.stage
        self.nbuf += 1
        t = st.enter_context(self.nc.sbuf_tensor("%s_%d" % (name, self.nbuf), list(shape), dt))
        b = Buf(t, name, persist)
        if not persist:
            self.stage_bufs.append(b)
        return b

    def dram(self, name, shape, dt, kind="Internal"):
        t = self.nc.dram_tensor(name, list(shape), dt, kind=kind).ap()
        return Buf(t, name)

    def _sem_of(self, b):
        if b.sem is None and (not b.persist) and self.free_sems:
            b.sem = self.free_sems.pop()
        if b.sem is None:
            b.sem = Sem(self.es.enter_context(self.nc.semaphore("dsem_%d_%s" % (len(self.allsems), b.name))))
            self.allsems.append(b.sem)
        return b.sem

    def _rec(self, eng, meth, args, kw, r, w, isdma=False, sembuf=None):
        op = Op()
        op.eng, op.meth, op.args, op.kw = eng, meth, args, kw
        op.isdma = isdma
        op.needed = False
        op.val = None
        op.barrier = False
        op.sem = self._sem_of(sembuf) if isdma else None
        idx = len(self.ops)
        deps = set()
        for b in r:
            if b.lw is not None:
                deps.add(b.lw)
        for b in w:
            if b.lw is not None:
                deps.add(b.lw)
            deps.update(b.rd.values())
        deps.discard(idx)
        op.deps = []
        for d in deps:
            if d < self.last_barrier:
                continue
            p = self.ops[d]
            if (not p.isdma) and (not isdma) and p.eng == "pe" and eng == "pe":
                continue
            p.needed = True
            op.deps.append(d)
        key = ("dma", id(op.sem)) if isdma else eng
        for b in r:
            b.rd[key] = idx
        for b in w:
            b.lw = idx
            b.rd = {}
        if not isdma:
            self.last[eng] = idx
        self.ops.append(op)
        return op

    def pe(self, meth, *args, r=(), w=(), **kw):
        return self._rec("pe", meth, args, kw, r, w)

    def act(self, meth, *args, r=(), w=(), **kw):
        return self._rec("act", meth, args, kw, r, w)

    def dve(self, meth, *args, r=(), w=(), **kw):
        return self._rec("dve", meth, args, kw, r, w)

    def pool(self, meth, *args, r=(), w=(), **kw):
        return self._rec("pool", meth, args, kw, r, w)

    def dma(self, q, out, in_, r=(), w=(), sem=None, **kw):
        return self._rec(q, "dma_start", (), dict(out=out, in_=in_, **kw), r, w, isdma=True, sembuf=sem)

    def idma(self, out, out_off, in_, in_off, r=(), w=(), sem=None, **kw):
        return self._rec("pool", "indirect_dma_start", (out, out_off, in_, in_off), kw, r, w, isdma=True, sembuf=sem)

    def collective(self, kind, in_ap, out_ap, out_buf, groups):
        return self._rec("pool", "collective_compute", (kind, ALU.bypass, groups, [in_ap], [out_ap]), {}, (), (out_buf,),
                         isdma=True, sembuf=out_buf)

    def barrier(self):
        op = Op()
        op.barrier = True
        op.isdma = False
        op.deps = []
        op.needed = False
        for e, idx in self.last.items():
            self.ops[idx].needed = True
        self.ops.append(op)
        self.last_barrier = len(self.ops)

    def _wait(self, eng, s, tgt):
        if tgt <= 0:
            return
        if self.waited[eng].get(id(s), 0) < tgt:
            self.eng[eng].wait_ge(s.h, tgt)
            self.waited[eng][id(s)] = tgt

    def flush(self):
        ops = self.ops
        while self.emitted < len(ops):
            op = ops[self.emitted]
            self.emitted += 1
            if op.barrier:
                for e in self.eng:
                    for s in self.allsems:
                        self._wait(e, s, s.count)
                continue
            need = {}
            for d in op.deps:
                p = ops[d]
                if p.isdma:
                    s = p.sem
                    tgt = s.count
                else:
                    s = self.esem[p.eng]
                    tgt = p.val
                    assert tgt is not None
                if need.get(id(s), (None, 0))[1] < tgt:
                    need[id(s)] = (s, tgt)
            for s, tgt in need.values():
                self._wait(op.eng, s, tgt)
            ins = getattr(self.eng[op.eng], op.meth)(*op.args, **op.kw)
            if op.isdma:
                op.sem.count += 16
                ins.then_inc(op.sem.h, 16)
            elif op.needed:
                s = self.esem[op.eng]
                s.count += 1
                op.val = s.count
                ins.then_inc(s.h, 1)
            op.args = None
            op.kw = None

    def finish(self):
        self.barrier()
        self.flush()
        self.es.close()


def bcast(ap, axis, n):
    pairs = [list(x) for x in ap.ap]
    pairs.insert(axis, [0, n])
    return bass.AP(ap.tensor, ap.offset, pairs)

class Consts:
    pass


def make_consts(k):
    c = Consts()
    nc = k.nc
    io = k.sb("iota_i", [P, P], I32, persist=True)
    k.pool("iota", io[:, :], [[1, P]], base=0, channel_multiplier=-1, w=[io])
    iof = k.sb("iota_f", [P, P], F32, persist=True)
    k.dve("tensor_copy", iof[:, :], io[:, :], r=[io], w=[iof])
    c.ident_f = k.sb("ident_f", [P, P], F32, persist=True)
    k.dve("tensor_single_scalar", c.ident_f[:, :], iof[:, :], 0.0, ALU.is_equal, r=[iof], w=[c.ident_f])
    c.ident_b = k.sb("ident_b", [P, P], BF16, persist=True)
    k.dve("tensor_copy", c.ident_b[:, :], c.ident_f[:, :], r=[c.ident_f], w=[c.ident_b])
    c.tri_ge_f = k.sb("tri_ge_f", [P, P], F32, persist=True)
    k.dve("tensor_single_scalar", c.tri_ge_f[:, :], iof[:, :], 0.0, ALU.is_ge, r=[iof], w=[c.tri_ge_f])
    c.tri_ge_b = k.sb("tri_ge_b", [P, P], BF16, persist=True)
    k.dve("tensor_copy", c.tri_ge_b[:, :], c.tri_ge_f[:, :], r=[c.tri_ge_f], w=[c.tri_ge_b])
    c.tri_gt_f = k.sb("tri_gt_f", [P, P], F32, persist=True)
    k.dve("tensor_single_scalar", c.tri_gt_f[:, :], iof[:, :], 0.0, ALU.is_gt, r=[iof], w=[c.tri_gt_f])
    c.ones_f = k.sb("ones_f", [P, P], F32, persist=True)
    k.dve("memset", c.ones_f[:, :], 1.0, w=[c.ones_f])
    c.eps = k.sb("eps_t", [P, 1], F32, persist=True)
    k.dve("memset", c.eps[:, :], EPS, w=[c.eps])
    c.one = k.sb("one_t", [P, 1], F32, persist=True)
    k.dve("memset", c.one[:, :], 1.0, w=[c.one])
    c.negpi = k.sb("negpi_t", [P, 1], F32, persist=True)
    k.dve("memset", c.negpi[:, :], -float(np.pi) * (1.0 - 1e-6), w=[c.negpi])
    ci = k.sb("col_i", [P, 64], I32, persist=True)
    k.pool("iota", ci[:, :], [[1, 64]], base=0, channel_multiplier=0, w=[ci])
    c.colidx = k.sb("col_f", [P, 64], F32, persist=True)
    k.dve("tensor_copy", c.colidx[:, :], ci[:, :], r=[ci], w=[c.colidx])
    pi_ = k.sb("part_i", [P, 1], I32, persist=True)
    k.pool("iota", pi_[:, :], [[0, 1]], base=0, channel_multiplier=1, w=[pi_])
    c.partidx = k.sb("part_f", [P, 1], F32, persist=True)
    k.dve("tensor_copy", c.partidx[:, :], pi_[:, :], r=[pi_], w=[c.partidx])
    c.LOG = k.sb("LOG", [P, 16, 72], F32, persist=True)
    c.MSK = k.sb("MSK", [P, 16, 2, 64], F32, persist=True)
    c.GT = k.sb("GT", [P, 16, 2], F32, persist=True)
    c.CUM = k.sb("CUM", [P, 16, 64], F32, persist=True)
    c.DESTI = k.sb("DESTI", [P, 16, 2], I32, persist=True)
    c.IDXW = k.sb("IDXW", [P, 96], I32, persist=True)
    c.IDX16 = k.sb("IDX16", [P, 96, 16], I32, persist=True)
    c.IDX4 = k.sb("IDX4", [P, 96, 4], I32, persist=True)
    return c


def rms(k, c, src, src_ap, G, W, gain_ap, out, out_ap, scr, scr2, stat, post_scale=None):
    sq = scr[:, 0:G * W].rearrange("p (g w) -> p g w", g=G)
    k.dve("tensor_tensor", sq, src_ap, src_ap, ALU.mult, r=[src], w=[scr])
    ssq = stat[:, 0:G]
    k.dve("tensor_reduce", ssq, sq, AX.X, ALU.add, r=[scr], w=[stat])
    std = stat[:, G:2 * G]
    k.act("activation", std, ssq, AF.Sqrt, bias=c.eps[:, 0:1], scale=1.0 / W, r=[stat, c.eps], w=[stat])
    k.dve("reciprocal", ssq, std, r=[stat], w=[stat])
    y = scr2[:, 0:G * W].rearrange("p (g w) -> p g w", g=G)
    k.dve("tensor_tensor", y, src_ap, bcast(ssq, 2, W), ALU.mult, r=[src, stat], w=[scr2])
    k.pool("tensor_tensor", out_ap, y, bcast(gain_ap, 1, G), ALU.mult, r=[scr2], w=[out])


def transposes(k, c, srcs, src_buf, dst_buf, dst_ap_fn, dt, ps_buf, evac="act"):
    n = srcs[0].shape[1]
    rows = srcs[0].shape[0]
    per = (1024 if dt == BF16 else 512) // P
    ident = c.ident_b if dt == BF16 else c.ident_f
    if dt == BF16:
        pst = ps_buf.t[:, :].bitcast(BF16)
    else:
        pst = ps_buf.t[:, :]
    i = 0
    while i < len(srcs):
        cnt = min(per, len(srcs) - i)
        for j in range(cnt):
            k.pe("transpose", pst[0:n, j * P:j * P + rows], srcs[i + j], ident[0:rows, 0:rows], r=[src_buf, ident], w=[ps_buf])
        src_v = pst[0:n, 0:cnt * P].rearrange("p (a b) -> p a b", a=cnt)[:, :, 0:rows]
        if evac == "act":
            k.act("copy", dst_ap_fn(i, cnt), src_v, r=[ps_buf], w=[dst_buf])
        else:
            k.dve("tensor_copy", dst_ap_fn(i, cnt), src_v, r=[ps_buf], w=[dst_buf])
        i += cnt


def rms_multi(k, c, specs):
    sqs, ssqs, stds, ys = [], [], [], []
    for (src, src_ap, G, W, gain_ap, out, out_ap, scr, scr2, stat) in specs:
        sq = scr[:, 0:G * W].rearrange("p (g w) -> p g w", g=G)
        k.dve("tensor_tensor", sq, src_ap, src_ap, ALU.mult, r=[src], w=[scr])
        sqs.append(sq)
    for i, (src, src_ap, G, W, gain_ap, out, out_ap, scr, scr2, stat) in enumerate(specs):
        ssq = stat[:, 0:G]
        k.dve("tensor_reduce", ssq, sqs[i], AX.X, ALU.add, r=[scr], w=[stat])
        ssqs.append(ssq)
    for i, (src, src_ap, G, W, gain_ap, out, out_ap, scr, scr2, stat) in enumerate(specs):
        std = stat[:, G:2 * G]
        k.act("activation", std, ssqs[i], AF.Sqrt, bias=c.eps[:, 0:1], scale=1.0 / W, r=[stat, c.eps], w=[stat])
        stds.append(std)
    for i, (src, src_ap, G, W, gain_ap, out, out_ap, scr, scr2, stat) in enumerate(specs):
        k.dve("reciprocal", ssqs[i], stds[i], r=[stat], w=[stat])
    for i, (src, src_ap, G, W, gain_ap, out, out_ap, scr, scr2, stat) in enumerate(specs):
        y = scr2[:, 0:G * W].rearrange("p (g w) -> p g w", g=G)
        k.dve("tensor_tensor", y, src_ap, bcast(ssqs[i], 2, W), ALU.mult, r=[src, stat], w=[scr2])
        ys.append(y)
    for i, (src, src_ap, G, W, gain_ap, out, out_ap, scr, scr2, stat) in enumerate(specs):
        k.pool("tensor_tensor", out_ap, ys[i], bcast(gain_ap, 1, G), ALU.mult, r=[scr2], w=[out])

NQT = 8
NKT = 32
SEQ = 4096


def attention(k, c, load_head, kshared, otx, gate_d=None):
    NH = 8
    pt = [k.sb("pt%d" % i, [P, 512], BF16) for i in range(4)]
    osb = [k.sb("osb%d" % i, [P, P], BF16) for i in range(2)]
    rec = [k.sb("rec%d" % i, [P, 1], F32) for i in range(2)]
    ost = [k.sb("ost%d" % i, [P, 512], BF16) for i in range(2)]
    sg = [k.sb("sg%d" % i, [P, 4, P], BF16) for i in range(2)] if gate_d is not None else None
    S = [k.ps[0], k.ps[1], k.ps[7]]
    oacc = [k.ps[2], k.ps[3], k.ps[4], k.ps[5]]
    pst = k.ps[6]
    st = dict(pcount=0, fin=0)

    def emit_qk(pairs, Q, kt):
        j = kt - 4 * Q
        q0 = max(j, 0) * P
        Sb = S[kt % 3]
        npair = len(pairs)
        for pi, (kb, kfn, qb, qfn) in enumerate(pairs):
            k.pe("matmul", Sb[:, q0:512], kfn(kt), qfn(Q * 512 + q0, (Q + 1) * 512),
                 start=(pi == 0), stop=(pi == npair - 1), r=[kb, qb], w=[Sb])

    def emit_rest(vbuf, Q, kt):
        j = kt - 4 * Q
        q0 = max(j, 0) * P
        Sb = S[kt % 3]
        Pb = pt[st["pcount"] % 4]
        st["pcount"] += 1
        k.act("activation", Pb[:, q0:512], Sb[:, q0:512], AF.Exp, r=[Sb], w=[Pb])
        if j >= 0:
            k.dve("tensor_tensor", Pb[:, j * P:(j + 1) * P], Pb[:, j * P:(j + 1) * P], c.tri_ge_b[:, :], ALU.mult,
                  r=[Pb, c.tri_ge_b], w=[Pb])
        for qs in range(max(j, 0), 4):
            ob = oacc[qs]
            k.pe("matmul", ob[:, 0:129], Pb[:, qs * P:(qs + 1) * P], vbuf[:, kt, 0:129],
                 start=(kt == 0), stop=(kt == 4 * Q + qs), r=[Pb, vbuf], w=[ob])

    def emit_fin(h, Q, sgt):
        stg = ost[Q % 2]
        for qs in range(4):
            ob = oacc[qs]
            rc = rec[st["fin"] % 2]
            o_ = osb[st["fin"] % 2]
            st["fin"] += 1
            k.dve("reciprocal", rc[:, :], ob[:, 128:129], r=[ob], w=[rc])
            k.dve("tensor_scalar", o_[:, :], ob[:, 0:128], rc[:, 0:1], None, ALU.mult, r=[ob, rc], w=[o_])
            if sgt is not None:
                k.dve("tensor_tensor", o_[:, :], o_[:, :], sgt[:, qs, :], ALU.mult, r=[o_, sgt], w=[o_])
            pv = pst.t[:, :].bitcast(BF16)
            k.pe("transpose", pv[:, 0:P], o_[:, :], c.ident_b[:, :], r=[o_, c.ident_b], w=[pst])
            k.act("copy", stg[:, qs * P:(qs + 1) * P], pv[:, 0:P], r=[pst], w=[stg])
        k.dma("sp", otx[Q // 4, h, :, (Q % 4) * 512:(Q % 4 + 1) * 512], stg[:, :], r=[stg], sem=stg)

    hb = [load_head(0, 0)]
    pending = None
    for h in range(NH):
        if h + 1 < NH:
            hb.append(load_head(h + 1, (h + 1) % 2))
        pairs, vbuf = hb[h]
        for Q in range(NQT):
            sgt = None
            if gate_d is not None:
                sgt = sg[Q % 2]
                k.dma("sp", sgt[:, :, :], gate_d[Q * 512:(Q + 1) * 512, h * P:(h + 1) * P].rearrange("(s p) d -> p s d", p=P),
                      r=[gate_d], w=[sgt], sem=sgt)
            nk = 4 * (Q + 1)
            emit_qk(pairs, Q, 0)
            emit_qk(pairs, Q, 1)
            if pending is not None:
                emit_fin(*pending)
                pending = None
            for kt in range(nk):
                if kt + 2 < nk:
                    emit_qk(pairs, Q, kt + 2)
                emit_rest(vbuf, Q, kt)
            pending = (h, Q, sgt)
    emit_fin(*pending)


def stage_A(k, c, d):
    SCALE = 1.0 / float(np.sqrt(192.0))
    NT = SEQ // P
    k.begin_stage()
    g_mix = k.sb("g_mix", [P, 2048], F32)
    g_ql = k.sb("g_ql", [P, 512], F32)
    g_kvl = k.sb("g_kvl", [P, 512], F32)
    g_qk = k.sb("g_qk", [P, 384], F32)
    g_q = k.sb("g_q", [P, 192], F32)
    freqs = k.sb("freqs", [P, 32], F32)
    for t_, s_ in ((g_mix, d["g_mix"]), (g_ql, d["g_ql"]), (g_kvl, d["g_kvl"]), (g_qk, d["g_qk"]), (freqs, d["freqs"])):
        k.dma("sp", t_[:, :], s_[:, :], r=[s_], w=[t_], sem=t_)
    k.dve("tensor_scalar", g_q[:, :], g_qk[:, 0:192], SCALE, None, ALU.mult, r=[g_qk], w=[g_q])
    w_in = k.sb("w_in", [P, 16, 1088], BF16)
    k.dma("pool", w_in[:, :, :], d["w_in"][:, :].rearrange("(kc p) n -> p kc n", p=P), r=[d["w_in"]], w=[w_in], sem=w_in)
    w_uq = k.sb("w_uq", [P, 4, 1536], BF16)
    k.dma("pool", w_uq[:, :, :], d["w_uq"][:, :].rearrange("(kc p) n -> p kc n", p=P), r=[d["w_uq"]], w=[w_uq], sem=w_uq)
    w_ukv = k.sb("w_ukv", [P, 4, 2048], BF16)
    k.dma("pool", w_ukv[:, :, :], d["w_ukv"][:, :].rearrange("(kc p) n -> p kc n", p=P), r=[d["w_ukv"]], w=[w_ukv], sem=w_ukv)

    xt = [k.sb("xt%d" % i, [P, 2048], F32) for i in range(2)]
    posi = [k.sb("posi%d" % i, [P, 1], I32) for i in range(2)]
    posf = k.sb("posf", [P, 1], F32)
    scr = k.sb("scr", [P, 2048], F32)
    scr2 = k.sb("scr2", [P, 2048], F32)
    stat = k.sb("stat", [P, 32], F32)
    sA = [k.sb("sA%d" % i, [P, 1024], F32) for i in range(2)]
    sB = [k.sb("sB%d" % i, [P, 512], F32) for i in range(2)]
    sC = [scr, scr2]
    stA = k.sb("stA", [P, 32], F32)
    stB = k.sb("stB", [P, 32], F32)
    stC = k.sb("stC", [P, 32], F32)
    hn = k.sb("hn", [P, 2048], BF16)
    hnT = k.sb("hnT", [P, 16, P], BF16)
    z = k.sb("z", [P, 1088], F32)
    cqn = k.sb("cqn", [P, 1024], BF16)
    latT = k.sb("latT", [P, 8, P], BF16)
    kr = k.sb("kr", [P, 64], F32)
    krb = k.sb("krb", [P, 64], BF16)
    krT = k.sb("krT", [64, P], BF16)
    qsb = k.sb("qsb", [P, 8, 192], F32)
    kvsb = k.sb("kvsb", [P, 8, 256], F32)
    qn = k.sb("qn", [P, 8, P], BF16)
    qr = k.sb("qr", [P, 8, 64], F32)
    qrb = k.sb("qrb", [P, 8, 64], BF16)
    kn = k.sb("kn", [P, 8, P], BF16)
    vb = k.sb("vb", [P, 8, P], BF16)
    qT = k.sb("qT", [P, 8, P], BF16)
    qrT = k.sb("qrT", [64, 8, P], BF16)
    kT = k.sb("kT", [P, 8, P], BF16)
    ang = k.sb("ang", [P, 32], F32)
    fr = k.sb("fr", [P, 2, 32], F32)
    fri = k.sb("fri", [P, 2, 32], I32)
    frf = k.sb("frf", [P, 2, 32], F32)
    cs = k.sb("cs", [P, 2, 32], F32)
    rt = k.sb("rt", [P, 8, 4, 32], F32)
    pstr = k.ps[7]

    def rope(src, src_ap_fn, G, dst, dst_ap_fn):
        x1 = src_ap_fn(0)
        x2 = src_ap_fn(1)
        sinb = bcast(cs[:, 0, :], 1, G)
        cosb = bcast(cs[:, 1, :], 1, G)
        t = [rt[:, 0:G, i, :] for i in range(4)]
        k.dve("tensor_tensor", t[0], x1, cosb, ALU.mult, r=[src, cs], w=[rt])
        k.dve("tensor_tensor", t[1], x2, sinb, ALU.mult, r=[src, cs], w=[rt])
        k.dve("tensor_tensor", t[2], x2, cosb, ALU.mult, r=[src, cs], w=[rt])
        k.dve("tensor_tensor", t[3], x1, sinb, ALU.mult, r=[src, cs], w=[rt])
        k.dve("tensor_tensor", dst_ap_fn(0), t[0], t[1], ALU.subtract, r=[rt], w=[dst])
        k.dve("tensor_tensor", dst_ap_fn(1), t[2], t[3], ALU.add, r=[rt], w=[dst])

    for i in range(NT):
        x_ = xt[i % 2]
        p_ = posi[i % 2]
        k.dma("sp", x_[:, :], d["x"][i * P:(i + 1) * P, :], r=[d["x"]], w=[x_], sem=x_)
        k.dma("sp", p_[:, :], d["pos"][i * P:(i + 1) * P, :], r=[d["pos"]], w=[p_], sem=p_)
        k.dve("tensor_copy", posf[:, :], p_[:, :], r=[p_], w=[posf])
        k.dve("tensor_scalar", ang[:, :], freqs[:, :], posf[:, 0:1], 1.0 / (2 * np.pi), ALU.mult, ALU.mult, r=[freqs, posf], w=[ang])
        k.dve("tensor_scalar", fr[:, 0, :], ang[:, :], 0.5, None, ALU.add, r=[ang], w=[fr])
        k.dve("tensor_scalar", fr[:, 1, :], ang[:, :], 0.75, None, ALU.add, r=[ang], w=[fr])
        k.dve("tensor_copy", fri[:, :, :], fr[:, :, :], r=[fr], w=[fri])
        k.dve("tensor_copy", frf[:, :, :], fri[:, :, :], r=[fri], w=[frf])
        k.dve("tensor_tensor", fr[:, :, :], fr[:, :, :], frf[:, :, :], ALU.subtract, r=[fr, frf], w=[fr])
        k.dve("tensor_single_scalar", frf[:, :, :], fr[:, :, :], 0.0, ALU.is_lt, r=[fr], w=[frf])
        k.dve("tensor_tensor", fr[:, :, :], fr[:, :, :], frf[:, :, :], ALU.add, r=[fr, frf], w=[fr])
        k.act("activation", cs[:, :, :], fr[:, :, :], AF.Sin, bias=c.negpi[:, 0:1], scale=2 * float(np.pi) * (1.0 - 1e-6),
              r=[fr, c.negpi], w=[cs])
        rms(k, c, x_, x_[:, :].rearrange("p (g w) -> p g w", g=1), 1, 2048, g_mix[:, :], hn,
            hn[:, :].rearrange("p (g w) -> p g w", g=1), scr, scr2, stat)
        transposes(k, c, [hn[:, kc * P:(kc + 1) * P] for kc in range(16)], hn, hnT,
                   lambda i0, cnt: hnT[:, i0:i0 + cnt, :], BF16, pstr)
        for n, (n0, n1) in enumerate(((0, 512), (512, 1024), (1024, 1088))):
            pz = k.ps[n]
            for kc in range(16):
                k.pe("matmul", pz[:, 0:n1 - n0], hnT[:, kc, :], w_in[:, kc, n0:n1], start=(kc == 0), stop=(kc == 15),
                     r=[hnT, w_in], w=[pz])
            k.act("copy", z[:, n0:n1], pz[:, 0:n1 - n0], r=[pz], w=[z])
        g1_ = lambda a_: a_.rearrange("p (g w) -> p g w", g=1)
        rms_multi(k, c, [
            (z, g1_(z[:, 0:512]), 1, 512, g_ql[:, :], cqn, g1_(cqn[:, 0:512]), sA[0], sA[1], stA),
            (z, g1_(z[:, 512:1024]), 1, 512, g_kvl[:, :], cqn, g1_(cqn[:, 512:1024]), sC[0], sC[1], stC),
            (z, g1_(z[:, 1024:1088]), 1, 64, g_qk[:, 320:384], kr, g1_(kr[:, :]), sB[0], sB[1], stB),
        ])
        rope(kr, lambda hf: kr[:, hf * 32:(hf + 1) * 32].rearrange("p (g w) -> p g w", g=1), 1,
             krb, lambda hf: krb[:, hf * 32:(hf + 1) * 32].rearrange("p (g w) -> p g w", g=1))
        transposes(k, c, [cqn[:, j * P:(j + 1) * P] for j in range(8)], cqn, latT,
                   lambda i0, cnt: latT[:, i0:i0 + cnt, :], BF16, pstr)
        transposes(k, c, [krb[:, :]], krb, krT, lambda i0, cnt: krT[:, :].rearrange("p (a b) -> p a b", a=1), BF16, pstr)
        k.dma("sp", d["KRT"][:, i * P:(i + 1) * P], krT[:, :], r=[krT], sem=krT)
        qflat = qsb[:, :, :].rearrange("p h d -> p (h d)")
        for n in range(3):
            pz = k.ps[3 + n]
            for kc in range(4):
                k.pe("matmul", pz[:, :], latT[:, kc, :], w_uq[:, kc, n * 512:(n + 1) * 512], start=(kc == 0), stop=(kc == 3),
                     r=[latT, w_uq], w=[pz])
            k.act("copy", qflat[:, n * 512:(n + 1) * 512], pz[:, :], r=[pz], w=[qsb])
        kvflat = kvsb[:, :, :].rearrange("p h d -> p (h d)")
        for n in range(4):
            pz = k.ps[(0, 1, 2, 6)[n]]
            for kc in range(4):
                k.pe("matmul", pz[:, :], latT[:, 4 + kc, :], w_ukv[:, kc, n * 512:(n + 1) * 512], start=(kc == 0), stop=(kc == 3),
                     r=[latT, w_ukv], w=[pz])
            k.act("copy", kvflat[:, n * 512:(n + 1) * 512], pz[:, :], r=[pz], w=[kvsb])
        rms_multi(k, c, [
            (qsb, qsb[:, :, 128:192], 8, 64, g_q[:, 128:192], qr, qr[:, :, :], sB[0], sB[1], stB),
            (qsb, qsb[:, :, 0:128], 8, 128, g_q[:, 0:128], qn, qn[:, :, :], sA[0], sA[1], stA),
            (kvsb, kvsb[:, :, 0:128], 8, 128, g_qk[:, 192:320], kn, kn[:, :, :], sC[0], sC[1], stC),
        ])
        rope(qr, lambda hf: qr[:, :, hf * 32:(hf + 1) * 32], 8, qrb, lambda hf: qrb[:, :, hf * 32:(hf + 1) * 32])
        k.pool("tensor_copy", vb[:, :, :], kvsb[:, :, 128:256], r=[kvsb], w=[vb])
        transposes(k, c, [qn[:, h, :] for h in range(8)], qn, qT, lambda i0, cnt: qT[:, i0:i0 + cnt, :], BF16, pstr)
        transposes(k, c, [qrb[:, h, :] for h in range(8)], qrb, qrT, lambda i0, cnt: qrT[:, i0:i0 + cnt, :], BF16, pstr)
        transposes(k, c, [kn[:, h, :] for h in range(8)], kn, kT, lambda i0, cnt: kT[:, i0:i0 + cnt, :], BF16, pstr)
        tok = slice(i * P, (i + 1) * P)
        k.dma("sp", d["QT"][:, :, tok].rearrange("h d t -> d h t"), qT[:, :, :], r=[qT], sem=qT)
        k.dma("sp", d["QRT"][:, :, tok].rearrange("h d t -> d h t"), qrT[:, :, :], r=[qrT], sem=qrT)
        k.dma("sp", d["KT"][:, :, tok].rearrange("h d t -> d h t"), kT[:, :, :], r=[kT], sem=kT)
        k.dma("sp", d["V"][:, tok, :].rearrange("h t d -> t h d"), vb[:, :, :], r=[vb], sem=vb)
    k.end_stage()

    k.begin_stage()
    krt_sb = k.sb("krt_sb", [64, SEQ], BF16)
    k.dma("sp", krt_sb[:, :], d["KRT"][:, :], r=[d["KRT"]], w=[krt_sb], sem=krt_sb)
    sets = []
    for s in range(2):
        st = dict(q=k.sb("aq%d" % s, [P, SEQ], BF16), qr=k.sb("aqr%d" % s, [64, SEQ], BF16),
                  k=k.sb("ak%d" % s, [P, SEQ], BF16), v=k.sb("av%d" % s, [P, NKT, 132], BF16))
        k.dve("memset", st["v"][:, :, 128:129], 1.0, w=[st["v"]])
        sets.append(st)

    def load_head(h, s):
        st = sets[s]
        k.dma("sp", st["q"][:, :], d["QT"][h, :, :], r=[d["QT"]], w=[st["q"]], sem=st["q"])
        k.dma("sp", st["qr"][:, :], d["QRT"][h, :, :], r=[d["QRT"]], w=[st["qr"]], sem=st["qr"])
        k.dma("sp", st["k"][:, :], d["KT"][h, :, :], r=[d["KT"]], w=[st["k"]], sem=st["k"])
        k.dma("sp", st["v"][:, :, 0:128], d["V"][h, :, :].rearrange("(n p) d -> p n d", p=P), r=[d["V"]], w=[st["v"]], sem=st["v"])
        pairs = [
            (st["k"], (lambda kt, b=st["k"]: b[:, kt * P:(kt + 1) * P]), st["q"], (lambda a, e, b=st["q"]: b[:, a:e])),
            (krt_sb, (lambda kt: krt_sb[:, kt * P:(kt + 1) * P]), st["qr"], (lambda a, e, b=st["qr"]: b[:, a:e])),
        ]
        return pairs, st["v"]

    attention(k, c, load_head, None, d["OTX"])
    k.end_stage()

SKIP_UNUSED = True
NBLK = 96
NTOK_T = 16


def stage_WO_MOE(k, c, d, last, upto=4):
    IOA = bass.IndirectOffsetOnAxis
    LOG, MSK, GT, CUM, DESTI, IDXW = c.LOG, c.MSK, c.GT, c.CUM, c.DESTI, c.IDXW
    k.begin_stage()
    w_o = k.sb("w_o", [P, 16, 2048], BF16)
    k.dma("pool", w_o[:, :, :], d["w_o"][:, :].rearrange("(h p) n -> p h n", p=P), r=[d["w_o"]], w=[w_o], sem=w_o)
    g_ffn = k.sb("g_ffn", [P, 2048], F32)
    k.dma("sp", g_ffn[:, :], d["g_ffn"][:, :], r=[d["g_ffn"]], w=[g_ffn], sem=g_ffn)
    w_r = k.sb("w_r", [P, 16, 72], F32)
    k.dma("sp", w_r[:, :, :], d["w_r"][:, :].rearrange("(kc p) n -> p kc n", p=P), r=[d["w_r"]], w=[w_r], sem=w_r)
    b_r = k.sb("b_r", [P, 72], F32)
    k.dma("sp", b_r[:, :], d["b_r"][:, :], r=[d["b_r"]], w=[b_r], sem=b_r)
    ot = [k.sb("ot%d" % i, [P, 16, P], BF16) for i in range(2)]
    xr = [k.sb("xr%d" % i, [P, 2048], F32) for i in range(2)]
    h1 = k.sb("h1", [P, 2048], F32)
    scr = k.sb("scr", [P, 2048], F32)
    scr2 = k.sb("scr2", [P, 2048], F32)
    stat = k.sb("stat", [P, 32], F32)
    hn2 = k.sb("hn2", [P, 2048], BF16)
    hn2T = k.sb("hn2T", [P, 16, P], F32)
    for i in range(NTOK_T):
        tok = slice(i * P, (i + 1) * P)
        o_ = ot[i % 2]
        x_ = xr[i % 2]
        k.dma("sp", o_[:, :, :], d["OTin"][:, :, tok].rearrange("h d t -> d h t"), r=[d["OTin"]], w=[o_], sem=o_)
        k.dma("sp", x_[:, :], d["xres"][tok, :], r=[d["xres"]], w=[x_], sem=x_)
        for n in range(4):
            pz = k.ps[n]
            for h in range(16):
                k.pe("matmul", pz[:, :], o_[:, h, :], w_o[:, h, n * 512:(n + 1) * 512], start=(h == 0), stop=(h == 15),
                     r=[o_, w_o], w=[pz])
            k.dve("tensor_tensor", h1[:, n * 512:(n + 1) * 512], x_[:, n * 512:(n + 1) * 512], pz[:, :], ALU.add,
                  r=[x_, pz], w=[h1])
        k.dma("sp", d["H1s"][tok, :], h1[:, :], r=[h1], sem=h1)
        g1_ = lambda a: a.rearrange("p (g w) -> p g w", g=1)
        rms(k, c, h1, g1_(h1[:, :]), 1, 2048, g_ffn[:, :], scr, g1_(scr[:, :]), scr, scr2, stat)
        k.pool("tensor_copy", hn2[:, :], scr[:, :], r=[scr], w=[hn2])
        k.dma("sp", d["HN2"][tok, :], hn2[:, :], r=[hn2], sem=hn2)
        transposes(k, c, [scr[:, kc * P:(kc + 1) * P] for kc in range(16)], scr, hn2T,
                   lambda i0, cnt: hn2T[:, i0:i0 + cnt, :], F32, k.ps[7])
        pl = k.ps[5]
        for kc in range(16):
            k.pe("matmul", pl[:, 0:72], hn2T[:, kc, :], w_r[:, kc, :], start=(kc == 0), stop=(kc == 15), r=[hn2T, w_r], w=[pl])
        k.dve("tensor_tensor", LOG[:, i, :], pl[:, 0:72], b_r[:, :], ALU.add, r=[pl, b_r], w=[LOG])
    k.end_stage()

    k.begin_stage()
    sm = k.sb("sm", [P, 16], F32)
    ohg = k.sb("ohg", [P, 8], F32)
    eg = k.sb("eg", [P, 8], F32)
    sel = k.sb("sel", [P, 8, 8], F32)
    ein = k.sb("ein", [P, 8], F32)
    oh1 = k.sb("oh1", [P, 8], F32)
    oh2 = k.sb("oh2", [P, 8], F32)
    e2 = k.sb("e2", [P, 8], F32)
    A = k.sb("A", [P, 64], F32)
    carry = k.sb("carry", [P, 64], F32)
    k.dve("memset", carry[:, :], 0.0, w=[carry])
    pc = k.ps[0]
    pc2 = k.ps[1]
    for i in range(NTOK_T):
        gl = LOG[:, i, 0:8]
        el = LOG[:, i, 8:72].rearrange("p (g e) -> p g e", g=8)
        k.dve("tensor_reduce", sm[:, 0:1], gl, AX.X, ALU.max, r=[LOG], w=[sm])
        k.dve("tensor_scalar", sm[:, 1:2], sm[:, 0:1], -1.0, None, ALU.mult, r=[sm], w=[sm])
        k.dve("tensor_scalar", ohg[:, :], gl, sm[:, 0:1], None, ALU.is_equal, r=[LOG, sm], w=[ohg])
        k.act("activation", eg[:, :], gl, AF.Exp, bias=sm[:, 1:2], r=[LOG, sm], w=[eg])
        k.dve("tensor_reduce", sm[:, 2:3], eg[:, :], AX.X, ALU.add, r=[eg], w=[sm])
        k.dve("reciprocal", sm[:, 3:4], sm[:, 2:3], r=[sm], w=[sm])
        k.dve("tensor_tensor", sel[:, :, :], el, bcast(ohg[:, :], 2, 8), ALU.mult, r=[LOG, ohg], w=[sel])
        k.dve("tensor_reduce", ein[:, :], sel[:, :, :].rearrange("p g e -> p e g"), AX.X, ALU.add, r=[sel], w=[ein])
        k.dve("tensor_reduce", sm[:, 4:5], ein[:, :], AX.X, ALU.max, r=[ein], w=[sm])
        k.dve("tensor_scalar", sm[:, 5:6], sm[:, 4:5], -1.0, None, ALU.mult, r=[sm], w=[sm])
        k.dve("tensor_scalar", oh1[:, :], ein[:, :], sm[:, 4:5], None, ALU.is_equal, r=[ein, sm], w=[oh1])
        k.dve("scalar_tensor_tensor", e2[:, :], oh1[:, :], -1e30, ein[:, :], ALU.mult, ALU.add, r=[oh1, ein], w=[e2])
        k.dve("tensor_reduce", sm[:, 6:7], e2[:, :], AX.X, ALU.max, r=[e2], w=[sm])
        k.dve("tensor_scalar", oh2[:, :], e2[:, :], sm[:, 6:7], None, ALU.is_equal, r=[e2, sm], w=[oh2])
        k.act("activation", sm[:, 7:8], sm[:, 6:7], AF.Exp, bias=sm[:, 5:6], r=[sm], w=[sm])
        k.dve("tensor_scalar", sm[:, 8:9], sm[:, 7:8], 1.0, None, ALU.add, r=[sm], w=[sm])
        k.dve("reciprocal", sm[:, 9:10], sm[:, 8:9], r=[sm], w=[sm])
        k.dve("tensor_tensor", GT[:, i, 0:1], sm[:, 3:4], sm[:, 9:10], ALU.mult, r=[sm], w=[GT])
        k.dve("tensor_tensor", GT[:, i, 1:2], GT[:, i, 0:1], sm[:, 7:8], ALU.mult, r=[sm, GT], w=[GT])
        m1 = MSK[:, i, 0, :].rearrange("p (g e) -> p g e", g=8)
        m2 = MSK[:, i, 1, :].rearrange("p (g e) -> p g e", g=8)
        k.dve("tensor_tensor", m1, bcast(ohg[:, :], 2, 8), bcast(oh1[:, :], 1, 8), ALU.mult, r=[ohg, oh1], w=[MSK])
        k.dve("tensor_tensor", m2, bcast(ohg[:, :], 2, 8), bcast(oh2[:, :], 1, 8), ALU.mult, r=[ohg, oh2], w=[MSK])
        k.dve("tensor_tensor", A[:, :], MSK[:, i, 0, :], MSK[:, i, 1, :], ALU.add, r=[MSK], w=[A])
        k.pe("matmul", pc[:, 0:64], c.tri_gt_f[:, :], A[:, :], start=True, stop=True, r=[c.tri_gt_f, A], w=[pc])
        k.dve("tensor_tensor", CUM[:, i, :], pc[:, 0:64], carry[:, :], ALU.add, r=[pc, carry], w=[CUM])
        k.pe("matmul", pc2[:, 0:64], c.ones_f[:, :], A[:, :], start=True, stop=True, r=[c.ones_f, A], w=[pc2])
        k.dve("tensor_tensor", carry[:, :], carry[:, :], pc2[:, 0:64], ALU.add, r=[carry, pc2], w=[carry])
    t1 = k.sb("t1", [P, 64], F32)
    ti = k.sb("ti", [P, 64], I32)
    tf = k.sb("tf", [P, 64], F32)
    pend = k.sb("pend", [P, 64], F32)
    pstart = k.sb("pstart", [P, 64], F32)
    k.dve("tensor_scalar", t1[:, :], carry[:, :], 127.0, 1.0 / 128.0, ALU.add, ALU.mult, r=[carry], w=[t1])
    k.dve("tensor_copy", ti[:, :], t1[:, :], r=[t1], w=[ti])
    k.dve("tensor_copy", tf[:, :], ti[:, :], r=[ti], w=[tf])
    k.dve("tensor_tensor", pend[:, :], tf[:, :], t1[:, :], ALU.is_gt, r=[tf, t1], w=[pend])
    k.dve("tensor_tensor", tf[:, :], tf[:, :], pend[:, :], ALU.subtract, r=[tf, pend], w=[tf])
    k.dve("tensor_scalar", tf[:, :], tf[:, :], 128.0, None, ALU.mult, r=[tf], w=[tf])
    k.dve("tensor_tensor_scan", pend[:, :], c.ones_f[:, 0:64], tf[:, :], 0.0, ALU.mult, ALU.add, r=[c.ones_f, tf], w=[pend])
    k.dve("tensor_tensor", pstart[:, :], pend[:, :], tf[:, :], ALU.subtract, r=[pend, tf], w=[pstart])
    bvi = k.sb("bvi", [P, NBLK], I32)
    k.pool("iota", bvi[:, :], [[128, NBLK]], base=0, channel_multiplier=0, w=[bvi])
    bv = k.sb("bv", [P, NBLK], F32)
    k.dve("tensor_copy", bv[:, :], bvi[:, :], r=[bvi], w=[bv])
    cmp_ = k.sb("cmp", [P, NBLK, 64], F32)
    k.dve("tensor_tensor", cmp_[:, :, :], bcast(pend[:, :], 1, NBLK), bcast(bv[:, :], 2, 64), ALU.is_le, r=[pend, bv], w=[cmp_])
    be = k.sb("be", [P, NBLK], F32)
    k.dve("tensor_reduce", be[:, :], cmp_[:, :, :], AX.X, ALU.add, r=[cmp_], w=[be])
    k.dve("tensor_scalar", be[:, :], be[:, :], 63.0, 128.0, ALU.min, ALU.mult, r=[be], w=[be])
    k.dve("tensor_scalar", be[:, :], be[:, :], c.partidx[:, 0:1], None, ALU.add, r=[be, c.partidx], w=[be])
    if SKIP_UNUSED:
        usd = k.sb("usd", [P, NBLK], F32)
        k.dve("tensor_scalar", usd[:, :], bv[:, :], pend[:, 63:64], None, ALU.is_lt, r=[bv, pend], w=[usd])
        k.dve("tensor_scalar", usd[:, :], usd[:, :], -4194304.0, 4194304.0, ALU.mult, ALU.add, r=[usd], w=[usd])
        k.dve("tensor_tensor", be[:, :], be[:, :], usd[:, :], ALU.add, r=[be, usd], w=[be])
    k.dve("tensor_copy", IDXW[:, :], be[:, :], r=[be], w=[IDXW])
    i16f = k.sb("i16f", [P, NBLK, 16], F32)
    k.dve("scalar_tensor_tensor", i16f[:, :, 0:8], bcast(be[:, :], 2, 8), 8.0, bcast(c.colidx[:, 0:8], 1, NBLK), ALU.mult, ALU.add,
          r=[be, c.colidx], w=[i16f])
    k.dve("tensor_copy", c.IDX16[:, :, 0:8], i16f[:, :, 0:8], r=[i16f], w=[c.IDX16])
    k.dve("scalar_tensor_tensor", i16f[:, :, 0:4], bcast(be[:, :], 2, 4), 4.0, bcast(c.colidx[:, 0:4], 1, NBLK), ALU.mult, ALU.add,
          r=[be, c.colidx], w=[i16f])
    k.dve("tensor_copy", c.IDX4[:, :, :], i16f[:, :, 0:4], r=[i16f], w=[c.IDX4])
    dsel = k.sb("dsel", [P, 64], F32)
    pc_ = k.sb("pc_", [P, 64], F32)
    destf = k.sb("destf", [P, 2], F32)
    hnt = [k.sb("hnt%d" % i, [P, 2048], BF16) for i in range(2)]
    for i in range(NTOK_T):
        tok = slice(i * P, (i + 1) * P)
        k.dve("tensor_tensor", pc_[:, :], pstart[:, :], CUM[:, i, :], ALU.add, r=[pstart, CUM], w=[pc_])
        for s in range(2):
            k.dve("tensor_tensor", dsel[:, :], pc_[:, :], MSK[:, i, s, :], ALU.mult, r=[pc_, MSK], w=[dsel])
            k.dve("tensor_reduce", destf[:, s:s + 1], dsel[:, :], AX.X, ALU.add, r=[dsel], w=[destf])
        k.dve("tensor_copy", DESTI[:, i, :], destf[:, :], r=[destf], w=[DESTI])
        ht = hnt[i % 2]
        k.dma("sp", ht[:, :], d["HN2"][tok, :], r=[d["HN2"]], w=[ht], sem=ht)
        for s in range(2):
            k.idma(d["XBUF"][:, :], IOA(ap=DESTI[:, i, s:s + 1], axis=0), ht[:, :], None,
                   r=[ht, DESTI], sem=ht)
    if "DBG" in d:
        k.dma("sp", d["DBG"][:, 0:32], DESTI[:, :, :].rearrange("p a b -> p (a b)"), r=[DESTI], sem=DESTI)
        k.dma("sp", d["DBG"][:, 32:128], IDXW[:, :], r=[IDXW], sem=IDXW)
        k.dma("sp", d["DBGF"][:, 0:32], GT[:, :, :].rearrange("p a b -> p (a b)"), r=[GT], sem=GT)
        k.dma("sp", d["DBGF"][:, 32:32 + 1152], LOG[:, :, :].rearrange("p a b -> p (a b)"), r=[LOG], sem=LOG)
        k.dma("sp", d["DBGF"][:, 1184:1184 + 64], pend[:, :], r=[pend], sem=pend)
        k.dma("sp", d["DBGF"][:, 1248:1248 + 64], carry[:, :], r=[carry], sem=carry)
    k.end_stage()
    if upto <= 2:
        return

    k.begin_stage()
    wgu = [[k.sb("wgu%d_%d" % (i, cc_), [P, 2, 1024], BF16) for cc_ in range(8)] for i in range(2)]
    wdn = [[k.sb("wdn%d_%d" % (i, cc_), [P, 2048], BF16) for cc_ in range(4)] for i in range(2)]
    xb = [k.sb("xb%d" % i, [P, 2048], BF16) for i in range(2)]
    xbT = k.sb("xbT", [P, 16, P], BF16)
    sgl = k.sb("sgl", [P, 512], F32)
    actb = k.sb("actb", [P, 512], BF16)
    actT = k.sb("actT", [P, 4, P], BF16)
    ysb = [k.sb("ysb%d" % i, [P, 2048], F32) for i in range(2)]
    if SKIP_UNUSED:
        reg_gu = k.nc.gpsimd.to_reg(64 * 1024 - 1)
        reg_dn = k.nc.gpsimd.to_reg(64 * 512 - 1)
    wgu_src = d["w_gu"][:, :, :].rearrange("e (r two) n -> (e r) (two n)", two=2)
    wdn_src = d["w_dn"][:, :, :].rearrange("e r n -> (e r) n")

    def load_blk(b):
        s = b % 2
        for cc_ in range(8):
            k.idma(wgu[s][cc_][:, :, :].rearrange("p a n -> p (a n)"), None, wgu_src,
                   IOA(ap=c.IDX16[:, b, cc_:cc_ + 1], axis=0), r=[c.IDX16], w=[wgu[s][cc_]], sem=wgu[s][cc_],
                   **(dict(bounds_check=reg_gu, oob_is_err=False) if SKIP_UNUSED else {}))
        for kc in range(4):
            k.idma(wdn[s][kc][:, :], None, wdn_src, IOA(ap=c.IDX4[:, b, kc:kc + 1], axis=0), r=[c.IDX4], w=[wdn[s][kc]], sem=wdn[s][kc],
                   **(dict(bounds_check=reg_dn, oob_is_err=False) if SKIP_UNUSED else {}))
        k.dma("sp", xb[s][:, :], d["XBUF"][b * P:(b + 1) * P, :], r=[d["XBUF"]], w=[xb[s]], sem=xb[s])

    load_blk(0)
    for b in range(NBLK):
        s = b % 2
        if b + 1 < NBLK:
            load_blk(b + 1)
        xv = xb[s][:, :].rearrange("t (p kc) -> t p kc", kc=16)
        transposes(k, c, [xv[:, :, kc] for kc in range(16)], xb[s], xbT, lambda i0, cnt: xbT[:, i0:i0 + cnt, :], BF16, k.ps[7])
        for n in range(2):
            pz = k.ps[n]
            for kc in range(16):
                k.pe("matmul", pz[:, :], xbT[:, kc, :], wgu[s][kc // 2][:, kc % 2, n * 512:(n + 1) * 512], start=(kc == 0), stop=(kc == 15),
                     r=[xbT, wgu[s][kc // 2]], w=[pz])
        k.act("activation", sgl[:, :], k.ps[0][:, :], AF.Silu, r=[k.ps[0]], w=[sgl])
        k.dve("tensor_tensor", actb[:, :], sgl[:, :], k.ps[1][:, :], ALU.mult, r=[sgl, k.ps[1]], w=[actb])
        av = actb[:, :].rearrange("t (p kc) -> t p kc", kc=4)
        transposes(k, c, [av[:, :, kc] for kc in range(4)], actb, actT, lambda i0, cnt: actT[:, i0:i0 + cnt, :], BF16, k.ps[6])
        y_ = ysb[s]
        for n in range(4):
            pz = k.ps[2 + n]
            for kc in range(4):
                k.pe("matmul", pz[:, :], actT[:, kc, :], wdn[s][kc][:, n * 512:(n + 1) * 512], start=(kc == 0), stop=(kc == 3),
                     r=[actT, wdn[s][kc]], w=[pz])
            if n % 2 == 0:
                k.dve("tensor_copy", y_[:, n * 512:(n + 1) * 512], pz[:, :], r=[pz], w=[y_])
            else:
                k.act("copy", y_[:, n * 512:(n + 1) * 512], pz[:, :], r=[pz], w=[y_])
        k.dma("sp", d["YBUF"][b * P:(b + 1) * P, :], y_[:, :], r=[y_], sem=y_)
    k.end_stage()

    k.begin_stage()
    hh = [k.sb("hh%d" % i, [P, 2048], F32) for i in range(2)]
    y1 = [k.sb("y1_%d" % i, [P, 2048], F32) for i in range(2)]
    y2 = [k.sb("y2_%d" % i, [P, 2048], F32) for i in range(2)]
    scr = k.sb("scr", [P, 2048], F32)
    scr2 = k.sb("scr2", [P, 2048], F32)
    stat = k.sb("stat", [P, 32], F32)
    hnb = k.sb("hnb", [P, 2048], BF16)
    if not last:
        g_nx = k.sb("g_nx", [P, 2048], F32)
        k.dma("sp", g_nx[:, :], d["g_next"][:, :], r=[d["g_next"]], w=[g_nx], sem=g_nx)
    for i in range(NTOK_T):
        tok = slice(i * P, (i + 1) * P)
        h_ = hh[i % 2]
        a_ = y1[i % 2]
        b_ = y2[i % 2]
        k.dma("sp", h_[:, :], d["H1s"][tok, :], r=[d["H1s"]], w=[h_], sem=h_)
        k.idma(a_[:, :], None, d["YBUF"][:, :], IOA(ap=DESTI[:, i, 0:1], axis=0), r=[d["YBUF"], DESTI], w=[a_], sem=a_)
        k.idma(b_[:, :], None, d["YBUF"][:, :], IOA(ap=DESTI[:, i, 1:2], axis=0), r=[d["YBUF"], DESTI], w=[b_], sem=b_)
        k.dve("scalar_tensor_tensor", h_[:, :], a_[:, :], GT[:, i, 0:1], h_[:, :], ALU.mult, ALU.add, r=[a_, GT, h_], w=[h_])
        k.dve("scalar_tensor_tensor", h_[:, :], b_[:, :], GT[:, i, 1:2], h_[:, :], ALU.mult, ALU.add, r=[b_, GT, h_], w=[h_])
        k.dma("sp", d["HOUT"][tok, :], h_[:, :], r=[h_], sem=h_)
        if not last:
            g1_ = lambda a: a.rearrange("p (g w) -> p g w", g=1)
            rms(k, c, h_, g1_(h_[:, :]), 1, 2048, g_nx[:, :], hnb, g1_(hnb[:, :]), scr, scr2, stat)
            k.dma("sp", d["HN"][tok, :], hnb[:, :], r=[hnb], sem=hnb)
    k.end_stage()

def stage_C(k, c, d):
    SCALE = 1.0 / float(np.sqrt(128.0))
    NT = SEQ // P
    g1_ = lambda a: a.rearrange("p (g w) -> p g w", g=1)
    k.begin_stage()
    g_qk = k.sb("g_qk", [P, 256], F32)
    k.dma("sp", g_qk[:, :], d["g_qk"][:, :], r=[d["g_qk"]], w=[g_qk], sem=g_qk)
    g_q = k.sb("g_q", [P, 128], F32)
    k.dve("tensor_scalar", g_q[:, :], g_qk[:, 0:128], SCALE, None, ALU.mult, r=[g_qk], w=[g_q])
    w_qk = k.sb("w_qk", [P, 16, 2048], BF16)
    k.dma("pool", w_qk[:, :, :], d["w_qk"][:, :].rearrange("(kc p) n -> p kc n", p=P), r=[d["w_qk"]], w=[w_qk], sem=w_qk)
    hn = [k.sb("hn%d" % i, [P, 2048], BF16) for i in range(2)]
    hnT = k.sb("hnT", [P, 16, P], BF16)
    zsb = k.sb("zsb", [P, 16, P], F32)
    scr = k.sb("scr", [P, 2048], F32)
    scr2 = k.sb("scr2", [P, 2048], F32)
    stat = k.sb("stat", [P, 32], F32)
    scrb = k.sb("scrb", [P, 1024], F32)
    scr2b = k.sb("scr2b", [P, 1024], F32)
    statb = k.sb("statb", [P, 32], F32)
    qn = k.sb("qn", [P, 8, P], BF16)
    kn = k.sb("kn", [P, 8, P], BF16)
    qT = k.sb("qT", [P, 8, P], BF16)
    kT = k.sb("kT", [P, 8, P], BF16)
    zflat = zsb[:, :, :].rearrange("p h d -> p (h d)")
    for i in range(NT):
        tok = slice(i * P, (i + 1) * P)
        h_ = hn[i % 2]
        k.dma("sp", h_[:, :], d["HNf"][tok, :], r=[d["HNf"]], w=[h_], sem=h_)
        transposes(k, c, [h_[:, kc * P:(kc + 1) * P] for kc in range(16)], h_, hnT, lambda i0, cnt: hnT[:, i0:i0 + cnt, :], BF16, k.ps[7])
        for n in range(4):
            pz = k.ps[n]
            for kc in range(16):
                k.pe("matmul", pz[:, :], hnT[:, kc, :], w_qk[:, kc, n * 512:(n + 1) * 512], start=(kc == 0), stop=(kc == 15),
                     r=[hnT, w_qk], w=[pz])
            k.act("copy", zflat[:, n * 512:(n + 1) * 512], pz[:, :], r=[pz], w=[zsb])
        rms_multi(k, c, [
            (zsb, zsb[:, 0:8, :], 8, 128, g_q[:, :], qn, qn[:, :, :], scr, scr2, stat),
            (zsb, zsb[:, 8:16, :], 8, 128, g_qk[:, 128:256], kn, kn[:, :, :], scrb, scr2b, statb),
        ])
        transposes(k, c, [qn[:, h, :] for h in range(8)], qn, qT, lambda i0, cnt: qT[:, i0:i0 + cnt, :], BF16, k.ps[6])
        transposes(k, c, [kn[:, h, :] for h in range(8)], kn, kT, lambda i0, cnt: kT[:, i0:i0 + cnt, :], BF16, k.ps[5])
        k.dma("sp", d["QT"][:, :, tok].rearrange("h d t -> d h t"), qT[:, :, :], r=[qT], sem=qT)
        k.dma("sp", d["KT"][:, :, tok].rearrange("h d t -> d h t"), kT[:, :, :], r=[kT], sem=kT)
    k.end_stage()

    k.begin_stage()
    w_vg = k.sb("w_vg", [P, 16, 2056], BF16)
    k.dma("pool", w_vg[:, :, :], d["w_vg"][:, :].rearrange("(kc p) n -> p kc n", p=P), r=[d["w_vg"]], w=[w_vg], sem=w_vg)
    fb = k.sb("fb", [P, 8], F32)
    k.dma("sp", fb[:, :], d["fbias"][:, :], r=[d["fbias"]], w=[fb], sem=fb)
    hn = [k.sb("hn%d" % i, [P, 2048], BF16) for i in range(2)]
    hnT = k.sb("hnT", [P, 16, P], BF16)
    vb = k.sb("vb", [P, 8, P], BF16)
    sgb = k.sb("sgb", [P, 1024], BF16)
    fx = k.sb("fx", [P, 8], F32)
    fa = k.sb("fa", [P, 8], F32)
    fe = k.sb("fe", [P, 8], F32)
    fl = k.sb("fl", [P, 8], F32)
    fm = k.sb("fm", [P, 8], F32)
    lf = k.sb("lf", [P, 8], F32)
    cc = k.sb("cc", [P, 8], F32)
    hf = k.sb("hf", [P, 8], F32)
    r1 = k.sb("r1", [P, 8], F32)
    carry = k.sb("carryc", [P, 8], F32)
    k.dve("memset", carry[:, :], 0.0, w=[carry])
    caq = k.sb("caq", [P, 8, 6], BF16)
    cak = k.sb("cak", [P, 8, 6], BF16)
    k.dve("memset", caq[:, :, :], 1.0, w=[caq])
    k.dve("memset", cak[:, :, :], 1.0, w=[cak])
    caqT = k.sb("caqT", [48, P], BF16)
    cakT = k.sb("cakT", [48, P], BF16)
    vflat = vb[:, :, :].rearrange("p h d -> p (h d)")
    for i in range(NT):
        tok = slice(i * P, (i + 1) * P)
        h_ = hn[i % 2]
        k.dma("sp", h_[:, :], d["HNf"][tok, :], r=[d["HNf"]], w=[h_], sem=h_)
        transposes(k, c, [h_[:, kc * P:(kc + 1) * P] for kc in range(16)], h_, hnT, lambda i0, cnt: hnT[:, i0:i0 + cnt, :], BF16, k.ps[7])
        for n in range(5):
            pz = k.ps[n]
            n0 = n * 512
            n1 = min(n0 + 512, 2056)
            for kc in range(16):
                k.pe("matmul", pz[:, 0:n1 - n0], hnT[:, kc, :], w_vg[:, kc, n0:n1], start=(kc == 0), stop=(kc == 15),
                     r=[hnT, w_vg], w=[pz])
            if n < 2:
                k.act("copy", vflat[:, n0:n1], pz[:, :], r=[pz], w=[vb])
            elif n < 4:
                k.act("activation", sgb[:, n0 - 1024:n1 - 1024], pz[:, :], AF.Sigmoid, r=[pz], w=[sgb])
            else:
                k.dve("tensor_tensor", fx[:, :], pz[:, 0:8], fb[:, :], ALU.add, r=[pz, fb], w=[fx])
        k.dma("sp", d["V"][:, tok, :].rearrange("h t d -> t h d"), vb[:, :, :], r=[vb], sem=vb)
        k.dma("sp", d["SG"][tok, :], sgb[:, :], r=[sgb], sem=sgb)
        k.dve("tensor_scalar", fa[:, :], fx[:, :], -1.0, None, ALU.mult, r=[fx], w=[fa])
        k.dve("tensor_tensor", fa[:, :], fa[:, :], fx[:, :], ALU.max, r=[fa, fx], w=[fa])
        k.act("activation", fe[:, :], fa[:, :], AF.Exp, scale=-1.0, r=[fa], w=[fe])
        k.act("activation", fl[:, :], fe[:, :], AF.Ln, bias=c.one[:, 0:1], r=[fe, c.one], w=[fl])
        k.dve("tensor_scalar", fm[:, :], fx[:, :], 0.0, None, ALU.min, r=[fx], w=[fm])
        k.dve("tensor_tensor", lf[:, :], fm[:, :], fl[:, :], ALU.subtract, r=[fm, fl], w=[lf])
        pc = k.ps[5]
        pc2 = k.ps[6]
        k.pe("matmul", pc[:, 0:8], c.tri_ge_f[:, :], lf[:, :], start=True, stop=True, r=[c.tri_ge_f, lf], w=[pc])
        k.dve("tensor_tensor", cc[:, :], pc[:, 0:8], carry[:, :], ALU.add, r=[pc, carry], w=[cc])
        k.pe("matmul", pc2[:, 0:8], c.ones_f[:, :], lf[:, :], start=True, stop=True, r=[c.ones_f, lf], w=[pc2])
        k.dve("tensor_tensor", carry[:, :], carry[:, :], pc2[:, 0:8], ALU.add, r=[carry, pc2], w=[carry])
        k.dve("tensor_copy", caq[:, :, 3], cc[:, :], r=[cc], w=[caq])
        k.dve("tensor_copy", hf[:, :], caq[:, :, 3], r=[caq], w=[hf])
        k.dve("tensor_scalar", cak[:, :, 0], hf[:, :], -1.0, None, ALU.mult, r=[hf], w=[cak])
        k.dve("tensor_tensor", r1[:, :], cc[:, :], hf[:, :], ALU.subtract, r=[cc, hf], w=[r1])
        k.dve("tensor_copy", caq[:, :, 4], r1[:, :], r=[r1], w=[caq])
        k.dve("tensor_copy", hf[:, :], caq[:, :, 4], r=[caq], w=[hf])
        k.dve("tensor_scalar", cak[:, :, 1], hf[:, :], -1.0, None, ALU.mult, r=[hf], w=[cak])
        k.dve("tensor_tensor", r1[:, :], r1[:, :], hf[:, :], ALU.subtract, r=[r1, hf], w=[r1])
        k.dve("tensor_copy", caq[:, :, 5], r1[:, :], r=[r1], w=[caq])
        k.dve("tensor_scalar", cak[:, :, 2], caq[:, :, 5], -1.0, None, ALU.mult, r=[caq], w=[cak])
        transposes(k, c, [caq[:, :, :].rearrange("p h i -> p (h i)")], caq, caqT,
                   lambda i0, cnt: caqT[:, :].rearrange("p (a b) -> p a b", a=1), BF16, k.ps[7])
        transposes(k, c, [cak[:, :, :].rearrange("p h i -> p (h i)")], cak, cakT,
                   lambda i0, cnt: cakT[:, :].rearrange("p (a b) -> p a b", a=1), BF16, k.ps[7])
        k.dma("sp", d["CAQ"][:, tok], caqT[:, :], r=[caqT], sem=caqT)
        k.dma("sp", d["CAK"][:, tok], cakT[:, :], r=[cakT], sem=cakT)
    k.end_stage()

    k.begin_stage()
    sets = []
    for s in range(2):
        st = dict(q=k.sb("aq%d" % s, [P, SEQ], BF16), k=k.sb("ak%d" % s, [P, SEQ], BF16),
                  cq=k.sb("acq%d" % s, [6, SEQ], BF16), ck=k.sb("ack%d" % s, [6, SEQ], BF16),
                  v=k.sb("av%d" % s, [P, NKT, 132], BF16))
        k.dve("memset", st["v"][:, :, 128:129], 1.0, w=[st["v"]])
        sets.append(st)

    def load_head(h, s):
        st = sets[s]
        k.dma("sp", st["q"][:, :], d["QT"][h, :, :], r=[d["QT"]], w=[st["q"]], sem=st["q"])
        k.dma("sp", st["k"][:, :], d["KT"][h, :, :], r=[d["KT"]], w=[st["k"]], sem=st["k"])
        k.dma("sp", st["cq"][:, :], d["CAQ"][h * 6:(h + 1) * 6, :], r=[d["CAQ"]], w=[st["cq"]], sem=st["cq"])
        k.dma("sp", st["ck"][:, :], d["CAK"][h * 6:(h + 1) * 6, :], r=[d["CAK"]], w=[st["ck"]], sem=st["ck"])
        k.dma("sp", st["v"][:, :, 0:128], d["V"][h, :, :].rearrange("(n p) d -> p n d", p=P), r=[d["V"]], w=[st["v"]], sem=st["v"])
        pairs = [
            (st["k"], (lambda kt, b=st["k"]: b[:, kt * P:(kt + 1) * P]), st["q"], (lambda a, e, b=st["q"]: b[:, a:e])),
            (st["ck"], (lambda kt, b=st["ck"]: b[:, kt * P:(kt + 1) * P]), st["cq"], (lambda a, e, b=st["cq"]: b[:, a:e])),
        ]
        return pairs, st["v"]

    attention(k, c, load_head, None, d["OTX"], gate_d=d["SG"])
    k.end_stage()
def rep128(v):
    v = np.ascontiguousarray(np.asarray(v, dtype=np.float32).reshape(1, -1))
    return np.ascontiguousarray(np.broadcast_to(v, (P, v.shape[1])))


def new_nc():
    return bass.Bass("TRN2", target_bir_lowering=False)


def build_A():
    nc = new_nc()
    k = K(nc)
    d = {}
    def ext(name, shape, dt):
        d[name] = k.dram(name, shape, dt, kind="ExternalInput")
    ext("x", [4096, 2048], F32); ext("pos", [4096, 1], I32)
    ext("g_mix", [P, 2048], F32); ext("g_ql", [P, 512], F32); ext("g_kvl", [P, 512], F32)
    ext("g_qk", [P, 384], F32); ext("freqs", [P, 32], F32)
    ext("w_in", [2048, 1088], F32); ext("w_uq", [512, 1536], F32); ext("w_ukv", [512, 2048], F32)
    d["KRT"] = k.dram("KRT", [64, 4096], BF16)
    d["QT"] = k.dram("QT", [8, 128, 4096], BF16)
    d["QRT"] = k.dram("QRT", [8, 64, 4096], BF16)
    d["KT"] = k.dram("KT", [8, 128, 4096], BF16)
    d["V"] = k.dram("V", [8, 4096, 128], BF16)
    d["OTX"] = k.dram("OTX", [2, 8, 128, 2048], BF16, kind="ExternalOutput")
    k.begin_stage()
    c = make_consts(k)
    k.end_stage()
    stage_A(k, c, d)
    k.finish()
    return nc


def inputs_A(inp, core):
    b, j = core // 2, core % 2
    half = 32
    freqs = (10000.0 ** (-np.arange(half, dtype=np.float32) / half)).astype(np.float32)
    uq = inp["l0_mla_w_uq"].reshape(512, 16, 192)[:, 8 * j:8 * j + 8, :].reshape(512, 1536)
    ukv = inp["l0_mla_w_ukv"].reshape(512, 16, 256)[:, 8 * j:8 * j + 8, :].reshape(512, 2048)
    return {
        "x": np.ascontiguousarray(inp["x"][b]),
        "pos": np.ascontiguousarray(inp["positions"][b].reshape(4096, 1).astype(np.int32)),
        "g_mix": rep128(inp["l0_norm_mix"]), "g_ql": rep128(inp["l0_mla_q_lat_norm"]),
        "g_kvl": rep128(inp["l0_mla_kv_lat_norm"]), "g_qk": rep128(inp["l0_mla_qk_gain"].reshape(-1)),
        "freqs": rep128(freqs),
        "w_in": np.ascontiguousarray(inp["l0_mla_w_in"]), "w_uq": np.ascontiguousarray(uq), "w_ukv": np.ascontiguousarray(ukv),
    }


def build_B(layer, upto=4, dbg=False):
    last = (layer == 1)
    nc = new_nc()
    k = K(nc)
    d = {}
    def ext(name, shape, dt):
        d[name] = k.dram(name, shape, dt, kind="ExternalInput")
    ext("OTin", [16, 128, 2048], BF16); ext("xres", [2048, 2048], F32); ext("w_o", [2048, 2048], F32)
    ext("g_ffn", [P, 2048], F32); ext("w_r", [2048, 72], F32); ext("b_r", [P, 72], F32)
    if upto > 2:
        ext("w_gu", [64, 2048, 1024], F32); ext("w_dn", [64, 512, 2048], F32)
    if dbg:
        d["DBG"] = k.dram("DBG", [P, 128], I32, kind="ExternalOutput")
        d["DBGF"] = k.dram("DBGF", [P, 1312], F32, kind="ExternalOutput")
    if not last:
        ext("g_next", [P, 2048], F32)
        d["HN"] = k.dram("HN", [2048, 2048], BF16, kind="ExternalOutput")
    d["H1s"] = k.dram("H1s", [2048, 2048], F32)
    d["HN2"] = k.dram("HN2", [2048, 2048], BF16)
    d["XBUF"] = k.dram("XBUF", [NBLK * P, 2048], BF16)
    d["YBUF"] = k.dram("YBUF", [NBLK * P, 2048], F32)
    d["HOUT"] = k.dram("HOUT", [2048, 2048], F32, kind="ExternalOutput")
    k.begin_stage()
    c = make_consts(k)
    k.end_stage()
    stage_WO_MOE(k, c, d, last, upto)
    k.finish()
    return nc


def inputs_B(inp, core, layer, otin, xres):
    pre = "l%d_" % layer
    wo = inp["l0_mla_w_o"] if layer == 0 else inp["l1_fox_w_o"]
    m = {
        "OTin": (None if otin is None else np.ascontiguousarray(otin)), "xres": (None if xres is None else np.ascontiguousarray(xres)), "w_o": np.ascontiguousarray(wo),
        "g_ffn": rep128(inp[pre + "norm_ffn"]),
        "w_r": np.ascontiguousarray(np.concatenate([inp[pre + "router_group"], inp[pre + "router_expert"]], axis=1)),
        "b_r": rep128(np.concatenate([inp[pre + "router_group_bias"], inp[pre + "router_expert_bias"]])),
        "w_gu": np.ascontiguousarray(inp[pre + "w_gate_up"]), "w_dn": np.ascontiguousarray(inp[pre + "w_down"]),
    }
    if layer == 0:
        m["g_next"] = rep128(inp["l1_norm_mix"])
    return m


def build_C():
    nc = new_nc()
    k = K(nc)
    d = {}
    def ext(name, shape, dt):
        d[name] = k.dram(name, shape, dt, kind="ExternalInput")
    ext("HNf", [4096, 2048], BF16); ext("w_qk", [2048, 2048], F32); ext("w_vg", [2048, 2056], F32)
    ext("fbias", [P, 8], F32); ext("g_qk", [P, 256], F32)
    d["QT"] = k.dram("QT", [8, 128, 4096], BF16)
    d["KT"] = k.dram("KT", [8, 128, 4096], BF16)
    d["V"] = k.dram("V", [8, 4096, 128], BF16)
    d["SG"] = k.dram("SG", [4096, 1024], BF16)
    d["CAQ"] = k.dram("CAQ", [48, 4096], BF16)
    d["CAK"] = k.dram("CAK", [48, 4096], BF16)
    d["OTX"] = k.dram("OTX", [2, 8, 128, 2048], BF16, kind="ExternalOutput")
    k.begin_stage()
    c = make_consts(k)
    k.end_stage()
    stage_C(k, c, d)
    k.finish()
    return nc


def inputs_C(inp, core, hnf):
    j = core % 2
    w = inp["l1_fox_w_in"]
    o = 1024 * j
    w_qk = np.concatenate([w[:, o:o + 1024], w[:, 2048 + o:2048 + o + 1024]], axis=1)
    w_vg = np.concatenate([w[:, 4096 + o:4096 + o + 1024], w[:, 6160 + o:6160 + o + 1024], w[:, 6144 + 8 * j:6144 + 8 * j + 8]], axis=1)
    return {
        "HNf": (None if hnf is None else np.ascontiguousarray(hnf)), "w_qk": np.ascontiguousarray(w_qk), "w_vg": np.ascontiguousarray(w_vg),
        "fbias": rep128(inp["l1_fox_forget_bias"][8 * j:8 * j + 8]), "g_qk": rep128(inp["l1_fox_qk_gain"].reshape(-1)),
    }


def _run(nc, maps):
    return run_bass_kernel_spmd(nc, maps, core_ids=list(range(8))).results


def build_F():
    nc = new_nc()
    k = K(nc)
    def ext(name, shape, dt):
        return k.dram(name, shape, dt, kind="ExternalInput")
    def view(ap, name):
        return Buf(ap, name)
    x = ext("x", [4096, 2048], F32)
    base_A = dict(x=x, pos=ext("pos", [4096, 1], I32), g_mix=ext("g_mix", [P, 2048], F32), g_ql=ext("g_ql", [P, 512], F32),
                  g_kvl=ext("g_kvl", [P, 512], F32), g_qk=ext("g_qk", [P, 384], F32), freqs=ext("freqs", [P, 32], F32),
                  w_in=ext("w_in", [2048, 1088], F32))
    w_uq = ext("w_uq", [512, 3072], F32)
    w_ukv = ext("w_ukv", [512, 4096], F32)
    base_A["KRT"] = k.dram("KRT", [64, 4096], BF16)
    base_A["QT"] = k.dram("QT", [8, 128, 4096], BF16)
    base_A["QRT"] = k.dram("QRT", [8, 64, 4096], BF16)
    base_A["KT"] = k.dram("KT", [8, 128, 4096], BF16)
    base_A["V"] = k.dram("V", [8, 4096, 128], BF16)
    otf = k.dram("OTF", [16, 128, 4096], BF16)
    shared = dict(H1s=k.dram("H1s", [2048, 2048], F32), HN2=k.dram("HN2", [2048, 2048], BF16),
                  XBUF=k.dram("XBUF", [NBLK * P, 2048], BF16), YBUF=k.dram("YBUF", [NBLK * P, 2048], F32))
    h2 = k.dram("H2", [4096, 2048], F32)
    hnf = k.dram("HNF", [4096, 2048], BF16)
    out = k.dram("out", [2048, 2048], F32, kind="ExternalOutput")
    otin_own = k.dram("OTIN_OWN", [16, 128, 2048], BF16)
    xres_own = k.dram("XRES_OWN", [2048, 2048], F32)
    idxo_d = ext("idxo", [P, 16], I32)
    idxr_d = ext("idxr", [P, 16], I32)

    def moe_w(layer):
        p_ = "l%d_" % layer
        return dict(w_o=ext(p_ + "w_o", [2048, 2048], F32), g_ffn=ext(p_ + "g_ffn", [P, 2048], F32),
                    w_r=ext(p_ + "w_r", [2048, 72], F32), b_r=ext(p_ + "b_r", [P, 72], F32),
                    w_gu=ext(p_ + "w_gu", [64, 2048, 1024], F32), w_dn=ext(p_ + "w_dn", [64, 512, 2048], F32))
    mw0 = moe_w(0)
    mw1 = moe_w(1)
    g_next = ext("g_next", [P, 2048], F32)
    cw = [dict(w_qk=ext("w_qk%d" % hh, [2048, 2048], F32), w_vg=ext("w_vg%d" % hh, [2048, 2056], F32),
               fbias=ext("fbias%d" % hh, [P, 8], F32)) for hh in range(2)]
    l1_g_qk = ext("l1_g_qk", [P, 256], F32)
    sg = k.dram("SG", [4096, 1024], BF16)
    caq = k.dram("CAQ", [48, 4096], BF16)
    cak = k.dram("CAK", [48, 4096], BF16)

    k.begin_stage()
    c = make_consts(k)
    idxo = k.sb("idxo_sb", [P, 16], I32, persist=True)
    idxr = k.sb("idxr_sb", [P, 16], I32, persist=True)
    k.dma("sp", idxo[:, :], idxo_d[:, :], w=[idxo], sem=idxo)
    k.dma("sp", idxr[:, :], idxr_d[:, :], w=[idxr], sem=idxr)
    k.end_stage()

    def otx_view(hh):
        return view(otf.t[hh * 8:(hh + 1) * 8, :, :].rearrange("h d (a t) -> a h d t", a=2), "otxv")

    for hh in range(2):
        dA = dict(base_A)
        dA["w_uq"] = view(w_uq.t[:, hh * 1536:(hh + 1) * 1536], "w_uq_v")
        dA["w_ukv"] = view(w_ukv.t[:, hh * 2048:(hh + 1) * 2048], "w_ukv_v")
        dA["OTX"] = otx_view(hh)
        stage_A(k, c, dA)
    for th in range(2):
        ts = slice(th * 2048, (th + 1) * 2048)
        dB = dict(shared)
        dB.update(mw0)
        dB["OTin"] = view(otf.t[:, :, ts], "otin_v")
        dB["xres"] = view(x.t[ts, :], "xres_v")
        dB["g_next"] = g_next
        dB["HOUT"] = view(h2.t[ts, :], "h2_v")
        dB["HN"] = view(hnf.t[ts, :], "hn_v")
        stage_WO_MOE(k, c, dB, False)
    for hh in range(2):
        dC = dict(HNf=hnf, QT=base_A["QT"], KT=base_A["KT"], V=base_A["V"], SG=sg, CAQ=caq, CAK=cak, g_qk=l1_g_qk)
        dC.update(cw[hh])
        dC["OTX"] = otx_view(hh)
        stage_C(k, c, dC)
    IOA = bass.IndirectOffsetOnAxis
    k.begin_stage()
    tb_ = [k.sb("selb%d" % i, [P, 2048], BF16) for i in range(2)]
    tf_ = [k.sb("self%d" % i, [P, 2048], F32) for i in range(2)]
    otf2d = otf.t.rearrange("h d (a t) -> (h d a) t", a=2)
    for hh in range(16):
        t_ = tb_[hh % 2]
        k.idma(t_[:, :], None, otf2d, IOA(ap=idxo[:, hh:hh + 1], axis=0), r=[idxo], w=[t_], sem=t_)
        k.dma("sp", otin_own.t[hh, :, :], t_[:, :], r=[t_], sem=t_)
    for i in range(16):
        t_ = tf_[i % 2]
        k.idma(t_[:, :], None, h2.t[:, :], IOA(ap=idxr[:, i:i + 1], axis=0), r=[idxr], w=[t_], sem=t_)
        k.dma("sp", xres_own.t[i * P:(i + 1) * P, :], t_[:, :], r=[t_], sem=t_)
    k.end_stage()
    dD = dict(shared)
    dD.update(mw1)
    dD["OTin"] = otin_own
    dD["xres"] = xres_own
    dD["HOUT"] = out
    stage_WO_MOE(k, c, dD, True)
    k.finish()
    return nc


def inputs_F(inp, core):
    b, j = core // 2, core % 2
    half = 32
    freqs = (10000.0 ** (-np.arange(half, dtype=np.float32) / half)).astype(np.float32)
    m = {
        "x": np.ascontiguousarray(inp["x"][b]),
        "pos": np.ascontiguousarray(inp["positions"][b].reshape(4096, 1).astype(np.int32)),
        "g_mix": rep128(inp["l0_norm_mix"]), "g_ql": rep128(inp["l0_mla_q_lat_norm"]),
        "g_kvl": rep128(inp["l0_mla_kv_lat_norm"]), "g_qk": rep128(inp["l0_mla_qk_gain"].reshape(-1)),
        "freqs": rep128(freqs),
        "w_in": np.ascontiguousarray(inp["l0_mla_w_in"]), "w_uq": np.ascontiguousarray(inp["l0_mla_w_uq"]),
        "w_ukv": np.ascontiguousarray(inp["l0_mla_w_ukv"]),
        "g_next": rep128(inp["l1_norm_mix"]),
        "l1_g_qk": rep128(inp["l1_fox_qk_gain"].reshape(-1)),
    }
    m["idxo"] = np.ascontiguousarray(((np.arange(16)[None, :] * 128 + np.arange(128)[:, None]) * 2 + j).astype(np.int32))
    m["idxr"] = np.ascontiguousarray((j * 2048 + np.arange(16)[None, :] * 128 + np.arange(128)[:, None]).astype(np.int32))
    m["l0_w_o"] = np.ascontiguousarray(inp["l0_mla_w_o"])
    m["l0_g_ffn"] = rep128(inp["l0_norm_ffn"])
    m["l0_w_r"] = np.ascontiguousarray(np.concatenate([inp["l0_router_group"], inp["l0_router_expert"]], axis=1))
    m["l0_b_r"] = rep128(np.concatenate([inp["l0_router_group_bias"], inp["l0_router_expert_bias"]]))
    m["l0_w_gu"] = np.ascontiguousarray(inp["l0_w_gate_up"])
    m["l0_w_dn"] = np.ascontiguousarray(inp["l0_w_down"])
    m["l1_w_o"] = np.ascontiguousarray(inp["l1_fox_w_o"])
    m["l1_g_ffn"] = rep128(inp["l1_norm_ffn"])
    m["l1_w_r"] = np.ascontiguousarray(np.concatenate([inp["l1_router_group"], inp["l1_router_expert"]], axis=1))
    m["l1_b_r"] = rep128(np.concatenate([inp["l1_router_group_bias"], inp["l1_router_expert_bias"]]))
    m["l1_w_gu"] = np.ascontiguousarray(inp["l1_w_gate_up"])
    m["l1_w_dn"] = np.ascontiguousarray(inp["l1_w_down"])
    w = inp["l1_fox_w_in"]
    for hh in range(2):
        o = 1024 * hh
        m["w_qk%d" % hh] = np.ascontiguousarray(np.concatenate([w[:, o:o + 1024], w[:, 2048 + o:2048 + o + 1024]], axis=1))
        m["w_vg%d" % hh] = np.ascontiguousarray(np.concatenate(
            [w[:, 4096 + o:4096 + o + 1024], w[:, 6160 + o:6160 + o + 1024], w[:, 6144 + 8 * hh:6144 + 8 * hh + 8]], axis=1))
        m["fbias%d" % hh] = rep128(inp["l1_fox_forget_bias"][8 * hh:8 * hh + 8])
    return m


def kernel(**inputs):
    inp = {k_: np.asarray(v) for k_, v in inputs.items()}
    res = _run(build_F(), [inputs_F(inp, c_) for c_ in range(8)])
    out = np.empty((4, 4096, 2048), np.float32)
    for c_ in range(8):
        b, j = c_ // 2, c_ % 2
        out[b, j * 2048:(j + 1) * 2048] = np.asarray(res[c_]["out"])
    return out
```

```python
import numpy as np
import ml_dtypes
from contextlib import ExitStack
import concourse.bass as bass
import concourse.mybir as mybir
from concourse.bass_utils import run_bass_kernel_spmd

F32 = mybir.dt.float32
BF16 = mybir.dt.bfloat16
I32 = mybir.dt.int32
U32 = mybir.dt.uint32
AF = mybir.ActivationFunctionType
ALU = mybir.AluOpType
AX = mybir.AxisListType
P = 128
EPS = 1e-6


class Sem:
    def __init__(self, h):
        self.h = h
        self.count = 0


class Buf:
    def __init__(self, t, name, persist=True):
        self.t = t
        self.name = name
        self.lw = None
        self.rd = {}
        self.sem = None
        self.persist = persist

    def __getitem__(self, key):
        return self.t[key]


class Op:
    __slots__ = ("eng", "meth", "args", "kw", "deps", "needed", "isdma", "sem", "val", "barrier")


class K:
    def __init__(self, nc):
        self.nc = nc
        self.es = ExitStack()
        self.eng = {"pe": nc.tensor, "act": nc.scalar, "dve": nc.vector, "pool": nc.gpsimd, "sp": nc.sync}
        self.esem = {}
        for e in self.eng:
            self.esem[e] = Sem(self.es.enter_context(nc.semaphore("sem_" + e)))
        self.allsems = list(self.esem.values())
        self.ops = []
        self.emitted = 0
        self.waited = {e: {} for e in self.eng}
        self.last = {}
        self.nbuf = 0
        self.stage = None
        self.last_barrier = 0
        self.free_sems = []
        self.stage_bufs = []
        self.ps = [Buf(self.es.enter_context(nc.psum_tensor("psb%d" % i, [P, 512], F32)), "psb%d" % i) for i in range(8)]

    def begin_stage(self):
        self.stage = ExitStack()

    def end_stage(self):
        self.barrier()
        self.flush()
        self.stage.close()
        self.stage = None
        for b in self.stage_bufs:
            if b.sem is not None:
                self.free_sems.append(b.sem)
                b.sem = None
        self.stage_bufs = []

    def sb(self, name, shape, dt, persist=False):
        st = self.es if persist else self.stage
        self.nbuf += 1
        t = st.enter_context(self.nc.sbuf_tensor("%s_%d" % (name, self.nbuf), list(shape), dt))
        b = Buf(t, name, persist)
        if not persist:
            self.stage_bufs.append(b)
        return b

    def dram(self, name, shape, dt, kind="Internal"):
        t = self.nc.dram_tensor(name, list(shape), dt, kind=kind).ap()
        return Buf(t, name)

    def _sem_of(self, b):
        if b.sem is None and (not b.persist) and self.free_sems:
            b.sem = self.free_sems.pop()
        if b.sem is None:
            b.sem = Sem(self.es.enter_context(self.nc.semaphore("dsem_%d_%s" % (len(self.allsems), b.name))))
            self.allsems.append(b.sem)
        return b.sem

    def _rec(self, eng, meth, args, kw, r, w, isdma=False, sembuf=None):
        op = Op()
        op.eng, op.meth, op.args, op.kw = eng, meth, args, kw
        op.isdma = isdma
        op.needed = False
        op.val = None
        op.barrier = False
        op.sem = self._sem_of(sembuf) if isdma else None
        idx = len(self.ops)
        deps = set()
        for b in r:
            if b.lw is not None:
                deps.add(b.lw)
        for b in w:
            if b.lw is not None:
                deps.add(b.lw)
            deps.update(b.rd.values())
        deps.discard(idx)
        op.deps = []
        for d in deps:
            if d < self.last_barrier:
                continue
            p = self.ops[d]
            if (not p.isdma) and (not isdma) and p.eng == "pe" and eng == "pe":
                continue
            p.needed = True
            op.deps.append(d)
        key = ("dma", id(op.sem)) if isdma else eng
        for b in r:
            b.rd[key] = idx
        for b in w:
            b.lw = idx
            b.rd = {}
        if not isdma:
            self.last[eng] = idx
        self.ops.append(op)
        return op

    def pe(self, meth, *args, r=(), w=(), **kw):
        return self._rec("pe", meth, args, kw, r, w)

    def act(self, meth, *args, r=(), w=(), **kw):
        return self._rec("act", meth, args, kw, r, w)

    def dve(self, meth, *args, r=(), w=(), **kw):
        return self._rec("dve", meth, args, kw, r, w)

    def pool(self, meth, *args, r=(), w=(), **kw):
        return self._rec("pool", meth, args, kw, r, w)

    def dma(self, q, out, in_, r=(), w=(), sem=None, **kw):
        return self._rec(q, "dma_start", (), dict(out=out, in_=in_, **kw), r, w, isdma=True, sembuf=sem)

    def idma(self, out, out_off, in_, in_off, r=(), w=(), sem=None, **kw):
        return self._rec("pool", "indirect_dma_start", (out, out_off, in_, in_off), kw, r, w, isdma=True, sembuf=sem)

    def collective(self, kind, in_ap, out_ap, out_buf, groups):
        return self._rec("pool", "collective_compute", (kind, ALU.bypass, groups, [in_ap], [out_ap]), {}, (), (out_buf,),
                         isdma=True, sembuf=out_buf)

    def barrier(self):
        op = Op()
        op.barrier = True
        op.isdma = False
        op.deps = []
        op.needed = False
        for e, idx in self.last.items():
            self.ops[idx].needed = True
        self.ops.append(op)
        self.last_barrier = len(self.ops)

    def _wait(self, eng, s, tgt):
        if tgt <= 0:
            return
        if self.waited[eng].get(id(s), 0) < tgt:
            self.eng[eng].wait_ge(s.h, tgt)
            self.waited[eng][id(s)] = tgt

    def flush(self):
        ops = self.ops
        while self.emitted < len(ops):
            op = ops[self.emitted]
            self.emitted += 1
            if op.barrier:
                for e in self.eng:
                    for s in self.allsems:
                        self._wait(e, s, s.count)
                continue
            need = {}
            for d in op.deps:
                p = ops[d]
                if p.isdma:
                    s = p.sem
                    tgt = s.count
                else:
                    s = self.esem[p.eng]
                    tgt = p.val
                    assert tgt is not None
                if need.get(id(s), (None, 0))[1] < tgt:
                    need[id(s)] = (s, tgt)
            for s, tgt in need.values():
                self._wait(op.eng, s, tgt)
            ins = getattr(self.eng[op.eng], op.meth)(*op.args, **op.kw)
            if op.isdma:
                op.sem.count += 16
                ins.then_inc(op.sem.h, 16)
            elif op.needed:
                s = self.esem[op.eng]
                s.count += 1
                op.val = s.count
                ins.then_inc(s.h, 1)
            op.args = None
            op.kw = None

    def finish(self):
        self.barrier()
        self.flush()
        self.es.close()


def bcast(ap, axis, n):
    pairs = [list(x) for x in ap.ap]
    pairs.insert(axis, [0, n])
    return bass.AP(ap.tensor, ap.offset, pairs)

class Consts:
    pass


def make_consts(k):
    c = Consts()
    nc = k.nc
    io = k.sb("iota_i", [P, P], I32, persist=True)
    k.pool("iota", io[:, :], [[1, P]], base=0, channel_multiplier=-1, w=[io])
    iof = k.sb("iota_f", [P, P], F32, persist=True)
    k.dve("tensor_copy", iof[:, :], io[:, :], r=[io], w=[iof])
    c.ident_f = k.sb("ident_f", [P, P], F32, persist=True)
    k.dve("tensor_single_scalar", c.ident_f[:, :], iof[:, :], 0.0, ALU.is_equal, r=[iof], w=[c.ident_f])
    c.ident_b = k.sb("ident_b", [P, P], BF16, persist=True)
    k.dve("tensor_copy", c.ident_b[:, :], c.ident_f[:, :], r=[c.ident_f], w=[c.ident_b])
    c.tri_ge_f = k.sb("tri_ge_f", [P, P], F32, persist=True)
    k.dve("tensor_single_scalar", c.tri_ge_f[:, :], iof[:, :], 0.0, ALU.is_ge, r=[iof], w=[c.tri_ge_f])
    c.tri_ge_b = k.sb("tri_ge_b", [P, P], BF16, persist=True)
    k.dve("tensor_copy", c.tri_ge_b[:, :], c.tri_ge_f[:, :], r=[c.tri_ge_f], w=[c.tri_ge_b])
    c.tri_gt_f = k.sb("tri_gt_f", [P, P], F32, persist=True)
    k.dve("tensor_single_scalar", c.tri_gt_f[:, :], iof[:, :], 0.0, ALU.is_gt, r=[iof], w=[c.tri_gt_f])
    c.ones_f = k.sb("ones_f", [P, P], F32, persist=True)
    k.dve("memset", c.ones_f[:, :], 1.0, w=[c.ones_f])
    c.eps = k.sb("eps_t", [P, 1], F32, persist=True)
    k.dve("memset", c.eps[:, :], EPS, w=[c.eps])
    c.one = k.sb("one_t", [P, 1], F32, persist=True)
    k.dve("memset", c.one[:, :], 1.0, w=[c.one])
    c.negpi = k.sb("negpi_t", [P, 1], F32, persist=True)
    k.dve("memset", c.negpi[:, :], -float(np.pi) * (1.0 - 1e-6), w=[c.negpi])
    ci = k.sb("col_i", [P, 64], I32, persist=True)
    k.pool("iota", ci[:, :], [[1, 64]], base=0, channel_multiplier=0, w=[ci])
    c.colidx = k.sb("col_f", [P, 64], F32, persist=True)
    k.dve("tensor_copy", c.colidx[:, :], ci[:, :], r=[ci], w=[c.colidx])
    pi_ = k.sb("part_i", [P, 1], I32, persist=True)
    k.pool("iota", pi_[:, :], [[0, 1]], base=0, channel_multiplier=1, w=[pi_])
    c.partidx = k.sb("part_f", [P, 1], F32, persist=True)
    k.dve("tensor_copy", c.partidx[:, :], pi_[:, :], r=[pi_], w=[c.partidx])
    c.LOG = k.sb("LOG", [P, 16, 72], F32, persist=True)
    c.MSK = k.sb("MSK", [P, 16, 2, 64], F32, persist=True)
    c.GT = k.sb("GT", [P, 16, 2], F32, persist=True)
    c.CUM = k.sb("CUM", [P, 16, 64], F32, persist=True)
    c.DESTI = k.sb("DESTI", [P, 16, 2], I32, persist=True)
    c.IDXW = k.sb("IDXW", [P, 96], I32, persist=True)
    c.IDX16 = k.sb("IDX16", [P, 96, 16], I32, persist=True)
    c.IDX4 = k.sb("IDX4", [P, 96, 4], I32, persist=True)
    return c


def rms(k, c, src, src_ap, G, W, gain_ap, out, out_ap, scr, scr2, stat, post_scale=None):
    sq = scr[:, 0:G * W].rearrange("p (g w) -> p g w", g=G)
    k.dve("tensor_tensor", sq, src_ap, src_ap, ALU.mult, r=[src], w=[scr])
    ssq = stat[:, 0:G]
    k.dve("tensor_reduce", ssq, sq, AX.X, ALU.add, r=[scr], w=[stat])
    std = stat[:, G:2 * G]
    k.act("activation", std, ssq, AF.Sqrt, bias=c.eps[:, 0:1], scale=1.0 / W, r=[stat, c.eps], w=[stat])
    k.dve("reciprocal", ssq, std, r=[stat], w=[stat])
    y = scr2[:, 0:G * W].rearrange("p (g w) -> p g w", g=G)
    k.dve("tensor_tensor", y, src_ap, bcast(ssq, 2, W), ALU.mult, r=[src, stat], w=[scr2])
    k.pool("tensor_tensor", out_ap, y, bcast(gain_ap, 1, G), ALU.mult, r=[scr2], w=[out])


def transposes(k, c, srcs, src_buf, dst_buf, dst_ap_fn, dt, ps_buf, evac="act"):
    n = srcs[0].shape[1]
    rows = srcs[0].shape[0]
    per = (1024 if dt == BF16 else 512) // P
    ident = c.ident_b if dt == BF16 else c.ident_f
    if dt == BF16:
        pst = ps_buf.t[:, :].bitcast(BF16)
    else:
        pst = ps_buf.t[:, :]
    i = 0
    while i < len(srcs):
        cnt = min(per, len(srcs) - i)
        for j in range(cnt):
            k.pe("transpose", pst[0:n, j * P:j * P + rows], srcs[i + j], ident[0:rows, 0:rows], r=[src_buf, ident], w=[ps_buf])
        src_v = pst[0:n, 0:cnt * P].rearrange("p (a b) -> p a b", a=cnt)[:, :, 0:rows]
        if evac == "act":
            k.act("copy", dst_ap_fn(i, cnt), src_v, r=[ps_buf], w=[dst_buf])
        else:
            k.dve("tensor_copy", dst_ap_fn(i, cnt), src_v, r=[ps_buf], w=[dst_buf])
        i += cnt


def rms_multi(k, c, specs):
    sqs, ssqs, stds, ys = [], [], [], []
    for (src, src_ap, G, W, gain_ap, out, out_ap, scr, scr2, stat) in specs:
        sq = scr[:, 0:G * W].rearrange("p (g w) -> p g w", g=G)
        k.dve("tensor_tensor", sq, src_ap, src_ap, ALU.mult, r=[src], w=[scr])
        sqs.append(sq)
    for i, (src, src_ap, G, W, gain_ap, out, out_ap, scr, scr2, stat) in enumerate(specs):
        ssq = stat[:, 0:G]
        k.dve("tensor_reduce", ssq, sqs[i], AX.X, ALU.add, r=[scr], w=[stat])
        ssqs.append(ssq)
    for i, (src, src_ap, G, W, gain_ap, out, out_ap, scr, scr2, stat) in enumerate(specs):
        std = stat[:, G:2 * G]
        k.act("activation", std, ssqs[i], AF.Sqrt, bias=c.eps[:, 0:1], scale=1.0 / W, r=[stat, c.eps], w=[stat])
        stds.append(std)
    for i, (src, src_ap, G, W, gain_ap, out, out_ap, scr, scr2, stat) in enumerate(specs):
        k.dve("reciprocal", ssqs[i], stds[i], r=[stat], w=[stat])
    for i, (src, src_ap, G, W, gain_ap, out, out_ap, scr, scr2, stat) in enumerate(specs):
        y = scr2[:, 0:G * W].rearrange("p (g w) -> p g w", g=G)
        k.dve("tensor_tensor", y, src_ap, bcast(ssqs[i], 2, W), ALU.mult, r=[src, stat], w=[scr2])
        ys.append(y)
    for i, (src, src_ap, G, W, gain_ap, out, out_ap, scr, scr2, stat) in enumerate(specs):
        k.pool("tensor_tensor", out_ap, ys[i], bcast(gain_ap, 1, G), ALU.mult, r=[scr2], w=[out])

NQT = 8
NKT = 32
SEQ = 4096


def attention(k, c, load_head, kshared, otx, gate_d=None):
    NH = 8
    pt = [k.sb("pt%d" % i, [P, 512], BF16) for i in range(4)]
    osb = [k.sb("osb%d" % i, [P, P], BF16) for i in range(2)]
    rec = [k.sb("rec%d" % i, [P, 1], F32) for i in range(2)]
    ost = [k.sb("ost%d" % i, [P, 512], BF16) for i in range(2)]
    sg = [k.sb("sg%d" % i, [P, 4, P], BF16) for i in range(2)] if gate_d is not None else None
    S = [k.ps[0], k.ps[1], k.ps[7]]
    oacc = [k.ps[2], k.ps[3], k.ps[4], k.ps[5]]
    pst = k.ps[6]
    st = dict(pcount=0, fin=0)

    def emit_qk(pairs, Q, kt):
        j = kt - 4 * Q
        q0 = max(j, 0) * P
        Sb = S[kt % 3]
        npair = len(pairs)
        for pi, (kb, kfn, qb, qfn) in enumerate(pairs):
            k.pe("matmul", Sb[:, q0:512], kfn(kt), qfn(Q * 512 + q0, (Q + 1) * 512),
                 start=(pi == 0), stop=(pi == npair - 1), r=[kb, qb], w=[Sb])

    def emit_rest(vbuf, Q, kt):
        j = kt - 4 * Q
        q0 = max(j, 0) * P
        Sb = S[kt % 3]
        Pb = pt[st["pcount"] % 4]
        st["pcount"] += 1
        k.act("activation", Pb[:, q0:512], Sb[:, q0:512], AF.Exp, r=[Sb], w=[Pb])
        if j >= 0:
            k.dve("tensor_tensor", Pb[:, j * P:(j + 1) * P], Pb[:, j * P:(j + 1) * P], c.tri_ge_b[:, :], ALU.mult,
                  r=[Pb, c.tri_ge_b], w=[Pb])
        for qs in range(max(j, 0), 4):
            ob = oacc[qs]
            k.pe("matmul", ob[:, 0:129], Pb[:, qs * P:(qs + 1) * P], vbuf[:, kt, 0:129],
                 start=(kt == 0), stop=(kt == 4 * Q + qs), r=[Pb, vbuf], w=[ob])

    def emit_fin(h, Q, sgt):
        stg = ost[Q % 2]
        for qs in range(4):
            ob = oacc[qs]
            rc = rec[st["fin"] % 2]
            o_ = osb[st["fin"] % 2]
            st["fin"] += 1
            k.dve("reciprocal", rc[:, :], ob[:, 128:129], r=[ob], w=[rc])
            k.dve("tensor_scalar", o_[:, :], ob[:, 0:128], rc[:, 0:1], None, ALU.mult, r=[ob, rc], w=[o_])
            if sgt is not None:
                k.dve("tensor_tensor", o_[:, :], o_[:, :], sgt[:, qs, :], ALU.mult, r=[o_, sgt], w=[o_])
            pv = pst.t[:, :].bitcast(BF16)
            k.pe("transpose", pv[:, 0:P], o_[:, :], c.ident_b[:, :], r=[o_, c.ident_b], w=[pst])
            k.act("copy", stg[:, qs * P:(qs + 1) * P], pv[:, 0:P], r=[pst], w=[stg])
        k.dma("sp", otx[Q // 4, h, :, (Q % 4) * 512:(Q % 4 + 1) * 512], stg[:, :], r=[stg], sem=stg)

    hb = [load_head(0, 0)]
    pending = None
    for h in range(NH):
        if h + 1 < NH:
            hb.append(load_head(h + 1, (h + 1) % 2))
        pairs, vbuf = hb[h]
        for Q in range(NQT):
            sgt = None
            if gate_d is not None:
                sgt = sg[Q % 2]
                k.dma("sp", sgt[:, :, :], gate_d[Q * 512:(Q + 1) * 512, h * P:(h + 1) * P].rearrange("(s p) d -> p s d", p=P),
                      r=[gate_d], w=[sgt], sem=sgt)
            nk = 4 * (Q + 1)
            emit_qk(pairs, Q, 0)
            emit_qk(pairs, Q, 1)
            if pending is not None:
                emit_fin(*pending)
                pending = None
            for kt in range(nk):
                if kt + 2 < nk:
                    emit_qk(pairs, Q, kt + 2)
                emit_rest(vbuf, Q, kt)
            pending = (h, Q, sgt)
    emit_fin(*pending)


def stage_A(k, c, d):
    SCALE = 1.0 / float(np.sqrt(192.0))
    NT = SEQ // P
    k.begin_stage()
    g_mix = k.sb("g_mix", [P, 2048], F32)
    g_ql = k.sb("g_ql", [P, 512], F32)
    g_kvl = k.sb("g_kvl", [P, 512], F32)
    g_qk = k.sb("g_qk", [P, 384], F32)
    g_q = k.sb("g_q", [P, 192], F32)
    freqs = k.sb("freqs", [P, 32], F32)
    for t_, s_ in ((g_mix, d["g_mix"]), (g_ql, d["g_ql"]), (g_kvl, d["g_kvl"]), (g_qk, d["g_qk"]), (freqs, d["freqs"])):
        k.dma("sp", t_[:, :], s_[:, :], r=[s_], w=[t_], sem=t_)
    k.dve("tensor_scalar", g_q[:, :], g_qk[:, 0:192], SCALE, None, ALU.mult, r=[g_qk], w=[g_q])
    w_in = k.sb("w_in", [P, 16, 1088], BF16)
    k.dma("pool", w_in[:, :, :], d["w_in"][:, :].rearrange("(kc p) n -> p kc n", p=P), r=[d["w_in"]], w=[w_in], sem=w_in)
    w_uq = k.sb("w_uq", [P, 4, 1536], BF16)
    k.dma("pool", w_uq[:, :, :], d["w_uq"][:, :].rearrange("(kc p) n -> p kc n", p=P), r=[d["w_uq"]], w=[w_uq], sem=w_uq)
    w_ukv = k.sb("w_ukv", [P, 4, 2048], BF16)
    k.dma("pool", w_ukv[:, :, :], d["w_ukv"][:, :].rearrange("(kc p) n -> p kc n", p=P), r=[d["w_ukv"]], w=[w_ukv], sem=w_ukv)

    xt = [k.sb("xt%d" % i, [P, 2048], F32) for i in range(2)]
    posi = [k.sb("posi%d" % i, [P, 1], I32) for i in range(2)]
    posf = k.sb("posf", [P, 1], F32)
    scr = k.sb("scr", [P, 2048], F32)
    scr2 = k.sb("scr2", [P, 2048], F32)
    stat = k.sb("stat", [P, 32], F32)
    sA = [k.sb("sA%d" % i, [P, 1024], F32) for i in range(2)]
    sB = [k.sb("sB%d" % i, [P, 512], F32) for i in range(2)]
    sC = [scr, scr2]
    stA = k.sb("stA", [P, 32], F32)
    stB = k.sb("stB", [P, 32], F32)
    stC = k.sb("stC", [P, 32], F32)
    hn = k.sb("hn", [P, 2048], BF16)
    hnT = k.sb("hnT", [P, 16, P], BF16)
    z = k.sb("z", [P, 1088], F32)
    cqn = k.sb("cqn", [P, 1024], BF16)
    latT = k.sb("latT", [P, 8, P], BF16)
    kr = k.sb("kr", [P, 64], F32)
    krb = k.sb("krb", [P, 64], BF16)
    krT = k.sb("krT", [64, P], BF16)
    qsb = k.sb("qsb", [P, 8, 192], F32)
    kvsb = k.sb("kvsb", [P, 8, 256], F32)
    qn = k.sb("qn", [P, 8, P], BF16)
    qr = k.sb("qr", [P, 8, 64], F32)
    qrb = k.sb("qrb", [P, 8, 64], BF16)
    kn = k.sb("kn", [P, 8, P], BF16)
    vb = k.sb("vb", [P, 8, P], BF16)
    qT = k.sb("qT", [P, 8, P], BF16)
    qrT = k.sb("qrT", [64, 8, P], BF16)
    kT = k.sb("kT", [P, 8, P], BF16)
    ang = k.sb("ang", [P, 32], F32)
    fr = k.sb("fr", [P, 2, 32], F32)
    fri = k.sb("fri", [P, 2, 32], I32)
    frf = k.sb("frf", [P, 2, 32], F32)
    cs = k.sb("cs", [P, 2, 32], F32)
    rt = k.sb("rt", [P, 8, 4, 32], F32)
    pstr = k.ps[7]

    def rope(src, src_ap_fn, G, dst, dst_ap_fn):
        x1 = src_ap_fn(0)
        x2 = src_ap_fn(1)
        sinb = bcast(cs[:, 0, :], 1, G)
        cosb = bcast(cs[:, 1, :], 1, G)
        t = [rt[:, 0:G, i, :] for i in range(4)]
        k.dve("tensor_tensor", t[0], x1, cosb, ALU.mult, r=[src, cs], w=[rt])
        k.dve("tensor_tensor", t[1], x2, sinb, ALU.mult, r=[src, cs], w=[rt])
        k.dve("tensor_tensor", t[2], x2, cosb, ALU.mult, r=[src, cs], w=[rt])
        k.dve("tensor_tensor", t[3], x1, sinb, ALU.mult, r=[src, cs], w=[rt])
        k.dve("tensor_tensor", dst_ap_fn(0), t[0], t[1], ALU.subtract, r=[rt], w=[dst])
        k.dve("tensor_tensor", dst_ap_fn(1), t[2], t[3], ALU.add, r=[rt], w=[dst])

    def ld_a1(i):
        k.dma("sp", xt[i % 2][:, :], d["x"][i * P:(i + 1) * P, :], r=[d["x"]], w=[xt[i % 2]], sem=xt[i % 2])
        k.dma("sp", posi[i % 2][:, :], d["pos"][i * P:(i + 1) * P, :], r=[d["pos"]], w=[posi[i % 2]], sem=posi[i % 2])

    ld_a1(0)
    for i in range(NT):
        x_ = xt[i % 2]
        p_ = posi[i % 2]
        if i + 1 < NT:
            ld_a1(i + 1)
        k.dve("tensor_copy", posf[:, :], p_[:, :], r=[p_], w=[posf])
        k.dve("tensor_scalar", ang[:, :], freqs[:, :], posf[:, 0:1], 1.0 / (2 * np.pi), ALU.mult, ALU.mult, r=[freqs, posf], w=[ang])
        k.dve("tensor_scalar", fr[:, 0, :], ang[:, :], 0.5, None, ALU.add, r=[ang], w=[fr])
        k.dve("tensor_scalar", fr[:, 1, :], ang[:, :], 0.75, None, ALU.add, r=[ang], w=[fr])
        k.dve("tensor_copy", fri[:, :, :], fr[:, :, :], r=[fr], w=[fri])
        k.dve("tensor_copy", frf[:, :, :], fri[:, :, :], r=[fri], w=[frf])
        k.dve("tensor_tensor", fr[:, :, :], fr[:, :, :], frf[:, :, :], ALU.subtract, r=[fr, frf], w=[fr])
        k.dve("tensor_single_scalar", frf[:, :, :], fr[:, :, :], 0.0, ALU.is_lt, r=[fr], w=[frf])
        k.dve("tensor_tensor", fr[:, :, :], fr[:, :, :], frf[:, :, :], ALU.add, r=[fr, frf], w=[fr])
        k.act("activation", cs[:, :, :], fr[:, :, :], AF.Sin, bias=c.negpi[:, 0:1], scale=2 * float(np.pi) * (1.0 - 1e-6),
              r=[fr, c.negpi], w=[cs])
        rms(k, c, x_, x_[:, :].rearrange("p (g w) -> p g w", g=1), 1, 2048, g_mix[:, :], hn,
            hn[:, :].rearrange("p (g w) -> p g w", g=1), scr, scr2, stat)
        transposes(k, c, [hn[:, kc * P:(kc + 1) * P] for kc in range(16)], hn, hnT,
                   lambda i0, cnt: hnT[:, i0:i0 + cnt, :], BF16, pstr)
        for n, (n0, n1) in enumerate(((0, 512), (512, 1024), (1024, 1088))):
            pz = k.ps[n]
            for kc in range(16):
                k.pe("matmul", pz[:, 0:n1 - n0], hnT[:, kc, :], w_in[:, kc, n0:n1], start=(kc == 0), stop=(kc == 15),
                     r=[hnT, w_in], w=[pz])
            k.act("copy", z[:, n0:n1], pz[:, 0:n1 - n0], r=[pz], w=[z])
        g1_ = lambda a_: a_.rearrange("p (g w) -> p g w", g=1)
        rms_multi(k, c, [
            (z, g1_(z[:, 0:512]), 1, 512, g_ql[:, :], cqn, g1_(cqn[:, 0:512]), sA[0], sA[1], stA),
            (z, g1_(z[:, 512:1024]), 1, 512, g_kvl[:, :], cqn, g1_(cqn[:, 512:1024]), sC[0], sC[1], stC),
            (z, g1_(z[:, 1024:1088]), 1, 64, g_qk[:, 320:384], kr, g1_(kr[:, :]), sB[0], sB[1], stB),
        ])
        rope(kr, lambda hf: kr[:, hf * 32:(hf + 1) * 32].rearrange("p (g w) -> p g w", g=1), 1,
             krb, lambda hf: krb[:, hf * 32:(hf + 1) * 32].rearrange("p (g w) -> p g w", g=1))
        transposes(k, c, [cqn[:, j * P:(j + 1) * P] for j in range(8)], cqn, latT,
                   lambda i0, cnt: latT[:, i0:i0 + cnt, :], BF16, pstr)
        transposes(k, c, [krb[:, :]], krb, krT, lambda i0, cnt: krT[:, :].rearrange("p (a b) -> p a b", a=1), BF16, pstr)
        k.dma("sp", d["KRT"][:, i * P:(i + 1) * P], krT[:, :], r=[krT], sem=krT)
        qflat = qsb[:, :, :].rearrange("p h d -> p (h d)")
        for n in range(3):
            pz = k.ps[3 + n]
            for kc in range(4):
                k.pe("matmul", pz[:, :], latT[:, kc, :], w_uq[:, kc, n * 512:(n + 1) * 512], start=(kc == 0), stop=(kc == 3),
                     r=[latT, w_uq], w=[pz])
            k.act("copy", qflat[:, n * 512:(n + 1) * 512], pz[:, :], r=[pz], w=[qsb])
        kvflat = kvsb[:, :, :].rearrange("p h d -> p (h d)")
        for n in range(4):
            pz = k.ps[(0, 1, 2, 6)[n]]
            for kc in range(4):
                k.pe("matmul", pz[:, :], latT[:, 4 + kc, :], w_ukv[:, kc, n * 512:(n + 1) * 512], start=(kc == 0), stop=(kc == 3),
                     r=[latT, w_ukv], w=[pz])
            k.act("copy", kvflat[:, n * 512:(n + 1) * 512], pz[:, :], r=[pz], w=[kvsb])
        rms_multi(k, c, [
            (qsb, qsb[:, :, 128:192], 8, 64, g_q[:, 128:192], qr, qr[:, :, :], sB[0], sB[1], stB),
            (qsb, qsb[:, :, 0:128], 8, 128, g_q[:, 0:128], qn, qn[:, :, :], sA[0], sA[1], stA),
            (kvsb, kvsb[:, :, 0:128], 8, 128, g_qk[:, 192:320], kn, kn[:, :, :], sC[0], sC[1], stC),
        ])
        rope(qr, lambda hf: qr[:, :, hf * 32:(hf + 1) * 32], 8, qrb, lambda hf: qrb[:, :, hf * 32:(hf + 1) * 32])
        k.pool("tensor_copy", vb[:, :, :], kvsb[:, :, 128:256], r=[kvsb], w=[vb])
        transposes(k, c, [qn[:, h, :] for h in range(8)], qn, qT, lambda i0, cnt: qT[:, i0:i0 + cnt, :], BF16, pstr)
        transposes(k, c, [qrb[:, h, :] for h in range(8)], qrb, qrT, lambda i0, cnt: qrT[:, i0:i0 + cnt, :], BF16, pstr)
        transposes(k, c, [kn[:, h, :] for h in range(8)], kn, kT, lambda i0, cnt: kT[:, i0:i0 + cnt, :], BF16, pstr)
        tok = slice(i * P, (i + 1) * P)
        k.dma("sp", d["QT"][:, :, tok].rearrange("h d t -> d h t"), qT[:, :, :], r=[qT], sem=qT)
        k.dma("sp", d["QRT"][:, :, tok].rearrange("h d t -> d h t"), qrT[:, :, :], r=[qrT], sem=qrT)
        k.dma("sp", d["KT"][:, :, tok].rearrange("h d t -> d h t"), kT[:, :, :], r=[kT], sem=kT)
        k.dma("sp", d["V"][:, tok, :].rearrange("h t d -> t h d"), vb[:, :, :], r=[vb], sem=vb)
    k.end_stage()

    k.begin_stage()
    krt_sb = k.sb("krt_sb", [64, SEQ], BF16)
    k.dma("sp", krt_sb[:, :], d["KRT"][:, :], r=[d["KRT"]], w=[krt_sb], sem=krt_sb)
    sets = []
    for s in range(2):
        st = dict(q=k.sb("aq%d" % s, [P, SEQ], BF16), qr=k.sb("aqr%d" % s, [64, SEQ], BF16),
                  k=k.sb("ak%d" % s, [P, SEQ], BF16), v=k.sb("av%d" % s, [P, NKT, 132], BF16))
        k.dve("memset", st["v"][:, :, 128:129], 1.0, w=[st["v"]])
        sets.append(st)

    def load_head(h, s):
        st = sets[s]
        k.dma("sp", st["q"][:, :], d["QT"][h, :, :], r=[d["QT"]], w=[st["q"]], sem=st["q"])
        k.dma("sp", st["qr"][:, :], d["QRT"][h, :, :], r=[d["QRT"]], w=[st["qr"]], sem=st["qr"])
        k.dma("sp", st["k"][:, :], d["KT"][h, :, :], r=[d["KT"]], w=[st["k"]], sem=st["k"])
        k.dma("sp", st["v"][:, :, 0:128], d["V"][h, :, :].rearrange("(n p) d -> p n d", p=P), r=[d["V"]], w=[st["v"]], sem=st["v"])
        pairs = [
            (st["k"], (lambda kt, b=st["k"]: b[:, kt * P:(kt + 1) * P]), st["q"], (lambda a, e, b=st["q"]: b[:, a:e])),
            (krt_sb, (lambda kt: krt_sb[:, kt * P:(kt + 1) * P]), st["qr"], (lambda a, e, b=st["qr"]: b[:, a:e])),
        ]
        return pairs, st["v"]

    attention(k, c, load_head, None, d["OTX"])
    k.end_stage()

SKIP_UNUSED = True
NBLK = 96
NTOK_T = 16


def stage_WO_MOE(k, c, d, last, upto=4):
    IOA = bass.IndirectOffsetOnAxis
    LOG, MSK, GT, CUM, DESTI, IDXW = c.LOG, c.MSK, c.GT, c.CUM, c.DESTI, c.IDXW
    k.begin_stage()
    w_o = k.sb("w_o", [P, 16, 2048], BF16)
    k.dma("pool", w_o[:, :, :], d["w_o"][:, :].rearrange("(h p) n -> p h n", p=P), r=[d["w_o"]], w=[w_o], sem=w_o)
    g_ffn = k.sb("g_ffn", [P, 2048], F32)
    k.dma("sp", g_ffn[:, :], d["g_ffn"][:, :], r=[d["g_ffn"]], w=[g_ffn], sem=g_ffn)
    w_r = k.sb("w_r", [P, 16, 72], F32)
    k.dma("sp", w_r[:, :, :], d["w_r"][:, :].rearrange("(kc p) n -> p kc n", p=P), r=[d["w_r"]], w=[w_r], sem=w_r)
    b_r = k.sb("b_r", [P, 72], F32)
    k.dma("sp", b_r[:, :], d["b_r"][:, :], r=[d["b_r"]], w=[b_r], sem=b_r)
    ot = [k.sb("ot%d" % i, [P, 16, P], BF16) for i in range(2)]
    xr = [k.sb("xr%d" % i, [P, 2048], F32) for i in range(2)]
    h1 = k.sb("h1", [P, 2048], F32)
    scr = k.sb("scr", [P, 2048], F32)
    scr2 = k.sb("scr2", [P, 2048], F32)
    stat = k.sb("stat", [P, 32], F32)
    hn2 = k.sb("hn2", [P, 2048], BF16)
    hn2T = k.sb("hn2T", [P, 16, P], F32)
    def ld_b1(i):
        tk = slice(i * P, (i + 1) * P)
        k.dma("sp", ot[i % 2][:, :, :], d["OTin"][:, :, tk].rearrange("h d t -> d h t"), r=[d["OTin"]], w=[ot[i % 2]], sem=ot[i % 2])
        k.dma("sp", xr[i % 2][:, :], d["xres"][tk, :], r=[d["xres"]], w=[xr[i % 2]], sem=xr[i % 2])

    ld_b1(0)
    for i in range(NTOK_T):
        tok = slice(i * P, (i + 1) * P)
        o_ = ot[i % 2]
        x_ = xr[i % 2]
        if i + 1 < NTOK_T:
            ld_b1(i + 1)
        for n in range(4):
            pz = k.ps[n]
            for h in range(16):
                k.pe("matmul", pz[:, :], o_[:, h, :], w_o[:, h, n * 512:(n + 1) * 512], start=(h == 0), stop=(h == 15),
                     r=[o_, w_o], w=[pz])
            k.dve("tensor_tensor", h1[:, n * 512:(n + 1) * 512], x_[:, n * 512:(n + 1) * 512], pz[:, :], ALU.add,
                  r=[x_, pz], w=[h1])
        k.dma("sp", d["H1s"][tok, :], h1[:, :], r=[h1], sem=h1)
        g1_ = lambda a: a.rearrange("p (g w) -> p g w", g=1)
        rms(k, c, h1, g1_(h1[:, :]), 1, 2048, g_ffn[:, :], scr, g1_(scr[:, :]), scr, scr2, stat)
        k.pool("tensor_copy", hn2[:, :], scr[:, :], r=[scr], w=[hn2])
        k.dma("sp", d["HN2"][tok, :], hn2[:, :], r=[hn2], sem=hn2)
        transposes(k, c, [scr[:, kc * P:(kc + 1) * P] for kc in range(16)], scr, hn2T,
                   lambda i0, cnt: hn2T[:, i0:i0 + cnt, :], F32, k.ps[7])
        pl = k.ps[5]
        for kc in range(16):
            k.pe("matmul", pl[:, 0:72], hn2T[:, kc, :], w_r[:, kc, :], start=(kc == 0), stop=(kc == 15), r=[hn2T, w_r], w=[pl])
        k.dve("tensor_tensor", LOG[:, i, :], pl[:, 0:72], b_r[:, :], ALU.add, r=[pl, b_r], w=[LOG])
    k.end_stage()

    k.begin_stage()
    sm = k.sb("sm", [P, 16], F32)
    ohg = k.sb("ohg", [P, 8], F32)
    eg = k.sb("eg", [P, 8], F32)
    sel = k.sb("sel", [P, 8, 8], F32)
    ein = k.sb("ein", [P, 8], F32)
    oh1 = k.sb("oh1", [P, 8], F32)
    oh2 = k.sb("oh2", [P, 8], F32)
    e2 = k.sb("e2", [P, 8], F32)
    A = k.sb("A", [P, 64], F32)
    carry = k.sb("carry", [P, 64], F32)
    k.dve("memset", carry[:, :], 0.0, w=[carry])
    pc = k.ps[0]
    pc2 = k.ps[1]
    for i in range(NTOK_T):
        gl = LOG[:, i, 0:8]
        el = LOG[:, i, 8:72].rearrange("p (g e) -> p g e", g=8)
        k.dve("tensor_reduce", sm[:, 0:1], gl, AX.X, ALU.max, r=[LOG], w=[sm])
        k.dve("tensor_scalar", sm[:, 1:2], sm[:, 0:1], -1.0, None, ALU.mult, r=[sm], w=[sm])
        k.dve("tensor_scalar", ohg[:, :], gl, sm[:, 0:1], None, ALU.is_equal, r=[LOG, sm], w=[ohg])
        k.act("activation", eg[:, :], gl, AF.Exp, bias=sm[:, 1:2], r=[LOG, sm], w=[eg])
        k.dve("tensor_reduce", sm[:, 2:3], eg[:, :], AX.X, ALU.add, r=[eg], w=[sm])
        k.dve("reciprocal", sm[:, 3:4], sm[:, 2:3], r=[sm], w=[sm])
        k.dve("tensor_tensor", sel[:, :, :], el, bcast(ohg[:, :], 2, 8), ALU.mult, r=[LOG, ohg], w=[sel])
        k.dve("tensor_reduce", ein[:, :], sel[:, :, :].rearrange("p g e -> p e g"), AX.X, ALU.add, r=[sel], w=[ein])
        k.dve("tensor_reduce", sm[:, 4:5], ein[:, :], AX.X, ALU.max, r=[ein], w=[sm])
        k.dve("tensor_scalar", sm[:, 5:6], sm[:, 4:5], -1.0, None, ALU.mult, r=[sm], w=[sm])
        k.dve("tensor_scalar", oh1[:, :], ein[:, :], sm[:, 4:5], None, ALU.is_equal, r=[ein, sm], w=[oh1])
        k.dve("scalar_tensor_tensor", e2[:, :], oh1[:, :], -1e30, ein[:, :], ALU.mult, ALU.add, r=[oh1, ein], w=[e2])
        k.dve("tensor_reduce", sm[:, 6:7], e2[:, :], AX.X, ALU.max, r=[e2], w=[sm])
        k.dve("tensor_scalar", oh2[:, :], e2[:, :], sm[:, 6:7], None, ALU.is_equal, r=[e2, sm], w=[oh2])
        k.act("activation", sm[:, 7:8], sm[:, 6:7], AF.Exp, bias=sm[:, 5:6], r=[sm], w=[sm])
        k.dve("tensor_scalar", sm[:, 8:9], sm[:, 7:8], 1.0, None, ALU.add, r=[sm], w=[sm])
        k.dve("reciprocal", sm[:, 9:10], sm[:, 8:9], r=[sm], w=[sm])
        k.dve("tensor_tensor", GT[:, i, 0:1], sm[:, 3:4], sm[:, 9:10], ALU.mult, r=[sm], w=[GT])
        k.dve("tensor_tensor", GT[:, i, 1:2], GT[:, i, 0:1], sm[:, 7:8], ALU.mult, r=[sm, GT], w=[GT])
        m1 = MSK[:, i, 0, :].rearrange("p (g e) -> p g e", g=8)
        m2 = MSK[:, i, 1, :].rearrange("p (g e) -> p g e", g=8)
        k.dve("tensor_tensor", m1, bcast(ohg[:, :], 2, 8), bcast(oh1[:, :], 1, 8), ALU.mult, r=[ohg, oh1], w=[MSK])
        k.dve("tensor_tensor", m2, bcast(ohg[:, :], 2, 8), bcast(oh2[:, :], 1, 8), ALU.mult, r=[ohg, oh2], w=[MSK])
        k.dve("tensor_tensor", A[:, :], MSK[:, i, 0, :], MSK[:, i, 1, :], ALU.add, r=[MSK], w=[A])
        k.pe("matmul", pc[:, 0:64], c.tri_gt_f[:, :], A[:, :], start=True, stop=True, r=[c.tri_gt_f, A], w=[pc])
        k.dve("tensor_tensor", CUM[:, i, :], pc[:, 0:64], carry[:, :], ALU.add, r=[pc, carry], w=[CUM])
        k.pe("matmul", pc2[:, 0:64], c.ones_f[:, :], A[:, :], start=True, stop=True, r=[c.ones_f, A], w=[pc2])
        k.dve("tensor_tensor", carry[:, :], carry[:, :], pc2[:, 0:64], ALU.add, r=[carry, pc2], w=[carry])
    t1 = k.sb("t1", [P, 64], F32)
    ti = k.sb("ti", [P, 64], I32)
    tf = k.sb("tf", [P, 64], F32)
    pend = k.sb("pend", [P, 64], F32)
    pstart = k.sb("pstart", [P, 64], F32)
    k.dve("tensor_scalar", t1[:, :], carry[:, :], 127.0, 1.0 / 128.0, ALU.add, ALU.mult, r=[carry], w=[t1])
    k.dve("tensor_copy", ti[:, :], t1[:, :], r=[t1], w=[ti])
    k.dve("tensor_copy", tf[:, :], ti[:, :], r=[ti], w=[tf])
    k.dve("tensor_tensor", pend[:, :], tf[:, :], t1[:, :], ALU.is_gt, r=[tf, t1], w=[pend])
    k.dve("tensor_tensor", tf[:, :], tf[:, :], pend[:, :], ALU.subtract, r=[tf, pend], w=[tf])
    k.dve("tensor_scalar", tf[:, :], tf[:, :], 128.0, None, ALU.mult, r=[tf], w=[tf])
    k.dve("tensor_tensor_scan", pend[:, :], c.ones_f[:, 0:64], tf[:, :], 0.0, ALU.mult, ALU.add, r=[c.ones_f, tf], w=[pend])
    k.dve("tensor_tensor", pstart[:, :], pend[:, :], tf[:, :], ALU.subtract, r=[pend, tf], w=[pstart])
    bvi = k.sb("bvi", [P, NBLK], I32)
    k.pool("iota", bvi[:, :], [[128, NBLK]], base=0, channel_multiplier=0, w=[bvi])
    bv = k.sb("bv", [P, NBLK], F32)
    k.dve("tensor_copy", bv[:, :], bvi[:, :], r=[bvi], w=[bv])
    cmp_ = k.sb("cmp", [P, NBLK, 64], F32)
    k.dve("tensor_tensor", cmp_[:, :, :], bcast(pend[:, :], 1, NBLK), bcast(bv[:, :], 2, 64), ALU.is_le, r=[pend, bv], w=[cmp_])
    be = k.sb("be", [P, NBLK], F32)
    k.dve("tensor_reduce", be[:, :], cmp_[:, :, :], AX.X, ALU.add, r=[cmp_], w=[be])
    k.dve("tensor_scalar", be[:, :], be[:, :], 63.0, 128.0, ALU.min, ALU.mult, r=[be], w=[be])
    k.dve("tensor_scalar", be[:, :], be[:, :], c.partidx[:, 0:1], None, ALU.add, r=[be, c.partidx], w=[be])
    if SKIP_UNUSED:
        usd = k.sb("usd", [P, NBLK], F32)
        k.dve("tensor_scalar", usd[:, :], bv[:, :], pend[:, 63:64], None, ALU.is_lt, r=[bv, pend], w=[usd])
        k.dve("tensor_scalar", usd[:, :], usd[:, :], -4194304.0, 4194304.0, ALU.mult, ALU.add, r=[usd], w=[usd])
        k.dve("tensor_tensor", be[:, :], be[:, :], usd[:, :], ALU.add, r=[be, usd], w=[be])
    k.dve("tensor_copy", IDXW[:, :], be[:, :], r=[be], w=[IDXW])
    i16f = k.sb("i16f", [P, NBLK, 16], F32)
    k.dve("scalar_tensor_tensor", i16f[:, :, 0:8], bcast(be[:, :], 2, 8), 8.0, bcast(c.colidx[:, 0:8], 1, NBLK), ALU.mult, ALU.add,
          r=[be, c.colidx], w=[i16f])
    k.dve("tensor_copy", c.IDX16[:, :, 0:8], i16f[:, :, 0:8], r=[i16f], w=[c.IDX16])
    k.dve("scalar_tensor_tensor", i16f[:, :, 0:4], bcast(be[:, :], 2, 4), 4.0, bcast(c.colidx[:, 0:4], 1, NBLK), ALU.mult, ALU.add,
          r=[be, c.colidx], w=[i16f])
    k.dve("tensor_copy", c.IDX4[:, :, :], i16f[:, :, 0:4], r=[i16f], w=[c.IDX4])
    dsel = k.sb("dsel", [P, 64], F32)
    pc_ = k.sb("pc_", [P, 64], F32)
    destf = k.sb("destf", [P, 2], F32)
    hnt = [k.sb("hnt%d" % i, [P, 2048], BF16) for i in range(2)]
    for i in range(NTOK_T):
        tok = slice(i * P, (i + 1) * P)
        k.dve("tensor_tensor", pc_[:, :], pstart[:, :], CUM[:, i, :], ALU.add, r=[pstart, CUM], w=[pc_])
        for s in range(2):
            k.dve("tensor_tensor", dsel[:, :], pc_[:, :], MSK[:, i, s, :], ALU.mult, r=[pc_, MSK], w=[dsel])
            k.dve("tensor_reduce", destf[:, s:s + 1], dsel[:, :], AX.X, ALU.add, r=[dsel], w=[destf])
        k.dve("tensor_copy", DESTI[:, i, :], destf[:, :], r=[destf], w=[DESTI])
        ht = hnt[i % 2]
        k.dma("sp", ht[:, :], d["HN2"][tok, :], r=[d["HN2"]], w=[ht], sem=ht)
        for s in range(2):
            k.idma(d["XBUF"][:, :], IOA(ap=DESTI[:, i, s:s + 1], axis=0), ht[:, :], None,
                   r=[ht, DESTI], sem=ht)
    if "DBG" in d:
        k.dma("sp", d["DBG"][:, 0:32], DESTI[:, :, :].rearrange("p a b -> p (a b)"), r=[DESTI], sem=DESTI)
        k.dma("sp", d["DBG"][:, 32:128], IDXW[:, :], r=[IDXW], sem=IDXW)
        k.dma("sp", d["DBGF"][:, 0:32], GT[:, :, :].rearrange("p a b -> p (a b)"), r=[GT], sem=GT)
        k.dma("sp", d["DBGF"][:, 32:32 + 1152], LOG[:, :, :].rearrange("p a b -> p (a b)"), r=[LOG], sem=LOG)
        k.dma("sp", d["DBGF"][:, 1184:1184 + 64], pend[:, :], r=[pend], sem=pend)
        k.dma("sp", d["DBGF"][:, 1248:1248 + 64], carry[:, :], r=[carry], sem=carry)
    k.end_stage()
    if upto <= 2:
        return

    k.begin_stage()
    wgu = [[k.sb("wgu%d_%d" % (i, cc_), [P, 2, 1024], BF16) for cc_ in range(8)] for i in range(2)]
    wdn = [[k.sb("wdn%d_%d" % (i, cc_), [P, 2048], BF16) for cc_ in range(4)] for i in range(2)]
    xb = [k.sb("xb%d" % i, [P, 2048], BF16) for i in range(2)]
    xbT = k.sb("xbT", [P, 16, P], BF16)
    sgl = k.sb("sgl", [P, 512], F32)
    actb = k.sb("actb", [P, 512], BF16)
    actT = k.sb("actT", [P, 4, P], BF16)
    ysb = [k.sb("ysb%d" % i, [P, 2048], F32) for i in range(2)]
    if SKIP_UNUSED:
        reg_gu = k.nc.gpsimd.to_reg(64 * 1024 - 1)
        reg_dn = k.nc.gpsimd.to_reg(64 * 512 - 1)
    wgu_src = d["w_gu"][:, :, :].rearrange("e (r two) n -> (e r) (two n)", two=2)
    wdn_src = d["w_dn"][:, :, :].rearrange("e r n -> (e r) n")

    def load_blk(b):
        s = b % 2
        for cc_ in range(8):
            k.idma(wgu[s][cc_][:, :, :].rearrange("p a n -> p (a n)"), None, wgu_src,
                   IOA(ap=c.IDX16[:, b, cc_:cc_ + 1], axis=0), r=[c.IDX16], w=[wgu[s][cc_]], sem=wgu[s][cc_],
                   **(dict(bounds_check=reg_gu, oob_is_err=False) if SKIP_UNUSED else {}))
        for kc in range(4):
            k.idma(wdn[s][kc][:, :], None, wdn_src, IOA(ap=c.IDX4[:, b, kc:kc + 1], axis=0), r=[c.IDX4], w=[wdn[s][kc]], sem=wdn[s][kc],
                   **(dict(bounds_check=reg_dn, oob_is_err=False) if SKIP_UNUSED else {}))
        k.dma("sp", xb[s][:, :], d["XBUF"][b * P:(b + 1) * P, :], r=[d["XBUF"]], w=[xb[s]], sem=xb[s])

    load_blk(0)
    for b in range(NBLK):
        s = b % 2
        if b + 1 < NBLK:
            load_blk(b + 1)
        xv = xb[s][:, :].rearrange("t (p kc) -> t p kc", kc=16)
        transposes(k, c, [xv[:, :, kc] for kc in range(16)], xb[s], xbT, lambda i0, cnt: xbT[:, i0:i0 + cnt, :], BF16, k.ps[7])
        for n in range(2):
            pz = k.ps[n]
            for kc in range(16):
                k.pe("matmul", pz[:, :], xbT[:, kc, :], wgu[s][kc // 2][:, kc % 2, n * 512:(n + 1) * 512], start=(kc == 0), stop=(kc == 15),
                     r=[xbT, wgu[s][kc // 2]], w=[pz])
        k.act("activation", sgl[:, :], k.ps[0][:, :], AF.Silu, r=[k.ps[0]], w=[sgl])
        k.dve("tensor_tensor", actb[:, :], sgl[:, :], k.ps[1][:, :], ALU.mult, r=[sgl, k.ps[1]], w=[actb])
        av = actb[:, :].rearrange("t (p kc) -> t p kc", kc=4)
        transposes(k, c, [av[:, :, kc] for kc in range(4)], actb, actT, lambda i0, cnt: actT[:, i0:i0 + cnt, :], BF16, k.ps[6])
        y_ = ysb[s]
        for n in range(4):
            pz = k.ps[2 + n]
            for kc in range(4):
                k.pe("matmul", pz[:, :], actT[:, kc, :], wdn[s][kc][:, n * 512:(n + 1) * 512], start=(kc == 0), stop=(kc == 3),
                     r=[actT, wdn[s][kc]], w=[pz])
            if n % 2 == 0:
                k.dve("tensor_copy", y_[:, n * 512:(n + 1) * 512], pz[:, :], r=[pz], w=[y_])
            else:
                k.act("copy", y_[:, n * 512:(n + 1) * 512], pz[:, :], r=[pz], w=[y_])
        k.dma("sp", d["YBUF"][b * P:(b + 1) * P, :], y_[:, :], r=[y_], sem=y_)
    k.end_stage()

    k.begin_stage()
    hh = [k.sb("hh%d" % i, [P, 2048], F32) for i in range(2)]
    y1 = [k.sb("y1_%d" % i, [P, 2048], F32) for i in range(2)]
    y2 = [k.sb("y2_%d" % i, [P, 2048], F32) for i in range(2)]
    scr = k.sb("scr", [P, 2048], F32)
    scr2 = k.sb("scr2", [P, 2048], F32)
    stat = k.sb("stat", [P, 32], F32)
    hnb = k.sb("hnb", [P, 2048], BF16)
    if not last:
        g_nx = k.sb("g_nx", [P, 2048], F32)
        k.dma("sp", g_nx[:, :], d["g_next"][:, :], r=[d["g_next"]], w=[g_nx], sem=g_nx)
    def ld_b4(i):
        tk = slice(i * P, (i + 1) * P)
        k.dma("sp", hh[i % 2][:, :], d["H1s"][tk, :], r=[d["H1s"]], w=[hh[i % 2]], sem=hh[i % 2])
        k.idma(y1[i % 2][:, :], None, d["YBUF"][:, :], IOA(ap=DESTI[:, i, 0:1], axis=0), r=[d["YBUF"], DESTI], w=[y1[i % 2]], sem=y1[i % 2])
        k.idma(y2[i % 2][:, :], None, d["YBUF"][:, :], IOA(ap=DESTI[:, i, 1:2], axis=0), r=[d["YBUF"], DESTI], w=[y2[i % 2]], sem=y2[i % 2])

    ld_b4(0)
    for i in range(NTOK_T):
        tok = slice(i * P, (i + 1) * P)
        h_ = hh[i % 2]
        a_ = y1[i % 2]
        b_ = y2[i % 2]
        if i + 1 < NTOK_T:
            ld_b4(i + 1)
        k.dve("scalar_tensor_tensor", h_[:, :], a_[:, :], GT[:, i, 0:1], h_[:, :], ALU.mult, ALU.add, r=[a_, GT, h_], w=[h_])
        k.dve("scalar_tensor_tensor", h_[:, :], b_[:, :], GT[:, i, 1:2], h_[:, :], ALU.mult, ALU.add, r=[b_, GT, h_], w=[h_])
        k.dma("sp", d["HOUT"][tok, :], h_[:, :], r=[h_], sem=h_)
        if not last:
            g1_ = lambda a: a.rearrange("p (g w) -> p g w", g=1)
            rms(k, c, h_, g1_(h_[:, :]), 1, 2048, g_nx[:, :], hnb, g1_(hnb[:, :]), scr, scr2, stat)
            k.dma("sp", d["HN"][tok, :], hnb[:, :], r=[hnb], sem=hnb)
    k.end_stage()

def stage_C(k, c, d):
    SCALE = 1.0 / float(np.sqrt(128.0))
    NT = SEQ // P
    g1_ = lambda a: a.rearrange("p (g w) -> p g w", g=1)
    k.begin_stage()
    g_qk = k.sb("g_qk", [P, 256], F32)
    k.dma("sp", g_qk[:, :], d["g_qk"][:, :], r=[d["g_qk"]], w=[g_qk], sem=g_qk)
    g_q = k.sb("g_q", [P, 128], F32)
    k.dve("tensor_scalar", g_q[:, :], g_qk[:, 0:128], SCALE, None, ALU.mult, r=[g_qk], w=[g_q])
    w_qk = k.sb("w_qk", [P, 16, 2048], BF16)
    k.dma("pool", w_qk[:, :, :], d["w_qk"][:, :].rearrange("(kc p) n -> p kc n", p=P), r=[d["w_qk"]], w=[w_qk], sem=w_qk)
    hn = [k.sb("hn%d" % i, [P, 2048], BF16) for i in range(2)]
    hnT = k.sb("hnT", [P, 16, P], BF16)
    zsb = k.sb("zsb", [P, 16, P], F32)
    scr = k.sb("scr", [P, 2048], F32)
    scr2 = k.sb("scr2", [P, 2048], F32)
    stat = k.sb("stat", [P, 32], F32)
    scrb = k.sb("scrb", [P, 1024], F32)
    scr2b = k.sb("scr2b", [P, 1024], F32)
    statb = k.sb("statb", [P, 32], F32)
    qn = k.sb("qn", [P, 8, P], BF16)
    kn = k.sb("kn", [P, 8, P], BF16)
    qT = k.sb("qT", [P, 8, P], BF16)
    kT = k.sb("kT", [P, 8, P], BF16)
    zflat = zsb[:, :, :].rearrange("p h d -> p (h d)")
    def ld_hn(i):
        k.dma("sp", hn[i % 2][:, :], d["HNf"][i * P:(i + 1) * P, :], r=[d["HNf"]], w=[hn[i % 2]], sem=hn[i % 2])

    ld_hn(0)
    for i in range(NT):
        tok = slice(i * P, (i + 1) * P)
        h_ = hn[i % 2]
        if i + 1 < NT:
            ld_hn(i + 1)
        transposes(k, c, [h_[:, kc * P:(kc + 1) * P] for kc in range(16)], h_, hnT, lambda i0, cnt: hnT[:, i0:i0 + cnt, :], BF16, k.ps[7])
        for n in range(4):
            pz = k.ps[n]
            for kc in range(16):
                k.pe("matmul", pz[:, :], hnT[:, kc, :], w_qk[:, kc, n * 512:(n + 1) * 512], start=(kc == 0), stop=(kc == 15),
                     r=[hnT, w_qk], w=[pz])
            k.act("copy", zflat[:, n * 512:(n + 1) * 512], pz[:, :], r=[pz], w=[zsb])
        rms_multi(k, c, [
            (zsb, zsb[:, 0:8, :], 8, 128, g_q[:, :], qn, qn[:, :, :], scr, scr2, stat),
            (zsb, zsb[:, 8:16, :], 8, 128, g_qk[:, 128:256], kn, kn[:, :, :], scrb, scr2b, statb),
        ])
        transposes(k, c, [qn[:, h, :] for h in range(8)], qn, qT, lambda i0, cnt: qT[:, i0:i0 + cnt, :], BF16, k.ps[6])
        transposes(k, c, [kn[:, h, :] for h in range(8)], kn, kT, lambda i0, cnt: kT[:, i0:i0 + cnt, :], BF16, k.ps[5])
        k.dma("sp", d["QT"][:, :, tok].rearrange("h d t -> d h t"), qT[:, :, :], r=[qT], sem=qT)
        k.dma("sp", d["KT"][:, :, tok].rearrange("h d t -> d h t"), kT[:, :, :], r=[kT], sem=kT)
    k.end_stage()

    k.begin_stage()
    w_vg = k.sb("w_vg", [P, 16, 2056], BF16)
    k.dma("pool", w_vg[:, :, :], d["w_vg"][:, :].rearrange("(kc p) n -> p kc n", p=P), r=[d["w_vg"]], w=[w_vg], sem=w_vg)
    fb = k.sb("fb", [P, 8], F32)
    k.dma("sp", fb[:, :], d["fbias"][:, :], r=[d["fbias"]], w=[fb], sem=fb)
    hn = [k.sb("hn%d" % i, [P, 2048], BF16) for i in range(2)]
    hnT = k.sb("hnT", [P, 16, P], BF16)
    vb = k.sb("vb", [P, 8, P], BF16)
    sgb = k.sb("sgb", [P, 1024], BF16)
    fx = k.sb("fx", [P, 8], F32)
    fa = k.sb("fa", [P, 8], F32)
    fe = k.sb("fe", [P, 8], F32)
    fl = k.sb("fl", [P, 8], F32)
    fm = k.sb("fm", [P, 8], F32)
    lf = k.sb("lf", [P, 8], F32)
    cc = k.sb("cc", [P, 8], F32)
    hf = k.sb("hf", [P, 8], F32)
    r1 = k.sb("r1", [P, 8], F32)
    carry = k.sb("carryc", [P, 8], F32)
    k.dve("memset", carry[:, :], 0.0, w=[carry])
    caq = k.sb("caq", [P, 8, 6], BF16)
    cak = k.sb("cak", [P, 8, 6], BF16)
    k.dve("memset", caq[:, :, :], 1.0, w=[caq])
    k.dve("memset", cak[:, :, :], 1.0, w=[cak])
    caqT = k.sb("caqT", [48, P], BF16)
    cakT = k.sb("cakT", [48, P], BF16)
    vflat = vb[:, :, :].rearrange("p h d -> p (h d)")
    def ld_hn(i):
        k.dma("sp", hn[i % 2][:, :], d["HNf"][i * P:(i + 1) * P, :], r=[d["HNf"]], w=[hn[i % 2]], sem=hn[i % 2])

    ld_hn(0)
    for i in range(NT):
        tok = slice(i * P, (i + 1) * P)
        h_ = hn[i % 2]
        if i + 1 < NT:
            ld_hn(i + 1)
        transposes(k, c, [h_[:, kc * P:(kc + 1) * P] for kc in range(16)], h_, hnT, lambda i0, cnt: hnT[:, i0:i0 + cnt, :], BF16, k.ps[7])
        for n in range(5):
            pz = k.ps[n]
            n0 = n * 512
            n1 = min(n0 + 512, 2056)
            for kc in range(16):
                k.pe("matmul", pz[:, 0:n1 - n0], hnT[:, kc, :], w_vg[:, kc, n0:n1], start=(kc == 0), stop=(kc == 15),
                     r=[hnT, w_vg], w=[pz])
            if n < 2:
                k.act("copy", vflat[:, n0:n1], pz[:, :], r=[pz], w=[vb])
            elif n < 4:
                k.act("activation", sgb[:, n0 - 1024:n1 - 1024], pz[:, :], AF.Sigmoid, r=[pz], w=[sgb])
            else:
                k.dve("tensor_tensor", fx[:, :], pz[:, 0:8], fb[:, :], ALU.add, r=[pz, fb], w=[fx])
        k.dma("sp", d["V"][:, tok, :].rearrange("h t d -> t h d"), vb[:, :, :], r=[vb], sem=vb)
        k.dma("sp", d["SG"][tok, :], sgb[:, :], r=[sgb], sem=sgb)
        k.dve("tensor_scalar", fa[:, :], fx[:, :], -1.0, None, ALU.mult, r=[fx], w=[fa])
        k.dve("tensor_tensor", fa[:, :], fa[:, :], fx[:, :], ALU.max, r=[fa, fx], w=[fa])
        k.act("activation", fe[:, :], fa[:, :], AF.Exp, scale=-1.0, r=[fa], w=[fe])
        k.act("activation", fl[:, :], fe[:, :], AF.Ln, bias=c.one[:, 0:1], r=[fe, c.one], w=[fl])
        k.dve("tensor_scalar", fm[:, :], fx[:, :], 0.0, None, ALU.min, r=[fx], w=[fm])
        k.dve("tensor_tensor", lf[:, :], fm[:, :], fl[:, :], ALU.subtract, r=[fm, fl], w=[lf])
        pc = k.ps[5]
        pc2 = k.ps[6]
        k.pe("matmul", pc[:, 0:8], c.tri_ge_f[:, :], lf[:, :], start=True, stop=True, r=[c.tri_ge_f, lf], w=[pc])
        k.dve("tensor_tensor", cc[:, :], pc[:, 0:8], carry[:, :], ALU.add, r=[pc, carry], w=[cc])
        k.pe("matmul", pc2[:, 0:8], c.ones_f[:, :], lf[:, :], start=True, stop=True, r=[c.ones_f, lf], w=[pc2])
        k.dve("tensor_tensor", carry[:, :], carry[:, :], pc2[:, 0:8], ALU.add, r=[carry, pc2], w=[carry])
        k.dve("tensor_copy", caq[:, :, 3], cc[:, :], r=[cc], w=[caq])
        k.dve("tensor_copy", hf[:, :], caq[:, :, 3], r=[caq], w=[hf])
        k.dve("tensor_scalar", cak[:, :, 0], hf[:, :], -1.0, None, ALU.mult, r=[hf], w=[cak])
        k.dve("tensor_tensor", r1[:, :], cc[:, :], hf[:, :], ALU.subtract, r=[cc, hf], w=[r1])
        k.dve("tensor_copy", caq[:, :, 4], r1[:, :], r=[r1], w=[caq])
        k.dve("tensor_copy", hf[:, :], caq[:, :, 4], r=[caq], w=[hf])
        k.dve("tensor_scalar", cak[:, :, 1], hf[:, :], -1.0, None, ALU.mult, r=[hf], w=[cak])
        k.dve("tensor_tensor", r1[:, :], r1[:, :], hf[:, :], ALU.subtract, r=[r1, hf], w=[r1])
        k.dve("tensor_copy", caq[:, :, 5], r1[:, :], r=[r1], w=[caq])
        k.dve("tensor_scalar", cak[:, :, 2], caq[:, :, 5], -1.0, None, ALU.mult, r=[caq], w=[cak])
        transposes(k, c, [caq[:, :, :].rearrange("p h i -> p (h i)")], caq, caqT,
                   lambda i0, cnt: caqT[:, :].rearrange("p (a b) -> p a b", a=1), BF16, k.ps[7])
        transposes(k, c, [cak[:, :, :].rearrange("p h i -> p (h i)")], cak, cakT,
                   lambda i0, cnt: cakT[:, :].rearrange("p (a b) -> p a b", a=1), BF16, k.ps[7])
        k.dma("sp", d["CAQ"][:, tok], caqT[:, :], r=[caqT], sem=caqT)
        k.dma("sp", d["CAK"][:, tok], cakT[:, :], r=[cakT], sem=cakT)
    k.end_stage()

    k.begin_stage()
    sets = []
    for s in range(2):
        st = dict(q=k.sb("aq%d" % s, [P, SEQ], BF16), k=k.sb("ak%d" % s, [P, SEQ], BF16),
                  cq=k.sb("acq%d" % s, [6, SEQ], BF16), ck=k.sb("ack%d" % s, [6, SEQ], BF16),
                  v=k.sb("av%d" % s, [P, NKT, 132], BF16))
        k.dve("memset", st["v"][:, :, 128:129], 1.0, w=[st["v"]])
        sets.append(st)

    def load_head(h, s):
        st = sets[s]
        k.dma("sp", st["q"][:, :], d["QT"][h, :, :], r=[d["QT"]], w=[st["q"]], sem=st["q"])
        k.dma("sp", st["k"][:, :], d["KT"][h, :, :], r=[d["KT"]], w=[st["k"]], sem=st["k"])
        k.dma("sp", st["cq"][:, :], d["CAQ"][h * 6:(h + 1) * 6, :], r=[d["CAQ"]], w=[st["cq"]], sem=st["cq"])
        k.dma("sp", st["ck"][:, :], d["CAK"][h * 6:(h + 1) * 6, :], r=[d["CAK"]], w=[st["ck"]], sem=st["ck"])
        k.dma("sp", st["v"][:, :, 0:128], d["V"][h, :, :].rearrange("(n p) d -> p n d", p=P), r=[d["V"]], w=[st["v"]], sem=st["v"])
        pairs = [
            (st["k"], (lambda kt, b=st["k"]: b[:, kt * P:(kt + 1) * P]), st["q"], (lambda a, e, b=st["q"]: b[:, a:e])),
            (st["ck"], (lambda kt, b=st["ck"]: b[:, kt * P:(kt + 1) * P]), st["cq"], (lambda a, e, b=st["cq"]: b[:, a:e])),
        ]
        return pairs, st["v"]

    attention(k, c, load_head, None, d["OTX"], gate_d=d["SG"])
    k.end_stage()
def rep128(v):
    v = np.ascontiguousarray(np.asarray(v, dtype=np.float32).reshape(1, -1))
    return np.ascontiguousarray(np.broadcast_to(v, (P, v.shape[1])))


def new_nc():
    return bass.Bass("TRN2", target_bir_lowering=False)


def build_A():
    nc = new_nc()
    k = K(nc)
    d = {}
    def ext(name, shape, dt):
        d[name] = k.dram(name, shape, dt, kind="ExternalInput")
    ext("x", [4096, 2048], F32); ext("pos", [4096, 1], I32)
    ext("g_mix", [P, 2048], F32); ext("g_ql", [P, 512], F32); ext("g_kvl", [P, 512], F32)
    ext("g_qk", [P, 384], F32); ext("freqs", [P, 32], F32)
    ext("w_in", [2048, 1088], F32); ext("w_uq", [512, 1536], F32); ext("w_ukv", [512, 2048], F32)
    d["KRT"] = k.dram("KRT", [64, 4096], BF16)
    d["QT"] = k.dram("QT", [8, 128, 4096], BF16)
    d["QRT"] = k.dram("QRT", [8, 64, 4096], BF16)
    d["KT"] = k.dram("KT", [8, 128, 4096], BF16)
    d["V"] = k.dram("V", [8, 4096, 128], BF16)
    d["OTX"] = k.dram("OTX", [2, 8, 128, 2048], BF16, kind="ExternalOutput")
    k.begin_stage()
    c = make_consts(k)
    k.end_stage()
    stage_A(k, c, d)
    k.finish()
    return nc


def inputs_A(inp, core):
    b, j = core // 2, core % 2
    half = 32
    freqs = (10000.0 ** (-np.arange(half, dtype=np.float32) / half)).astype(np.float32)
    uq = inp["l0_mla_w_uq"].reshape(512, 16, 192)[:, 8 * j:8 * j + 8, :].reshape(512, 1536)
    ukv = inp["l0_mla_w_ukv"].reshape(512, 16, 256)[:, 8 * j:8 * j + 8, :].reshape(512, 2048)
    return {
        "x": np.ascontiguousarray(inp["x"][b]),
        "pos": np.ascontiguousarray(inp["positions"][b].reshape(4096, 1).astype(np.int32)),
        "g_mix": rep128(inp["l0_norm_mix"]), "g_ql": rep128(inp["l0_mla_q_lat_norm"]),
        "g_kvl": rep128(inp["l0_mla_kv_lat_norm"]), "g_qk": rep128(inp["l0_mla_qk_gain"].reshape(-1)),
        "freqs": rep128(freqs),
        "w_in": np.ascontiguousarray(inp["l0_mla_w_in"]), "w_uq": np.ascontiguousarray(uq), "w_ukv": np.ascontiguousarray(ukv),
    }


def build_B(layer, upto=4, dbg=False):
    last = (layer == 1)
    nc = new_nc()
    k = K(nc)
    d = {}
    def ext(name, shape, dt):
        d[name] = k.dram(name, shape, dt, kind="ExternalInput")
    ext("OTin", [16, 128, 2048], BF16); ext("xres", [2048, 2048], F32); ext("w_o", [2048, 2048], F32)
    ext("g_ffn", [P, 2048], F32); ext("w_r", [2048, 72], F32); ext("b_r", [P, 72], F32)
    if upto > 2:
        ext("w_gu", [64, 2048, 1024], F32); ext("w_dn", [64, 512, 2048], F32)
    if dbg:
        d["DBG"] = k.dram("DBG", [P, 128], I32, kind="ExternalOutput")
        d["DBGF"] = k.dram("DBGF", [P, 1312], F32, kind="ExternalOutput")
    if not last:
        ext("g_next", [P, 2048], F32)
        d["HN"] = k.dram("HN", [2048, 2048], BF16, kind="ExternalOutput")
    d["H1s"] = k.dram("H1s", [2048, 2048], F32)
    d["HN2"] = k.dram("HN2", [2048, 2048], BF16)
    d["XBUF"] = k.dram("XBUF", [NBLK * P, 2048], BF16)
    d["YBUF"] = k.dram("YBUF", [NBLK * P, 2048], F32)
    d["HOUT"] = k.dram("HOUT", [2048, 2048], F32, kind="ExternalOutput")
    k.begin_stage()
    c = make_consts(k)
    k.end_stage()
    stage_WO_MOE(k, c, d, last, upto)
    k.finish()
    return nc


def inputs_B(inp, core, layer, otin, xres):
    pre = "l%d_" % layer
    wo = inp["l0_mla_w_o"] if layer == 0 else inp["l1_fox_w_o"]
    m = {
        "OTin": (None if otin is None else np.ascontiguousarray(otin)), "xres": (None if xres is None else np.ascontiguousarray(xres)), "w_o": np.ascontiguousarray(wo),
        "g_ffn": rep128(inp[pre + "norm_ffn"]),
        "w_r": np.ascontiguousarray(np.concatenate([inp[pre + "router_group"], inp[pre + "router_expert"]], axis=1)),
        "b_r": rep128(np.concatenate([inp[pre + "router_group_bias"], inp[pre + "router_expert_bias"]])),
        "w_gu": np.ascontiguousarray(inp[pre + "w_gate_up"]), "w_dn": np.ascontiguousarray(inp[pre + "w_down"]),
    }
    if layer == 0:
        m["g_next"] = rep128(inp["l1_norm_mix"])
    return m


def build_C():
    nc = new_nc()
    k = K(nc)
    d = {}
    def ext(name, shape, dt):
        d[name] = k.dram(name, shape, dt, kind="ExternalInput")
    ext("HNf", [4096, 2048], BF16); ext("w_qk", [2048, 2048], F32); ext("w_vg", [2048, 2056], F32)
    ext("fbias", [P, 8], F32); ext("g_qk", [P, 256], F32)
    d["QT"] = k.dram("QT", [8, 128, 4096], BF16)
    d["KT"] = k.dram("KT", [8, 128, 4096], BF16)
    d["V"] = k.dram("V", [8, 4096, 128], BF16)
    d["SG"] = k.dram("SG", [4096, 1024], BF16)
    d["CAQ"] = k.dram("CAQ", [48, 4096], BF16)
    d["CAK"] = k.dram("CAK", [48, 4096], BF16)
    d["OTX"] = k.dram("OTX", [2, 8, 128, 2048], BF16, kind="ExternalOutput")
    k.begin_stage()
    c = make_consts(k)
    k.end_stage()
    stage_C(k, c, d)
    k.finish()
    return nc


def inputs_C(inp, core, hnf):
    j = core % 2
    w = inp["l1_fox_w_in"]
    o = 1024 * j
    w_qk = np.concatenate([w[:, o:o + 1024], w[:, 2048 + o:2048 + o + 1024]], axis=1)
    w_vg = np.concatenate([w[:, 4096 + o:4096 + o + 1024], w[:, 6160 + o:6160 + o + 1024], w[:, 6144 + 8 * j:6144 + 8 * j + 8]], axis=1)
    return {
        "HNf": (None if hnf is None else np.ascontiguousarray(hnf)), "w_qk": np.ascontiguousarray(w_qk), "w_vg": np.ascontiguousarray(w_vg),
        "fbias": rep128(inp["l1_fox_forget_bias"][8 * j:8 * j + 8]), "g_qk": rep128(inp["l1_fox_qk_gain"].reshape(-1)),
    }


def _run(nc, maps):
    return run_bass_kernel_spmd(nc, maps, core_ids=list(range(8))).results


def build_F():
    nc = new_nc()
    k = K(nc)
    def ext(name, shape, dt):
        return k.dram(name, shape, dt, kind="ExternalInput")
    def view(ap, name):
        return Buf(ap, name)
    x = ext("x", [4096, 2048], F32)
    base_A = dict(x=x, pos=ext("pos", [4096, 1], I32), g_mix=ext("g_mix", [P, 2048], F32), g_ql=ext("g_ql", [P, 512], F32),
                  g_kvl=ext("g_kvl", [P, 512], F32), g_qk=ext("g_qk", [P, 384], F32), freqs=ext("freqs", [P, 32], F32),
                  w_in=ext("w_in", [2048, 1088], F32))
    w_uq = ext("w_uq", [512, 3072], F32)
    w_ukv = ext("w_ukv", [512, 4096], F32)
    base_A["KRT"] = k.dram("KRT", [64, 4096], BF16)
    base_A["QT"] = k.dram("QT", [8, 128, 4096], BF16)
    base_A["QRT"] = k.dram("QRT", [8, 64, 4096], BF16)
    base_A["KT"] = k.dram("KT", [8, 128, 4096], BF16)
    base_A["V"] = k.dram("V", [8, 4096, 128], BF16)
    otf = k.dram("OTF", [16, 128, 4096], BF16)
    shared = dict(H1s=k.dram("H1s", [2048, 2048], F32), HN2=k.dram("HN2", [2048, 2048], BF16),
                  XBUF=k.dram("XBUF", [NBLK * P, 2048], BF16), YBUF=k.dram("YBUF", [NBLK * P, 2048], F32))
    h2 = k.dram("H2", [4096, 2048], F32)
    hnf = k.dram("HNF", [4096, 2048], BF16)
    out = k.dram("out", [2048, 2048], F32, kind="ExternalOutput")
    otin_own = k.dram("OTIN_OWN", [16, 128, 2048], BF16)
    xres_own = k.dram("XRES_OWN", [2048, 2048], F32)
    idxo_d = ext("idxo", [P, 16], I32)
    idxr_d = ext("idxr", [P, 16], I32)

    def moe_w(layer):
        p_ = "l%d_" % layer
        return dict(w_o=ext(p_ + "w_o", [2048, 2048], F32), g_ffn=ext(p_ + "g_ffn", [P, 2048], F32),
                    w_r=ext(p_ + "w_r", [2048, 72], F32), b_r=ext(p_ + "b_r", [P, 72], F32),
                    w_gu=ext(p_ + "w_gu", [64, 2048, 1024], F32), w_dn=ext(p_ + "w_dn", [64, 512, 2048], F32))
    mw0 = moe_w(0)
    mw1 = moe_w(1)
    g_next = ext("g_next", [P, 2048], F32)
    cw = [dict(w_qk=ext("w_qk%d" % hh, [2048, 2048], F32), w_vg=ext("w_vg%d" % hh, [2048, 2056], F32),
               fbias=ext("fbias%d" % hh, [P, 8], F32)) for hh in range(2)]
    l1_g_qk = ext("l1_g_qk", [P, 256], F32)
    sg = k.dram("SG", [4096, 1024], BF16)
    caq = k.dram("CAQ", [48, 4096], BF16)
    cak = k.dram("CAK", [48, 4096], BF16)

    k.begin_stage()
    c = make_consts(k)
    idxo = k.sb("idxo_sb", [P, 16], I32, persist=True)
    idxr = k.sb("idxr_sb", [P, 16], I32, persist=True)
    k.dma("sp", idxo[:, :], idxo_d[:, :], w=[idxo], sem=idxo)
    k.dma("sp", idxr[:, :], idxr_d[:, :], w=[idxr], sem=idxr)
    k.end_stage()

    def otx_view(hh):
        return view(otf.t[hh * 8:(hh + 1) * 8, :, :].rearrange("h d (a t) -> a h d t", a=2), "otxv")

    for hh in range(2):
        dA = dict(base_A)
        dA["w_uq"] = view(w_uq.t[:, hh * 1536:(hh + 1) * 1536], "w_uq_v")
        dA["w_ukv"] = view(w_ukv.t[:, hh * 2048:(hh + 1) * 2048], "w_ukv_v")
        dA["OTX"] = otx_view(hh)
        stage_A(k, c, dA)
    for th in range(2):
        ts = slice(th * 2048, (th + 1) * 2048)
        dB = dict(shared)
        dB.update(mw0)
        dB["OTin"] = view(otf.t[:, :, ts], "otin_v")
        dB["xres"] = view(x.t[ts, :], "xres_v")
        dB["g_next"] = g_next
        dB["HOUT"] = view(h2.t[ts, :], "h2_v")
        dB["HN"] = view(hnf.t[ts, :], "hn_v")
        stage_WO_MOE(k, c, dB, False)
    for hh in range(2):
        dC = dict(HNf=hnf, QT=base_A["QT"], KT=base_A["KT"], V=base_A["V"], SG=sg, CAQ=caq, CAK=cak, g_qk=l1_g_qk)
        dC.update(cw[hh])
        dC["OTX"] = otx_view(hh)
        stage_C(k, c, dC)
    IOA = bass.IndirectOffsetOnAxis
    k.begin_stage()
    tb_ = [k.sb("selb%d" % i, [P, 2048], BF16) for i in range(2)]
    tf_ = [k.sb("self%d" % i, [P, 2048], F32) for i in range(2)]
    otf2d = otf.t.rearrange("h d (a t) -> (h d a) t", a=2)
    for hh in range(16):
        t_ = tb_[hh % 2]
        k.idma(t_[:, :], None, otf2d, IOA(ap=idxo[:, hh:hh + 1], axis=0), r=[idxo], w=[t_], sem=t_)
        k.dma("sp", otin_own.t[hh, :, :], t_[:, :], r=[t_], sem=t_)
    for i in range(16):
        t_ = tf_[i % 2]
        k.idma(t_[:, :], None, h2.t[:, :], IOA(ap=idxr[:, i:i + 1], axis=0), r=[idxr], w=[t_], sem=t_)
        k.dma("sp", xres_own.t[i * P:(i + 1) * P, :], t_[:, :], r=[t_], sem=t_)
    k.end_stage()
    dD = dict(shared)
    dD.update(mw1)
    dD["OTin"] = otin_own
    dD["xres"] = xres_own
    dD["HOUT"] = out
    stage_WO_MOE(k, c, dD, True)
    k.finish()
    return nc


def inputs_F(inp, core):
    b, j = core // 2, core % 2
    half = 32
    freqs = (10000.0 ** (-np.arange(half, dtype=np.float32) / half)).astype(np.float32)
    m = {
        "x": np.ascontiguousarray(inp["x"][b]),
        "pos": np.ascontiguousarray(inp["positions"][b].reshape(4096, 1).astype(np.int32)),
        "g_mix": rep128(inp["l0_norm_mix"]), "g_ql": rep128(inp["l0_mla_q_lat_norm"]),
        "g_kvl": rep128(inp["l0_mla_kv_lat_norm"]), "g_qk": rep128(inp["l0_mla_qk_gain"].reshape(-1)),
        "freqs": rep128(freqs),
        "w_in": np.ascontiguousarray(inp["l0_mla_w_in"]), "w_uq": np.ascontiguousarray(inp["l0_mla_w_uq"]),
        "w_ukv": np.ascontiguousarray(inp["l0_mla_w_ukv"]),
        "g_next": rep128(inp["l1_norm_mix"]),
        "l1_g_qk": rep128(inp["l1_fox_qk_gain"].reshape(-1)),
    }
    m["idxo"] = np.ascontiguousarray(((np.arange(16)[None, :] * 128 + np.arange(128)[:, None]) * 2 + j).astype(np.int32))
    m["idxr"] = np.ascontiguousarray((j * 2048 + np.arange(16)[None, :] * 128 + np.arange(128)[:, None]).astype(np.int32))
    m["l0_w_o"] = np.ascontiguousarray(inp["l0_mla_w_o"])
    m["l0_g_ffn"] = rep128(inp["l0_norm_ffn"])
    m["l0_w_r"] = np.ascontiguousarray(np.concatenate([inp["l0_router_group"], inp["l0_router_expert"]], axis=1))
    m["l0_b_r"] = rep128(np.concatenate([inp["l0_router_group_bias"], inp["l0_router_expert_bias"]]))
    m["l0_w_gu"] = np.ascontiguousarray(inp["l0_w_gate_up"])
    m["l0_w_dn"] = np.ascontiguousarray(inp["l0_w_down"])
    m["l1_w_o"] = np.ascontiguousarray(inp["l1_fox_w_o"])
    m["l1_g_ffn"] = rep128(inp["l1_norm_ffn"])
    m["l1_w_r"] = np.ascontiguousarray(np.concatenate([inp["l1_router_group"], inp["l1_router_expert"]], axis=1))
    m["l1_b_r"] = rep128(np.concatenate([inp["l1_router_group_bias"], inp["l1_router_expert_bias"]]))
    m["l1_w_gu"] = np.ascontiguousarray(inp["l1_w_gate_up"])
    m["l1_w_dn"] = np.ascontiguousarray(inp["l1_w_down"])
    w = inp["l1_fox_w_in"]
    for hh in range(2):
        o = 1024 * hh
        m["w_qk%d" % hh] = np.ascontiguousarray(np.concatenate([w[:, o:o + 1024], w[:, 2048 + o:2048 + o + 1024]], axis=1))
        m["w_vg%d" % hh] = np.ascontiguousarray(np.concatenate(
            [w[:, 4096 + o:4096 + o + 1024], w[:, 6160 + o:6160 + o + 1024], w[:, 6144 + 8 * hh:6144 + 8 * hh + 8]], axis=1))
        m["fbias%d" % hh] = rep128(inp["l1_fox_forget_bias"][8 * hh:8 * hh + 8])
    return m


def kernel(**inputs):
    inp = {k_: np.asarray(v) for k_, v in inputs.items()}
    res = _run(build_F(), [inputs_F(inp, c_) for c_ in range(8)])
    out = np.empty((4, 4096, 2048), np.float32)
    for c_ in range(8):
        b, j = c_ // 2, c_ % 2
        out[b, j * 2048:(j + 1) * 2048] = np.asarray(res[c_]["out"])
    return out
```

```python
import numpy as np
import ml_dtypes
from contextlib import ExitStack
import concourse.bass as bass
import concourse.mybir as mybir
from concourse.bass_utils import run_bass_kernel_spmd

F32 = mybir.dt.float32
BF16 = mybir.dt.bfloat16
I32 = mybir.dt.int32
U32 = mybir.dt.uint32
AF = mybir.ActivationFunctionType
ALU = mybir.AluOpType
AX = mybir.AxisListType
P = 128
EPS = 1e-6


class Sem:
    def __init__(self, h):
        self.h = h
        self.count = 0


class Buf:
    def __init__(self, t, name, persist=True):
        self.t = t
        self.name = name
        self.lw = None
        self.rd = {}
        self.sem = None
        self.persist = persist

    def __getitem__(self, key):
        return self.t[key]


class Op:
    __slots__ = ("eng", "meth", "args", "kw", "deps", "needed", "isdma", "sem", "val", "barrier")


class K:
    def __init__(self, nc):
        self.nc = nc
        self.es = ExitStack()
        self.eng = {"pe": nc.tensor, "act": nc.scalar, "dve": nc.vector, "pool": nc.gpsimd, "sp": nc.sync}
        self.esem = {}
        for e in self.eng:
            self.esem[e] = Sem(self.es.enter_context(nc.semaphore("sem_" + e)))
        self.allsems = list(self.esem.values())
        self.ops = []
        self.emitted = 0
        self.waited = {e: {} for e in self.eng}
        self.last = {}
        self.nbuf = 0
        self.stage = None
        self.last_barrier = 0
        self.free_sems = []
        self.stage_bufs = []
        self.ps = [Buf(self.es.enter_context(nc.psum_tensor("psb%d" % i, [P, 512], F32)), "psb%d" % i) for i in range(8)]

    def begin_stage(self):
        self.stage = ExitStack()

    def end_stage(self):
        self.barrier()
        self.flush()
        self.stage.close()
        self.stage = None
        for b in self.stage_bufs:
            if b.sem is not None:
                self.free_sems.append(b.sem)
                b.sem = None
        self.stage_bufs = []

    def sb(self, name, shape, dt, persist=False):
        st = self.es if persist else self.stage
        self.nbuf += 1
        t = st.enter_context(self.nc.sbuf_tensor("%s_%d" % (name, self.nbuf), list(shape), dt))
        b = Buf(t, name, persist)
        if not persist:
            self.stage_bufs.append(b)
        return b

    def dram(self, name, shape, dt, kind="Internal"):
        t = self.nc.dram_tensor(name, list(shape), dt, kind=kind).ap()
        return Buf(t, name)

    def _sem_of(self, b):
        if b.sem is None and (not b.persist) and self.free_sems:
            b.sem = self.free_sems.pop()
        if b.sem is None:
            b.sem = Sem(self.es.enter_context(self.nc.semaphore("dsem_%d_%s" % (len(self.allsems), b.name))))
            self.allsems.append(b.sem)
        return b.sem

    def _rec(self, eng, meth, args, kw, r, w, isdma=False, sembuf=None):
        op = Op()
        op.eng, op.meth, op.args, op.kw = eng, meth, args, kw
        op.isdma = isdma
        op.needed = False
        op.val = None
        op.barrier = False
        op.sem = self._sem_of(sembuf) if isdma else None
        idx = len(self.ops)
        deps = set()
        for b in r:
            if b.lw is not None:
                deps.add(b.lw)
        for b in w:
            if b.lw is not None:
                deps.add(b.lw)
            deps.update(b.rd.values())
        deps.discard(idx)
        op.deps = []
        for d in deps:
            if d < self.last_barrier:
                continue
            p = self.ops[d]
            if (not p.isdma) and (not isdma) and p.eng == "pe" and eng == "pe":
                continue
            p.needed = True
            op.deps.append(d)
        key = ("dma", id(op.sem)) if isdma else eng
        for b in r:
            b.rd[key] = idx
        for b in w:
            b.lw = idx
            b.rd = {}
        if not isdma:
            self.last[eng] = idx
        self.ops.append(op)
        return op

    def pe(self, meth, *args, r=(), w=(), **kw):
        return self._rec("pe", meth, args, kw, r, w)

    def act(self, meth, *args, r=(), w=(), **kw):
        return self._rec("act", meth, args, kw, r, w)

    def dve(self, meth, *args, r=(), w=(), **kw):
        return self._rec("dve", meth, args, kw, r, w)

    def pool(self, meth, *args, r=(), w=(), **kw):
        return self._rec("pool", meth, args, kw, r, w)

    def dma(self, q, out, in_, r=(), w=(), sem=None, **kw):
        return self._rec(q, "dma_start", (), dict(out=out, in_=in_, **kw), r, w, isdma=True, sembuf=sem)

    def idma(self, out, out_off, in_, in_off, r=(), w=(), sem=None, **kw):
        return self._rec("pool", "indirect_dma_start", (out, out_off, in_, in_off), kw, r, w, isdma=True, sembuf=sem)

    def collective(self, kind, in_ap, out_ap, out_buf, groups):
        return self._rec("pool", "collective_compute", (kind, ALU.bypass, groups, [in_ap], [out_ap]), {}, (), (out_buf,),
                         isdma=True, sembuf=out_buf)

    def barrier(self):
        op = Op()
        op.barrier = True
        op.isdma = False
        op.deps = []
        op.needed = False
        for e, idx in self.last.items():
            self.ops[idx].needed = True
        self.ops.append(op)
        self.last_barrier = len(self.ops)

    def _wait(self, eng, s, tgt):
        if tgt <= 0:
            return
        if self.waited[eng].get(id(s), 0) < tgt:
            self.eng[eng].wait_ge(s.h, tgt)
            self.waited[eng][id(s)] = tgt

    def flush(self):
        ops = self.ops
        while self.emitted < len(ops):
            op = ops[self.emitted]
            self.emitted += 1
            if op.barrier:
                for e in self.eng:
                    for s in self.allsems:
                        self._wait(e, s, s.count)
                continue
            need = {}
            for d in op.deps:
                p = ops[d]
                if p.isdma:
                    s = p.sem
                    tgt = s.count
                else:
                    s = self.esem[p.eng]
                    tgt = p.val
                    assert tgt is not None
                if need.get(id(s), (None, 0))[1] < tgt:
                    need[id(s)] = (s, tgt)
            for s, tgt in need.values():
                self._wait(op.eng, s, tgt)
            ins = getattr(self.eng[op.eng], op.meth)(*op.args, **op.kw)
            if op.isdma:
                op.sem.count += 16
                ins.then_inc(op.sem.h, 16)
            elif op.needed:
                s = self.esem[op.eng]
                s.count += 1
                op.val = s.count
                ins.then_inc(s.h, 1)
            op.args = None
            op.kw = None

    def finish(self):
        self.barrier()
        self.flush()
        self.es.close()


def bcast(ap, axis, n):
    pairs = [list(x) for x in ap.ap]
    pairs.insert(axis, [0, n])
    return bass.AP(ap.tensor, ap.offset, pairs)

class Consts:
    pass


def make_consts(k):
    c = Consts()
    nc = k.nc
    io = k.sb("iota_i", [P, P], I32, persist=True)
    k.pool("iota", io[:, :], [[1, P]], base=0, channel_multiplier=-1, w=[io])
    iof = k.sb("iota_f", [P, P], F32, persist=True)
    k.dve("tensor_copy", iof[:, :], io[:, :], r=[io], w=[iof])
    c.ident_f = k.sb("ident_f", [P, P], F32, persist=True)
    k.dve("tensor_single_scalar", c.ident_f[:, :], iof[:, :], 0.0, ALU.is_equal, r=[iof], w=[c.ident_f])
    c.ident_b = k.sb("ident_b", [P, P], BF16, persist=True)
    k.dve("tensor_copy", c.ident_b[:, :], c.ident_f[:, :], r=[c.ident_f], w=[c.ident_b])
    c.tri_ge_f = k.sb("tri_ge_f", [P, P], F32, persist=True)
    k.dve("tensor_single_scalar", c.tri_ge_f[:, :], iof[:, :], 0.0, ALU.is_ge, r=[iof], w=[c.tri_ge_f])
    c.tri_ge_b = k.sb("tri_ge_b", [P, P], BF16, persist=True)
    k.dve("tensor_copy", c.tri_ge_b[:, :], c.tri_ge_f[:, :], r=[c.tri_ge_f], w=[c.tri_ge_b])
    c.tri_gt_f = k.sb("tri_gt_f", [P, P], F32, persist=True)
    k.dve("tensor_single_scalar", c.tri_gt_f[:, :], iof[:, :], 0.0, ALU.is_gt, r=[iof], w=[c.tri_gt_f])
    c.ones_f = k.sb("ones_f", [P, P], F32, persist=True)
    k.dve("memset", c.ones_f[:, :], 1.0, w=[c.ones_f])
    c.eps = k.sb("eps_t", [P, 1], F32, persist=True)
    k.dve("memset", c.eps[:, :], EPS, w=[c.eps])
    c.one = k.sb("one_t", [P, 1], F32, persist=True)
    k.dve("memset", c.one[:, :], 1.0, w=[c.one])
    c.negpi = k.sb("negpi_t", [P, 1], F32, persist=True)
    k.dve("memset", c.negpi[:, :], -float(np.pi) * (1.0 - 1e-6), w=[c.negpi])
    ci = k.sb("col_i", [P, 64], I32, persist=True)
    k.pool("iota", ci[:, :], [[1, 64]], base=0, channel_multiplier=0, w=[ci])
    c.colidx = k.sb("col_f", [P, 64], F32, persist=True)
    k.dve("tensor_copy", c.colidx[:, :], ci[:, :], r=[ci], w=[c.colidx])
    pi_ = k.sb("part_i", [P, 1], I32, persist=True)
    k.pool("iota", pi_[:, :], [[0, 1]], base=0, channel_multiplier=1, w=[pi_])
    c.partidx = k.sb("part_f", [P, 1], F32, persist=True)
    k.dve("tensor_copy", c.partidx[:, :], pi_[:, :], r=[pi_], w=[c.partidx])
    c.LOG = k.sb("LOG", [P, 16, 72], F32, persist=True)
    c.MSK = k.sb("MSK", [P, 16, 2, 64], F32, persist=True)
    c.GT = k.sb("GT", [P, 16, 2], F32, persist=True)
    c.CUM = k.sb("CUM", [P, 16, 64], F32, persist=True)
    c.DESTI = k.sb("DESTI", [P, 16, 2], I32, persist=True)
    c.IDXW = k.sb("IDXW", [P, 96], I32, persist=True)
    c.IDX16 = k.sb("IDX16", [P, 96, 16], I32, persist=True)
    c.IDX4 = k.sb("IDX4", [P, 96, 4], I32, persist=True)
    return c


def rms(k, c, src, src_ap, G, W, gain_ap, out, out_ap, scr, scr2, stat, post_scale=None):
    sq = scr[:, 0:G * W].rearrange("p (g w) -> p g w", g=G)
    k.dve("tensor_tensor", sq, src_ap, src_ap, ALU.mult, r=[src], w=[scr])
    ssq = stat[:, 0:G]
    k.dve("tensor_reduce", ssq, sq, AX.X, ALU.add, r=[scr], w=[stat])
    std = stat[:, G:2 * G]
    k.act("activation", std, ssq, AF.Sqrt, bias=c.eps[:, 0:1], scale=1.0 / W, r=[stat, c.eps], w=[stat])
    k.dve("reciprocal", ssq, std, r=[stat], w=[stat])
    y = scr2[:, 0:G * W].rearrange("p (g w) -> p g w", g=G)
    k.dve("tensor_tensor", y, src_ap, bcast(ssq, 2, W), ALU.mult, r=[src, stat], w=[scr2])
    k.pool("tensor_tensor", out_ap, y, bcast(gain_ap, 1, G), ALU.mult, r=[scr2], w=[out])


def transposes(k, c, srcs, src_buf, dst_buf, dst_ap_fn, dt, ps_buf, evac="act"):
    n = srcs[0].shape[1]
    rows = srcs[0].shape[0]
    per = (1024 if dt == BF16 else 512) // P
    ident = c.ident_b if dt == BF16 else c.ident_f
    if dt == BF16:
        pst = ps_buf.t[:, :].bitcast(BF16)
    else:
        pst = ps_buf.t[:, :]
    i = 0
    while i < len(srcs):
        cnt = min(per, len(srcs) - i)
        for j in range(cnt):
            k.pe("transpose", pst[0:n, j * P:j * P + rows], srcs[i + j], ident[0:rows, 0:rows], r=[src_buf, ident], w=[ps_buf])
        src_v = pst[0:n, 0:cnt * P].rearrange("p (a b) -> p a b", a=cnt)[:, :, 0:rows]
        if evac == "act":
            k.act("copy", dst_ap_fn(i, cnt), src_v, r=[ps_buf], w=[dst_buf])
        else:
            k.dve("tensor_copy", dst_ap_fn(i, cnt), src_v, r=[ps_buf], w=[dst_buf])
        i += cnt


def rms_multi(k, c, specs):
    sqs, ssqs, stds, ys = [], [], [], []
    for (src, src_ap, G, W, gain_ap, out, out_ap, scr, scr2, stat) in specs:
        sq = scr[:, 0:G * W].rearrange("p (g w) -> p g w", g=G)
        k.dve("tensor_tensor", sq, src_ap, src_ap, ALU.mult, r=[src], w=[scr])
        sqs.append(sq)
    for i, (src, src_ap, G, W, gain_ap, out, out_ap, scr, scr2, stat) in enumerate(specs):
        ssq = stat[:, 0:G]
        k.dve("tensor_reduce", ssq, sqs[i], AX.X, ALU.add, r=[scr], w=[stat])
        ssqs.append(ssq)
    for i, (src, src_ap, G, W, gain_ap, out, out_ap, scr, scr2, stat) in enumerate(specs):
        std = stat[:, G:2 * G]
        k.act("activation", std, ssqs[i], AF.Sqrt, bias=c.eps[:, 0:1], scale=1.0 / W, r=[stat, c.eps], w=[stat])
        stds.append(std)
    for i, (src, src_ap, G, W, gain_ap, out, out_ap, scr, scr2, stat) in enumerate(specs):
        k.dve("reciprocal", ssqs[i], stds[i], r=[stat], w=[stat])
    for i, (src, src_ap, G, W, gain_ap, out, out_ap, scr, scr2, stat) in enumerate(specs):
        y = scr2[:, 0:G * W].rearrange("p (g w) -> p g w", g=G)
        k.dve("tensor_tensor", y, src_ap, bcast(ssqs[i], 2, W), ALU.mult, r=[src, stat], w=[scr2])
        ys.append(y)
    for i, (src, src_ap, G, W, gain_ap, out, out_ap, scr, scr2, stat) in enumerate(specs):
        k.pool("tensor_tensor", out_ap, ys[i], bcast(gain_ap, 1, G), ALU.mult, r=[scr2], w=[out])

NQT = 8
NKT = 32
SEQ = 4096


def attention(k, c, load_head, kshared, otx, gate_d=None):
    NH = 8
    pt = [k.sb("pt%d" % i, [P, 512], BF16) for i in range(4)]
    osb = [k.sb("osb%d" % i, [P, P], BF16) for i in range(2)]
    rec = [k.sb("rec%d" % i, [P, 1], F32) for i in range(2)]
    ost = [k.sb("ost%d" % i, [P, 512], BF16) for i in range(2)]
    sg = [k.sb("sg%d" % i, [P, 4, P], BF16) for i in range(2)] if gate_d is not None else None
    S = [k.ps[0], k.ps[1], k.ps[7]]
    oacc = [k.ps[2], k.ps[3], k.ps[4], k.ps[5]]
    pst = k.ps[6]
    st = dict(pcount=0, fin=0)

    def emit_qk(pairs, Q, kt):
        j = kt - 4 * Q
        q0 = max(j, 0) * P
        Sb = S[kt % 3]
        npair = len(pairs)
        for pi, (kb, kfn, qb, qfn) in enumerate(pairs):
            k.pe("matmul", Sb[:, q0:512], kfn(kt), qfn(Q * 512 + q0, (Q + 1) * 512),
                 start=(pi == 0), stop=(pi == npair - 1), r=[kb, qb], w=[Sb])

    def emit_rest(vbuf, Q, kt):
        j = kt - 4 * Q
        q0 = max(j, 0) * P
        Sb = S[kt % 3]
        Pb = pt[st["pcount"] % 4]
        st["pcount"] += 1
        k.act("activation", Pb[:, q0:512], Sb[:, q0:512], AF.Exp, r=[Sb], w=[Pb])
        if j >= 0:
            k.dve("tensor_tensor", Pb[:, j * P:(j + 1) * P], Pb[:, j * P:(j + 1) * P], c.tri_ge_b[:, :], ALU.mult,
                  r=[Pb, c.tri_ge_b], w=[Pb])
        for qs in range(max(j, 0), 4):
            ob = oacc[qs]
            k.pe("matmul", ob[:, 0:129], Pb[:, qs * P:(qs + 1) * P], vbuf[:, kt, 0:129],
                 start=(kt == 0), stop=(kt == 4 * Q + qs), r=[Pb, vbuf], w=[ob])

    def emit_fin(h, Q, sgt):
        stg = ost[Q % 2]
        for qs in range(4):
            ob = oacc[qs]
            rc = rec[st["fin"] % 2]
            o_ = osb[st["fin"] % 2]
            st["fin"] += 1
            k.dve("reciprocal", rc[:, :], ob[:, 128:129], r=[ob], w=[rc])
            k.dve("tensor_scalar", o_[:, :], ob[:, 0:128], rc[:, 0:1], None, ALU.mult, r=[ob, rc], w=[o_])
            if sgt is not None:
                k.dve("tensor_tensor", o_[:, :], o_[:, :], sgt[:, qs, :], ALU.mult, r=[o_, sgt], w=[o_])
            pv = pst.t[:, :].bitcast(BF16)
            k.pe("transpose", pv[:, 0:P], o_[:, :], c.ident_b[:, :], r=[o_, c.ident_b], w=[pst])
            k.act("copy", stg[:, qs * P:(qs + 1) * P], pv[:, 0:P], r=[pst], w=[stg])
        k.dma("sp", otx[Q // 4, h, :, (Q % 4) * 512:(Q % 4 + 1) * 512], stg[:, :], r=[stg], sem=stg)

    hb = [load_head(0, 0)]
    pending = None
    for h in range(NH):
        if h + 1 < NH:
            hb.append(load_head(h + 1, (h + 1) % 2))
        pairs, vbuf = hb[h]
        for Q in range(NQT):
            sgt = None
            if gate_d is not None:
                sgt = sg[Q % 2]
                k.dma("sp", sgt[:, :, :], gate_d[Q * 512:(Q + 1) * 512, h * P:(h + 1) * P].rearrange("(s p) d -> p s d", p=P),
                      r=[gate_d], w=[sgt], sem=sgt)
            nk = 4 * (Q + 1)
            emit_qk(pairs, Q, 0)
            emit_qk(pairs, Q, 1)
            if pending is not None:
                emit_fin(*pending)
                pending = None
            for kt in range(nk):
                if kt + 2 < nk:
                    emit_qk(pairs, Q, kt + 2)
                emit_rest(vbuf, Q, kt)
            pending = (h, Q, sgt)
    emit_fin(*pending)


def stage_A(k, c, d):
    SCALE = 1.0 / float(np.sqrt(192.0))
    NT = SEQ // P
    k.begin_stage()
    g_mix = k.sb("g_mix", [P, 2048], F32)
    g_ql = k.sb("g_ql", [P, 512], F32)
    g_kvl = k.sb("g_kvl", [P, 512], F32)
    g_qk = k.sb("g_qk", [P, 384], F32)
    g_q = k.sb("g_q", [P, 192], F32)
    freqs = k.sb("freqs", [P, 32], F32)
    for t_, s_ in ((g_mix, d["g_mix"]), (g_ql, d["g_ql"]), (g_kvl, d["g_kvl"]), (g_qk, d["g_qk"]), (freqs, d["freqs"])):
        k.dma("sp", t_[:, :], s_[:, :], r=[s_], w=[t_], sem=t_)
    k.dve("tensor_scalar", g_q[:, :], g_qk[:, 0:192], SCALE, None, ALU.mult, r=[g_qk], w=[g_q])
    w_in = k.sb("w_in", [P, 16, 1088], BF16)
    k.dma("pool", w_in[:, :, :], d["w_in"][:, :].rearrange("(kc p) n -> p kc n", p=P), r=[d["w_in"]], w=[w_in], sem=w_in)
    w_uq = k.sb("w_uq", [P, 4, 1536], BF16)
    k.dma("pool", w_uq[:, :, :], d["w_uq"][:, :].rearrange("(kc p) n -> p kc n", p=P), r=[d["w_uq"]], w=[w_uq], sem=w_uq)
    w_ukv = k.sb("w_ukv", [P, 4, 2048], BF16)
    k.dma("pool", w_ukv[:, :, :], d["w_ukv"][:, :].rearrange("(kc p) n -> p kc n", p=P), r=[d["w_ukv"]], w=[w_ukv], sem=w_ukv)

    xt = [k.sb("xt%d" % i, [P, 2048], F32) for i in range(2)]
    posi = [k.sb("posi%d" % i, [P, 1], I32) for i in range(2)]
    posf = k.sb("posf", [P, 1], F32)
    scr = k.sb("scr", [P, 2048], F32)
    scr2 = k.sb("scr2", [P, 2048], F32)
    stat = k.sb("stat", [P, 32], F32)
    sA = [k.sb("sA%d" % i, [P, 1024], F32) for i in range(2)]
    sB = [k.sb("sB%d" % i, [P, 512], F32) for i in range(2)]
    sC = [scr, scr2]
    stA = k.sb("stA", [P, 32], F32)
    stB = k.sb("stB", [P, 32], F32)
    stC = k.sb("stC", [P, 32], F32)
    hn = k.sb("hn", [P, 2048], BF16)
    hnT = k.sb("hnT", [P, 16, P], BF16)
    z = k.sb("z", [P, 1088], F32)
    cqn = k.sb("cqn", [P, 1024], BF16)
    latT = k.sb("latT", [P, 8, P], BF16)
    kr = k.sb("kr", [P, 64], F32)
    krb = k.sb("krb", [P, 64], BF16)
    krT = k.sb("krT", [64, P], BF16)
    qsb = k.sb("qsb", [P, 8, 192], F32)
    kvsb = k.sb("kvsb", [P, 8, 256], F32)
    qn = k.sb("qn", [P, 8, P], BF16)
    qr = k.sb("qr", [P, 8, 64], F32)
    qrb = k.sb("qrb", [P, 8, 64], BF16)
    kn = k.sb("kn", [P, 8, P], BF16)
    vb = k.sb("vb", [P, 8, P], BF16)
    qT = k.sb("qT", [P, 8, P], BF16)
    qrT = k.sb("qrT", [64, 8, P], BF16)
    kT = k.sb("kT", [P, 8, P], BF16)
    ang = k.sb("ang", [P, 32], F32)
    fr = k.sb("fr", [P, 2, 32], F32)
    fri = k.sb("fri", [P, 2, 32], I32)
    frf = k.sb("frf", [P, 2, 32], F32)
    cs = k.sb("cs", [P, 2, 32], F32)
    rt = k.sb("rt", [P, 8, 4, 32], F32)
    pstr = k.ps[7]

    def rope(src, src_ap_fn, G, dst, dst_ap_fn):
        x1 = src_ap_fn(0)
        x2 = src_ap_fn(1)
        sinb = bcast(cs[:, 0, :], 1, G)
        cosb = bcast(cs[:, 1, :], 1, G)
        t = [rt[:, 0:G, i, :] for i in range(4)]
        k.dve("tensor_tensor", t[0], x1, cosb, ALU.mult, r=[src, cs], w=[rt])
        k.dve("tensor_tensor", t[1], x2, sinb, ALU.mult, r=[src, cs], w=[rt])
        k.dve("tensor_tensor", t[2], x2, cosb, ALU.mult, r=[src, cs], w=[rt])
        k.dve("tensor_tensor", t[3], x1, sinb, ALU.mult, r=[src, cs], w=[rt])
        k.dve("tensor_tensor", dst_ap_fn(0), t[0], t[1], ALU.subtract, r=[rt], w=[dst])
        k.dve("tensor_tensor", dst_ap_fn(1), t[2], t[3], ALU.add, r=[rt], w=[dst])

    def ld_a1(i):
        k.dma("sp", xt[i % 2][:, :], d["x"][i * P:(i + 1) * P, :], r=[d["x"]], w=[xt[i % 2]], sem=xt[i % 2])
        k.dma("sp", posi[i % 2][:, :], d["pos"][i * P:(i + 1) * P, :], r=[d["pos"]], w=[posi[i % 2]], sem=posi[i % 2])

    ld_a1(0)
    for i in range(NT):
        x_ = xt[i % 2]
        p_ = posi[i % 2]
        if i + 1 < NT:
            ld_a1(i + 1)
        k.dve("tensor_copy", posf[:, :], p_[:, :], r=[p_], w=[posf])
        k.dve("tensor_scalar", ang[:, :], freqs[:, :], posf[:, 0:1], 1.0 / (2 * np.pi), ALU.mult, ALU.mult, r=[freqs, posf], w=[ang])
        k.dve("tensor_scalar", fr[:, 0, :], ang[:, :], 0.5, None, ALU.add, r=[ang], w=[fr])
        k.dve("tensor_scalar", fr[:, 1, :], ang[:, :], 0.75, None, ALU.add, r=[ang], w=[fr])
        k.dve("tensor_copy", fri[:, :, :], fr[:, :, :], r=[fr], w=[fri])
        k.dve("tensor_copy", frf[:, :, :], fri[:, :, :], r=[fri], w=[frf])
        k.dve("tensor_tensor", fr[:, :, :], fr[:, :, :], frf[:, :, :], ALU.subtract, r=[fr, frf], w=[fr])
        k.dve("tensor_single_scalar", frf[:, :, :], fr[:, :, :], 0.0, ALU.is_lt, r=[fr], w=[frf])
        k.dve("tensor_tensor", fr[:, :, :], fr[:, :, :], frf[:, :, :], ALU.add, r=[fr, frf], w=[fr])
        k.act("activation", cs[:, :, :], fr[:, :, :], AF.Sin, bias=c.negpi[:, 0:1], scale=2 * float(np.pi) * (1.0 - 1e-6),
              r=[fr, c.negpi], w=[cs])
        rms(k, c, x_, x_[:, :].rearrange("p (g w) -> p g w", g=1), 1, 2048, g_mix[:, :], hn,
            hn[:, :].rearrange("p (g w) -> p g w", g=1), scr, scr2, stat)
        transposes(k, c, [hn[:, kc * P:(kc + 1) * P] for kc in range(16)], hn, hnT,
                   lambda i0, cnt: hnT[:, i0:i0 + cnt, :], BF16, pstr)
        for n, (n0, n1) in enumerate(((0, 512), (512, 1024), (1024, 1088))):
            pz = k.ps[n]
            for kc in range(16):
                k.pe("matmul", pz[:, 0:n1 - n0], hnT[:, kc, :], w_in[:, kc, n0:n1], start=(kc == 0), stop=(kc == 15),
                     r=[hnT, w_in], w=[pz])
            k.act("copy", z[:, n0:n1], pz[:, 0:n1 - n0], r=[pz], w=[z])
        g1_ = lambda a_: a_.rearrange("p (g w) -> p g w", g=1)
        rms_multi(k, c, [
            (z, g1_(z[:, 0:512]), 1, 512, g_ql[:, :], cqn, g1_(cqn[:, 0:512]), sA[0], sA[1], stA),
            (z, g1_(z[:, 512:1024]), 1, 512, g_kvl[:, :], cqn, g1_(cqn[:, 512:1024]), sC[0], sC[1], stC),
            (z, g1_(z[:, 1024:1088]), 1, 64, g_qk[:, 320:384], kr, g1_(kr[:, :]), sB[0], sB[1], stB),
        ])
        rope(kr, lambda hf: kr[:, hf * 32:(hf + 1) * 32].rearrange("p (g w) -> p g w", g=1), 1,
             krb, lambda hf: krb[:, hf * 32:(hf + 1) * 32].rearrange("p (g w) -> p g w", g=1))
        transposes(k, c, [cqn[:, j * P:(j + 1) * P] for j in range(8)], cqn, latT,
                   lambda i0, cnt: latT[:, i0:i0 + cnt, :], BF16, pstr)
        transposes(k, c, [krb[:, :]], krb, krT, lambda i0, cnt: krT[:, :].rearrange("p (a b) -> p a b", a=1), BF16, pstr)
        k.dma("sp", d["KRT"][:, i * P:(i + 1) * P], krT[:, :], r=[krT], sem=krT)
        qflat = qsb[:, :, :].rearrange("p h d -> p (h d)")
        for n in range(3):
            pz = k.ps[3 + n]
            for kc in range(4):
                k.pe("matmul", pz[:, :], latT[:, kc, :], w_uq[:, kc, n * 512:(n + 1) * 512], start=(kc == 0), stop=(kc == 3),
                     r=[latT, w_uq], w=[pz])
            k.act("copy", qflat[:, n * 512:(n + 1) * 512], pz[:, :], r=[pz], w=[qsb])
        kvflat = kvsb[:, :, :].rearrange("p h d -> p (h d)")
        for n in range(4):
            pz = k.ps[(0, 1, 2, 6)[n]]
            for kc in range(4):
                k.pe("matmul", pz[:, :], latT[:, 4 + kc, :], w_ukv[:, kc, n * 512:(n + 1) * 512], start=(kc == 0), stop=(kc == 3),
                     r=[latT, w_ukv], w=[pz])
            k.act("copy", kvflat[:, n * 512:(n + 1) * 512], pz[:, :], r=[pz], w=[kvsb])
        rms_multi(k, c, [
            (qsb, qsb[:, :, 128:192], 8, 64, g_q[:, 128:192], qr, qr[:, :, :], sB[0], sB[1], stB),
            (qsb, qsb[:, :, 0:128], 8, 128, g_q[:, 0:128], qn, qn[:, :, :], sA[0], sA[1], stA),
            (kvsb, kvsb[:, :, 0:128], 8, 128, g_qk[:, 192:320], kn, kn[:, :, :], sC[0], sC[1], stC),
        ])
        rope(qr, lambda hf: qr[:, :, hf * 32:(hf + 1) * 32], 8, qrb, lambda hf: qrb[:, :, hf * 32:(hf + 1) * 32])
        k.pool("tensor_copy", vb[:, :, :], kvsb[:, :, 128:256], r=[kvsb], w=[vb])
        transposes(k, c, [qn[:, h, :] for h in range(8)], qn, qT, lambda i0, cnt: qT[:, i0:i0 + cnt, :], BF16, pstr)
        transposes(k, c, [qrb[:, h, :] for h in range(8)], qrb, qrT, lambda i0, cnt: qrT[:, i0:i0 + cnt, :], BF16, pstr)
        transposes(k, c, [kn[:, h, :] for h in range(8)], kn, kT, lambda i0, cnt: kT[:, i0:i0 + cnt, :], BF16, pstr)
        tok = slice(i * P, (i + 1) * P)
        k.dma("sp", d["QT"][:, :, tok].rearrange("h d t -> d h t"), qT[:, :, :], r=[qT], sem=qT)
        k.dma("sp", d["QRT"][:, :, tok].rearrange("h d t -> d h t"), qrT[:, :, :], r=[qrT], sem=qrT)
        k.dma("sp", d["KT"][:, :, tok].rearrange("h d t -> d h t"), kT[:, :, :], r=[kT], sem=kT)
        k.dma("sp", d["V"][:, tok, :].rearrange("h t d -> t h d"), vb[:, :, :], r=[vb], sem=vb)
    k.end_stage()

    k.begin_stage()
    krt_sb = k.sb("krt_sb", [64, SEQ], BF16)
    k.dma("sp", krt_sb[:, :], d["KRT"][:, :], r=[d["KRT"]], w=[krt_sb], sem=krt_sb)
    sets = []
    for s in range(2):
        st = dict(q=k.sb("aq%d" % s, [P, SEQ], BF16), qr=k.sb("aqr%d" % s, [64, SEQ], BF16),
                  k=k.sb("ak%d" % s, [P, SEQ], BF16), v=k.sb("av%d" % s, [P, NKT, 132], BF16))
        k.dve("memset", st["v"][:, :, 128:129], 1.0, w=[st["v"]])
        sets.append(st)

    def load_head(h, s):
        st = sets[s]
        k.dma("sp", st["q"][:, :], d["QT"][h, :, :], r=[d["QT"]], w=[st["q"]], sem=st["q"])
        k.dma("sp", st["qr"][:, :], d["QRT"][h, :, :], r=[d["QRT"]], w=[st["qr"]], sem=st["qr"])
        k.dma("sp", st["k"][:, :], d["KT"][h, :, :], r=[d["KT"]], w=[st["k"]], sem=st["k"])
        k.dma("sp", st["v"][:, :, 0:128], d["V"][h, :, :].rearrange("(n p) d -> p n d", p=P), r=[d["V"]], w=[st["v"]], sem=st["v"])
        pairs = [
            (st["k"], (lambda kt, b=st["k"]: b[:, kt * P:(kt + 1) * P]), st["q"], (lambda a, e, b=st["q"]: b[:, a:e])),
            (krt_sb, (lambda kt: krt_sb[:, kt * P:(kt + 1) * P]), st["qr"], (lambda a, e, b=st["qr"]: b[:, a:e])),
        ]
        return pairs, st["v"]

    attention(k, c, load_head, None, d["OTX"])
    k.end_stage()

SKIP_UNUSED = True
NBLK = 96
NTOK_T = 16


def stage_WO_MOE(k, c, d, last, upto=4):
    IOA = bass.IndirectOffsetOnAxis
    LOG, MSK, GT, CUM, DESTI, IDXW = c.LOG, c.MSK, c.GT, c.CUM, c.DESTI, c.IDXW
    k.begin_stage()
    w_o = k.sb("w_o", [P, 16, 2048], BF16)
    k.dma("pool", w_o[:, :, :], d["w_o"][:, :].rearrange("(h p) n -> p h n", p=P), r=[d["w_o"]], w=[w_o], sem=w_o)
    g_ffn = k.sb("g_ffn", [P, 2048], F32)
    k.dma("sp", g_ffn[:, :], d["g_ffn"][:, :], r=[d["g_ffn"]], w=[g_ffn], sem=g_ffn)
    w_r = k.sb("w_r", [P, 16, 72], F32)
    k.dma("sp", w_r[:, :, :], d["w_r"][:, :].rearrange("(kc p) n -> p kc n", p=P), r=[d["w_r"]], w=[w_r], sem=w_r)
    b_r = k.sb("b_r", [P, 72], F32)
    k.dma("sp", b_r[:, :], d["b_r"][:, :], r=[d["b_r"]], w=[b_r], sem=b_r)
    ot = [k.sb("ot%d" % i, [P, 16, P], BF16) for i in range(2)]
    xr = [k.sb("xr%d" % i, [P, 2048], F32) for i in range(2)]
    h1s = [k.sb("h1_%d" % i, [P, 2048], F32) for i in range(2)]
    scr = k.sb("scr", [P, 2048], F32)
    scr2 = k.sb("scr2", [P, 2048], F32)
    stat = k.sb("stat", [P, 32], F32)
    hn2s = [k.sb("hn2_%d" % i, [P, 2048], BF16) for i in range(2)]
    hn2T = k.sb("hn2T", [P, 16, P], F32)
    def ld_b1(i):
        tk = slice(i * P, (i + 1) * P)
        k.dma("sp", ot[i % 2][:, :, :], d["OTin"][:, :, tk].rearrange("h d t -> d h t"), r=[d["OTin"]], w=[ot[i % 2]], sem=ot[i % 2])
        k.dma("sp", xr[i % 2][:, :], d["xres"][tk, :], r=[d["xres"]], w=[xr[i % 2]], sem=xr[i % 2])

    ld_b1(0)
    for i in range(NTOK_T):
        tok = slice(i * P, (i + 1) * P)
        o_ = ot[i % 2]
        x_ = xr[i % 2]
        h1 = h1s[i % 2]
        hn2 = hn2s[i % 2]
        if i + 1 < NTOK_T:
            ld_b1(i + 1)
        for n in range(4):
            pz = k.ps[n]
            for h in range(16):
                k.pe("matmul", pz[:, :], o_[:, h, :], w_o[:, h, n * 512:(n + 1) * 512], start=(h == 0), stop=(h == 15),
                     r=[o_, w_o], w=[pz])
            k.dve("tensor_tensor", h1[:, n * 512:(n + 1) * 512], x_[:, n * 512:(n + 1) * 512], pz[:, :], ALU.add,
                  r=[x_, pz], w=[h1])
        k.dma("sp", d["H1s"][tok, :], h1[:, :], r=[h1], sem=h1)
        g1_ = lambda a: a.rearrange("p (g w) -> p g w", g=1)
        rms(k, c, h1, g1_(h1[:, :]), 1, 2048, g_ffn[:, :], scr, g1_(scr[:, :]), scr, scr2, stat)
        k.pool("tensor_copy", hn2[:, :], scr[:, :], r=[scr], w=[hn2])
        k.dma("sp", d["HN2"][tok, :], hn2[:, :], r=[hn2], sem=hn2)
        transposes(k, c, [scr[:, kc * P:(kc + 1) * P] for kc in range(16)], scr, hn2T,
                   lambda i0, cnt: hn2T[:, i0:i0 + cnt, :], F32, k.ps[7])
        pl = k.ps[5]
        for kc in range(16):
            k.pe("matmul", pl[:, 0:72], hn2T[:, kc, :], w_r[:, kc, :], start=(kc == 0), stop=(kc == 15), r=[hn2T, w_r], w=[pl])
        k.dve("tensor_tensor", LOG[:, i, :], pl[:, 0:72], b_r[:, :], ALU.add, r=[pl, b_r], w=[LOG])
    k.end_stage()

    k.begin_stage()
    sm = k.sb("sm", [P, 16], F32)
    ohg = k.sb("ohg", [P, 8], F32)
    eg = k.sb("eg", [P, 8], F32)
    sel = k.sb("sel", [P, 8, 8], F32)
    ein = k.sb("ein", [P, 8], F32)
    oh1 = k.sb("oh1", [P, 8], F32)
    oh2 = k.sb("oh2", [P, 8], F32)
    e2 = k.sb("e2", [P, 8], F32)
    A = k.sb("A", [P, 64], F32)
    carry = k.sb("carry", [P, 64], F32)
    k.dve("memset", carry[:, :], 0.0, w=[carry])
    pc = k.ps[0]
    pc2 = k.ps[1]
    for i in range(NTOK_T):
        gl = LOG[:, i, 0:8]
        el = LOG[:, i, 8:72].rearrange("p (g e) -> p g e", g=8)
        k.dve("tensor_reduce", sm[:, 0:1], gl, AX.X, ALU.max, r=[LOG], w=[sm])
        k.dve("tensor_scalar", sm[:, 1:2], sm[:, 0:1], -1.0, None, ALU.mult, r=[sm], w=[sm])
        k.dve("tensor_scalar", ohg[:, :], gl, sm[:, 0:1], None, ALU.is_equal, r=[LOG, sm], w=[ohg])
        k.act("activation", eg[:, :], gl, AF.Exp, bias=sm[:, 1:2], r=[LOG, sm], w=[eg])
        k.dve("tensor_reduce", sm[:, 2:3], eg[:, :], AX.X, ALU.add, r=[eg], w=[sm])
        k.dve("reciprocal", sm[:, 3:4], sm[:, 2:3], r=[sm], w=[sm])
        k.dve("tensor_tensor", sel[:, :, :], el, bcast(ohg[:, :], 2, 8), ALU.mult, r=[LOG, ohg], w=[sel])
        k.dve("tensor_reduce", ein[:, :], sel[:, :, :].rearrange("p g e -> p e g"), AX.X, ALU.add, r=[sel], w=[ein])
        k.dve("tensor_reduce", sm[:, 4:5], ein[:, :], AX.X, ALU.max, r=[ein], w=[sm])
        k.dve("tensor_scalar", sm[:, 5:6], sm[:, 4:5], -1.0, None, ALU.mult, r=[sm], w=[sm])
        k.dve("tensor_scalar", oh1[:, :], ein[:, :], sm[:, 4:5], None, ALU.is_equal, r=[ein, sm], w=[oh1])
        k.dve("scalar_tensor_tensor", e2[:, :], oh1[:, :], -1e30, ein[:, :], ALU.mult, ALU.add, r=[oh1, ein], w=[e2])
        k.dve("tensor_reduce", sm[:, 6:7], e2[:, :], AX.X, ALU.max, r=[e2], w=[sm])
        k.dve("tensor_scalar", oh2[:, :], e2[:, :], sm[:, 6:7], None, ALU.is_equal, r=[e2, sm], w=[oh2])
        k.act("activation", sm[:, 7:8], sm[:, 6:7], AF.Exp, bias=sm[:, 5:6], r=[sm], w=[sm])
        k.dve("tensor_scalar", sm[:, 8:9], sm[:, 7:8], 1.0, None, ALU.add, r=[sm], w=[sm])
        k.dve("reciprocal", sm[:, 9:10], sm[:, 8:9], r=[sm], w=[sm])
        k.dve("tensor_tensor", GT[:, i, 0:1], sm[:, 3:4], sm[:, 9:10], ALU.mult, r=[sm], w=[GT])
        k.dve("tensor_tensor", GT[:, i, 1:2], GT[:, i, 0:1], sm[:, 7:8], ALU.mult, r=[sm, GT], w=[GT])
        m1 = MSK[:, i, 0, :].rearrange("p (g e) -> p g e", g=8)
        m2 = MSK[:, i, 1, :].rearrange("p (g e) -> p g e", g=8)
        k.dve("tensor_tensor", m1, bcast(ohg[:, :], 2, 8), bcast(oh1[:, :], 1, 8), ALU.mult, r=[ohg, oh1], w=[MSK])
        k.dve("tensor_tensor", m2, bcast(ohg[:, :], 2, 8), bcast(oh2[:, :], 1, 8), ALU.mult, r=[ohg, oh2], w=[MSK])
        k.dve("tensor_tensor", A[:, :], MSK[:, i, 0, :], MSK[:, i, 1, :], ALU.add, r=[MSK], w=[A])
        k.pe("matmul", pc[:, 0:64], c.tri_gt_f[:, :], A[:, :], start=True, stop=True, r=[c.tri_gt_f, A], w=[pc])
        k.dve("tensor_tensor", CUM[:, i, :], pc[:, 0:64], carry[:, :], ALU.add, r=[pc, carry], w=[CUM])
        k.pe("matmul", pc2[:, 0:64], c.ones_f[:, :], A[:, :], start=True, stop=True, r=[c.ones_f, A], w=[pc2])
        k.dve("tensor_tensor", carry[:, :], carry[:, :], pc2[:, 0:64], ALU.add, r=[carry, pc2], w=[carry])
    t1 = k.sb("t1", [P, 64], F32)
    ti = k.sb("ti", [P, 64], I32)
    tf = k.sb("tf", [P, 64], F32)
    pend = k.sb("pend", [P, 64], F32)
    pstart = k.sb("pstart", [P, 64], F32)
    k.dve("tensor_scalar", t1[:, :], carry[:, :], 127.0, 1.0 / 128.0, ALU.add, ALU.mult, r=[carry], w=[t1])
    k.dve("tensor_copy", ti[:, :], t1[:, :], r=[t1], w=[ti])
    k.dve("tensor_copy", tf[:, :], ti[:, :], r=[ti], w=[tf])
    k.dve("tensor_tensor", pend[:, :], tf[:, :], t1[:, :], ALU.is_gt, r=[tf, t1], w=[pend])
    k.dve("tensor_tensor", tf[:, :], tf[:, :], pend[:, :], ALU.subtract, r=[tf, pend], w=[tf])
    k.dve("tensor_scalar", tf[:, :], tf[:, :], 128.0, None, ALU.mult, r=[tf], w=[tf])
    k.dve("tensor_tensor_scan", pend[:, :], c.ones_f[:, 0:64], tf[:, :], 0.0, ALU.mult, ALU.add, r=[c.ones_f, tf], w=[pend])
    k.dve("tensor_tensor", pstart[:, :], pend[:, :], tf[:, :], ALU.subtract, r=[pend, tf], w=[pstart])
    bvi = k.sb("bvi", [P, NBLK], I32)
    k.pool("iota", bvi[:, :], [[128, NBLK]], base=0, channel_multiplier=0, w=[bvi])
    bv = k.sb("bv", [P, NBLK], F32)
    k.dve("tensor_copy", bv[:, :], bvi[:, :], r=[bvi], w=[bv])
    cmp_ = k.sb("cmp", [P, NBLK, 64], F32)
    k.dve("tensor_tensor", cmp_[:, :, :], bcast(pend[:, :], 1, NBLK), bcast(bv[:, :], 2, 64), ALU.is_le, r=[pend, bv], w=[cmp_])
    be = k.sb("be", [P, NBLK], F32)
    k.dve("tensor_reduce", be[:, :], cmp_[:, :, :], AX.X, ALU.add, r=[cmp_], w=[be])
    k.dve("tensor_scalar", be[:, :], be[:, :], 63.0, 128.0, ALU.min, ALU.mult, r=[be], w=[be])
    k.dve("tensor_scalar", be[:, :], be[:, :], c.partidx[:, 0:1], None, ALU.add, r=[be, c.partidx], w=[be])
    if SKIP_UNUSED:
        usd = k.sb("usd", [P, NBLK], F32)
        k.dve("tensor_scalar", usd[:, :], bv[:, :], pend[:, 63:64], None, ALU.is_lt, r=[bv, pend], w=[usd])
        k.dve("tensor_scalar", usd[:, :], usd[:, :], -4194304.0, 4194304.0, ALU.mult, ALU.add, r=[usd], w=[usd])
        k.dve("tensor_tensor", be[:, :], be[:, :], usd[:, :], ALU.add, r=[be, usd], w=[be])
    k.dve("tensor_copy", IDXW[:, :], be[:, :], r=[be], w=[IDXW])
    i16f = k.sb("i16f", [P, NBLK, 16], F32)
    k.dve("scalar_tensor_tensor", i16f[:, :, 0:8], bcast(be[:, :], 2, 8), 8.0, bcast(c.colidx[:, 0:8], 1, NBLK), ALU.mult, ALU.add,
          r=[be, c.colidx], w=[i16f])
    k.dve("tensor_copy", c.IDX16[:, :, 0:8], i16f[:, :, 0:8], r=[i16f], w=[c.IDX16])
    k.dve("scalar_tensor_tensor", i16f[:, :, 0:4], bcast(be[:, :], 2, 4), 4.0, bcast(c.colidx[:, 0:4], 1, NBLK), ALU.mult, ALU.add,
          r=[be, c.colidx], w=[i16f])
    k.dve("tensor_copy", c.IDX4[:, :, :], i16f[:, :, 0:4], r=[i16f], w=[c.IDX4])
    dsel = k.sb("dsel", [P, 64], F32)
    pc_ = k.sb("pc_", [P, 64], F32)
    destf = k.sb("destf", [P, 2], F32)
    hnt = [k.sb("hnt%d" % i, [P, 2048], BF16) for i in range(2)]
    for i in range(NTOK_T):
        tok = slice(i * P, (i + 1) * P)
        k.dve("tensor_tensor", pc_[:, :], pstart[:, :], CUM[:, i, :], ALU.add, r=[pstart, CUM], w=[pc_])
        for s in range(2):
            k.dve("tensor_tensor", dsel[:, :], pc_[:, :], MSK[:, i, s, :], ALU.mult, r=[pc_, MSK], w=[dsel])
            k.dve("tensor_reduce", destf[:, s:s + 1], dsel[:, :], AX.X, ALU.add, r=[dsel], w=[destf])
        k.dve("tensor_copy", DESTI[:, i, :], destf[:, :], r=[destf], w=[DESTI])
        ht = hnt[i % 2]
        k.dma("sp", ht[:, :], d["HN2"][tok, :], r=[d["HN2"]], w=[ht], sem=ht)
        for s in range(2):
            k.idma(d["XBUF"][:, :], IOA(ap=DESTI[:, i, s:s + 1], axis=0), ht[:, :], None,
                   r=[ht, DESTI], sem=ht)
    if "DBG" in d:
        k.dma("sp", d["DBG"][:, 0:32], DESTI[:, :, :].rearrange("p a b -> p (a b)"), r=[DESTI], sem=DESTI)
        k.dma("sp", d["DBG"][:, 32:128], IDXW[:, :], r=[IDXW], sem=IDXW)
        k.dma("sp", d["DBGF"][:, 0:32], GT[:, :, :].rearrange("p a b -> p (a b)"), r=[GT], sem=GT)
        k.dma("sp", d["DBGF"][:, 32:32 + 1152], LOG[:, :, :].rearrange("p a b -> p (a b)"), r=[LOG], sem=LOG)
        k.dma("sp", d["DBGF"][:, 1184:1184 + 64], pend[:, :], r=[pend], sem=pend)
        k.dma("sp", d["DBGF"][:, 1248:1248 + 64], carry[:, :], r=[carry], sem=carry)
    k.end_stage()
    if upto <= 2:
        return

    k.begin_stage()
    wgu = [[k.sb("wgu%d_%d" % (i, cc_), [P, 2, 1024], BF16) for cc_ in range(8)] for i in range(2)]
    wdn = [[k.sb("wdn%d_%d" % (i, cc_), [P, 2048], BF16) for cc_ in range(4)] for i in range(2)]
    xb = [k.sb("xb%d" % i, [P, 2048], BF16) for i in range(2)]
    xbT = k.sb("xbT", [P, 16, P], BF16)
    sgl = k.sb("sgl", [P, 512], F32)
    actb = k.sb("actb", [P, 512], BF16)
    actT = k.sb("actT", [P, 4, P], BF16)
    ysb = [k.sb("ysb%d" % i, [P, 2048], F32) for i in range(2)]
    if SKIP_UNUSED:
        reg_gu = k.nc.gpsimd.to_reg(64 * 1024 - 1)
        reg_dn = k.nc.gpsimd.to_reg(64 * 512 - 1)
    wgu_src = d["w_gu"][:, :, :].rearrange("e (r two) n -> (e r) (two n)", two=2)
    wdn_src = d["w_dn"][:, :, :].rearrange("e r n -> (e r) n")

    def load_blk(b):
        s = b % 2
        for cc_ in range(8):
            k.idma(wgu[s][cc_][:, :, :].rearrange("p a n -> p (a n)"), None, wgu_src,
                   IOA(ap=c.IDX16[:, b, cc_:cc_ + 1], axis=0), r=[c.IDX16], w=[wgu[s][cc_]], sem=wgu[s][cc_],
                   **(dict(bounds_check=reg_gu, oob_is_err=False) if SKIP_UNUSED else {}))
        for kc in range(4):
            k.idma(wdn[s][kc][:, :], None, wdn_src, IOA(ap=c.IDX4[:, b, kc:kc + 1], axis=0), r=[c.IDX4], w=[wdn[s][kc]], sem=wdn[s][kc],
                   **(dict(bounds_check=reg_dn, oob_is_err=False) if SKIP_UNUSED else {}))
        k.dma("sp", xb[s][:, :], d["XBUF"][b * P:(b + 1) * P, :], r=[d["XBUF"]], w=[xb[s]], sem=xb[s])

    load_blk(0)
    for b in range(NBLK):
        s = b % 2
        if b + 1 < NBLK:
            load_blk(b + 1)
        xv = xb[s][:, :].rearrange("t (p kc) -> t p kc", kc=16)
        transposes(k, c, [xv[:, :, kc] for kc in range(16)], xb[s], xbT, lambda i0, cnt: xbT[:, i0:i0 + cnt, :], BF16, k.ps[7])
        for n in range(2):
            pz = k.ps[n]
            for kc in range(16):
                k.pe("matmul", pz[:, :], xbT[:, kc, :], wgu[s][kc // 2][:, kc % 2, n * 512:(n + 1) * 512], start=(kc == 0), stop=(kc == 15),
                     r=[xbT, wgu[s][kc // 2]], w=[pz])
        k.act("activation", sgl[:, :], k.ps[0][:, :], AF.Silu, r=[k.ps[0]], w=[sgl])
        k.dve("tensor_tensor", actb[:, :], sgl[:, :], k.ps[1][:, :], ALU.mult, r=[sgl, k.ps[1]], w=[actb])
        av = actb[:, :].rearrange("t (p kc) -> t p kc", kc=4)
        transposes(k, c, [av[:, :, kc] for kc in range(4)], actb, actT, lambda i0, cnt: actT[:, i0:i0 + cnt, :], BF16, k.ps[6])
        y_ = ysb[s]
        for n in range(4):
            pz = k.ps[2 + n]
            for kc in range(4):
                k.pe("matmul", pz[:, :], actT[:, kc, :], wdn[s][kc][:, n * 512:(n + 1) * 512], start=(kc == 0), stop=(kc == 3),
                     r=[actT, wdn[s][kc]], w=[pz])
            if n % 2 == 0:
                k.dve("tensor_copy", y_[:, n * 512:(n + 1) * 512], pz[:, :], r=[pz], w=[y_])
            else:
                k.act("copy", y_[:, n * 512:(n + 1) * 512], pz[:, :], r=[pz], w=[y_])
        k.dma("sp", d["YBUF"][b * P:(b + 1) * P, :], y_[:, :], r=[y_], sem=y_)
    k.end_stage()

    k.begin_stage()
    hh = [k.sb("hh%d" % i, [P, 2048], F32) for i in range(2)]
    y1 = [k.sb("y1_%d" % i, [P, 2048], F32) for i in range(2)]
    y2 = [k.sb("y2_%d" % i, [P, 2048], F32) for i in range(2)]
    scr = k.sb("scr", [P, 2048], F32)
    scr2 = k.sb("scr2", [P, 2048], F32)
    stat = k.sb("stat", [P, 32], F32)
    hnb = k.sb("hnb", [P, 2048], BF16)
    if not last:
        g_nx = k.sb("g_nx", [P, 2048], F32)
        k.dma("sp", g_nx[:, :], d["g_next"][:, :], r=[d["g_next"]], w=[g_nx], sem=g_nx)
    def ld_b4(i):
        tk = slice(i * P, (i + 1) * P)
        k.dma("sp", hh[i % 2][:, :], d["H1s"][tk, :], r=[d["H1s"]], w=[hh[i % 2]], sem=hh[i % 2])
        k.idma(y1[i % 2][:, :], None, d["YBUF"][:, :], IOA(ap=DESTI[:, i, 0:1], axis=0), r=[d["YBUF"], DESTI], w=[y1[i % 2]], sem=y1[i % 2])
        k.idma(y2[i % 2][:, :], None, d["YBUF"][:, :], IOA(ap=DESTI[:, i, 1:2], axis=0), r=[d["YBUF"], DESTI], w=[y2[i % 2]], sem=y2[i % 2])

    ld_b4(0)
    for i in range(NTOK_T):
        tok = slice(i * P, (i + 1) * P)
        h_ = hh[i % 2]
        a_ = y1[i % 2]
        b_ = y2[i % 2]
        if i + 1 < NTOK_T:
            ld_b4(i + 1)
        k.dve("scalar_tensor_tensor", h_[:, :], a_[:, :], GT[:, i, 0:1], h_[:, :], ALU.mult, ALU.add, r=[a_, GT, h_], w=[h_])
        k.dve("scalar_tensor_tensor", h_[:, :], b_[:, :], GT[:, i, 1:2], h_[:, :], ALU.mult, ALU.add, r=[b_, GT, h_], w=[h_])
        k.dma("sp", d["HOUT"][tok, :], h_[:, :], r=[h_], sem=h_)
        if not last:
            g1_ = lambda a: a.rearrange("p (g w) -> p g w", g=1)
            rms(k, c, h_, g1_(h_[:, :]), 1, 2048, g_nx[:, :], hnb, g1_(hnb[:, :]), scr, scr2, stat)
            k.dma("sp", d["HN"][tok, :], hnb[:, :], r=[hnb], sem=hnb)
    k.end_stage()

def stage_C(k, c, d):
    SCALE = 1.0 / float(np.sqrt(128.0))
    NT = SEQ // P
    g1_ = lambda a: a.rearrange("p (g w) -> p g w", g=1)
    k.begin_stage()
    g_qk = k.sb("g_qk", [P, 256], F32)
    k.dma("sp", g_qk[:, :], d["g_qk"][:, :], r=[d["g_qk"]], w=[g_qk], sem=g_qk)
    g_q = k.sb("g_q", [P, 128], F32)
    k.dve("tensor_scalar", g_q[:, :], g_qk[:, 0:128], SCALE, None, ALU.mult, r=[g_qk], w=[g_q])
    w_qk = k.sb("w_qk", [P, 16, 2048], BF16)
    k.dma("pool", w_qk[:, :, :], d["w_qk"][:, :].rearrange("(kc p) n -> p kc n", p=P), r=[d["w_qk"]], w=[w_qk], sem=w_qk)
    hn = [k.sb("hn%d" % i, [P, 2048], BF16) for i in range(2)]
    hnT = k.sb("hnT", [P, 16, P], BF16)
    zsb = k.sb("zsb", [P, 16, P], F32)
    scr = k.sb("scr", [P, 2048], F32)
    scr2 = k.sb("scr2", [P, 2048], F32)
    stat = k.sb("stat", [P, 32], F32)
    scrb = k.sb("scrb", [P, 1024], F32)
    scr2b = k.sb("scr2b", [P, 1024], F32)
    statb = k.sb("statb", [P, 32], F32)
    qn = k.sb("qn", [P, 8, P], BF16)
    kn = k.sb("kn", [P, 8, P], BF16)
    qTs = [k.sb("qT%d" % i, [P, 8, P], BF16) for i in range(2)]
    kTs = [k.sb("kT%d" % i, [P, 8, P], BF16) for i in range(2)]
    zflat = zsb[:, :, :].rearrange("p h d -> p (h d)")
    def ld_hn(i):
        k.dma("sp", hn[i % 2][:, :], d["HNf"][i * P:(i + 1) * P, :], r=[d["HNf"]], w=[hn[i % 2]], sem=hn[i % 2])

    ld_hn(0)
    for i in range(NT):
        tok = slice(i * P, (i + 1) * P)
        h_ = hn[i % 2]
        qT = qTs[i % 2]
        kT = kTs[i % 2]
        if i + 1 < NT:
            ld_hn(i + 1)
        transposes(k, c, [h_[:, kc * P:(kc + 1) * P] for kc in range(16)], h_, hnT, lambda i0, cnt: hnT[:, i0:i0 + cnt, :], BF16, k.ps[7])
        for n in range(4):
            pz = k.ps[n]
            for kc in range(16):
                k.pe("matmul", pz[:, :], hnT[:, kc, :], w_qk[:, kc, n * 512:(n + 1) * 512], start=(kc == 0), stop=(kc == 15),
                     r=[hnT, w_qk], w=[pz])
            k.act("copy", zflat[:, n * 512:(n + 1) * 512], pz[:, :], r=[pz], w=[zsb])
        rms_multi(k, c, [
            (zsb, zsb[:, 0:8, :], 8, 128, g_q[:, :], qn, qn[:, :, :], scr, scr2, stat),
            (zsb, zsb[:, 8:16, :], 8, 128, g_qk[:, 128:256], kn, kn[:, :, :], scrb, scr2b, statb),
        ])
        transposes(k, c, [qn[:, h, :] for h in range(8)], qn, qT, lambda i0, cnt: qT[:, i0:i0 + cnt, :], BF16, k.ps[6])
        transposes(k, c, [kn[:, h, :] for h in range(8)], kn, kT, lambda i0, cnt: kT[:, i0:i0 + cnt, :], BF16, k.ps[5])
        k.dma("sp", d["QT"][:, :, tok].rearrange("h d t -> d h t"), qT[:, :, :], r=[qT], sem=qT)
        k.dma("sp", d["KT"][:, :, tok].rearrange("h d t -> d h t"), kT[:, :, :], r=[kT], sem=kT)
    k.end_stage()

    k.begin_stage()
    w_vg = k.sb("w_vg", [P, 16, 2056], BF16)
    k.dma("pool", w_vg[:, :, :], d["w_vg"][:, :].rearrange("(kc p) n -> p kc n", p=P), r=[d["w_vg"]], w=[w_vg], sem=w_vg)
    fb = k.sb("fb", [P, 8], F32)
    k.dma("sp", fb[:, :], d["fbias"][:, :], r=[d["fbias"]], w=[fb], sem=fb)
    hn = [k.sb("hn%d" % i, [P, 2048], BF16) for i in range(2)]
    hnT = k.sb("hnT", [P, 16, P], BF16)
    vbs = [k.sb("vb%d" % i, [P, 8, P], BF16) for i in range(2)]
    sgbs = [k.sb("sgb%d" % i, [P, 1024], BF16) for i in range(2)]
    fx = k.sb("fx", [P, 8], F32)
    fa = k.sb("fa", [P, 8], F32)
    fe = k.sb("fe", [P, 8], F32)
    fl = k.sb("fl", [P, 8], F32)
    fm = k.sb("fm", [P, 8], F32)
    lf = k.sb("lf", [P, 8], F32)
    cc = k.sb("cc", [P, 8], F32)
    hf = k.sb("hf", [P, 8], F32)
    r1 = k.sb("r1", [P, 8], F32)
    carry = k.sb("carryc", [P, 8], F32)
    k.dve("memset", carry[:, :], 0.0, w=[carry])
    caq = k.sb("caq", [P, 8, 6], BF16)
    cak = k.sb("cak", [P, 8, 6], BF16)
    k.dve("memset", caq[:, :, :], 1.0, w=[caq])
    k.dve("memset", cak[:, :, :], 1.0, w=[cak])
    caqTs = [k.sb("caqT%d" % i, [48, P], BF16) for i in range(2)]
    cakTs = [k.sb("cakT%d" % i, [48, P], BF16) for i in range(2)]
    def ld_hn(i):
        k.dma("sp", hn[i % 2][:, :], d["HNf"][i * P:(i + 1) * P, :], r=[d["HNf"]], w=[hn[i % 2]], sem=hn[i % 2])

    ld_hn(0)
    for i in range(NT):
        tok = slice(i * P, (i + 1) * P)
        h_ = hn[i % 2]
        vb = vbs[i % 2]
        sgb = sgbs[i % 2]
        caqT = caqTs[i % 2]
        cakT = cakTs[i % 2]
        vflat = vb[:, :, :].rearrange("p h d -> p (h d)")
        if i + 1 < NT:
            ld_hn(i + 1)
        transposes(k, c, [h_[:, kc * P:(kc + 1) * P] for kc in range(16)], h_, hnT, lambda i0, cnt: hnT[:, i0:i0 + cnt, :], BF16, k.ps[7])
        for n in range(5):
            pz = k.ps[n]
            n0 = n * 512
            n1 = min(n0 + 512, 2056)
            for kc in range(16):
                k.pe("matmul", pz[:, 0:n1 - n0], hnT[:, kc, :], w_vg[:, kc, n0:n1], start=(kc == 0), stop=(kc == 15),
                     r=[hnT, w_vg], w=[pz])
            if n < 2:
                k.act("copy", vflat[:, n0:n1], pz[:, :], r=[pz], w=[vb])
            elif n < 4:
                k.act("activation", sgb[:, n0 - 1024:n1 - 1024], pz[:, :], AF.Sigmoid, r=[pz], w=[sgb])
            else:
                k.dve("tensor_tensor", fx[:, :], pz[:, 0:8], fb[:, :], ALU.add, r=[pz, fb], w=[fx])
        k.dma("sp", d["V"][:, tok, :].rearrange("h t d -> t h d"), vb[:, :, :], r=[vb], sem=vb)
        k.dma("sp", d["SG"][tok, :], sgb[:, :], r=[sgb], sem=sgb)
        k.dve("tensor_scalar", fa[:, :], fx[:, :], -1.0, None, ALU.mult, r=[fx], w=[fa])
        k.dve("tensor_tensor", fa[:, :], fa[:, :], fx[:, :], ALU.max, r=[fa, fx], w=[fa])
        k.act("activation", fe[:, :], fa[:, :], AF.Exp, scale=-1.0, r=[fa], w=[fe])
        k.act("activation", fl[:, :], fe[:, :], AF.Ln, bias=c.one[:, 0:1], r=[fe, c.one], w=[fl])
        k.dve("tensor_scalar", fm[:, :], fx[:, :], 0.0, None, ALU.min, r=[fx], w=[fm])
        k.dve("tensor_tensor", lf[:, :], fm[:, :], fl[:, :], ALU.subtract, r=[fm, fl], w=[lf])
        pc = k.ps[5]
        pc2 = k.ps[6]
        k.pe("matmul", pc[:, 0:8], c.tri_ge_f[:, :], lf[:, :], start=True, stop=True, r=[c.tri_ge_f, lf], w=[pc])
        k.dve("tensor_tensor", cc[:, :], pc[:, 0:8], carry[:, :], ALU.add, r=[pc, carry], w=[cc])
        k.pe("matmul", pc2[:, 0:8], c.ones_f[:, :], lf[:, :], start=True, stop=True, r=[c.ones_f, lf], w=[pc2])
        k.dve("tensor_tensor", carry[:, :], carry[:, :], pc2[:, 0:8], ALU.add, r=[carry, pc2], w=[carry])
        k.dve("tensor_copy", caq[:, :, 3], cc[:, :], r=[cc], w=[caq])
        k.dve("tensor_copy", hf[:, :], caq[:, :, 3], r=[caq], w=[hf])
        k.dve("tensor_scalar", cak[:, :, 0], hf[:, :], -1.0, None, ALU.mult, r=[hf], w=[cak])
        k.dve("tensor_tensor", r1[:, :], cc[:, :], hf[:, :], ALU.subtract, r=[cc, hf], w=[r1])
        k.dve("tensor_copy", caq[:, :, 4], r1[:, :], r=[r1], w=[caq])
        k.dve("tensor_copy", hf[:, :], caq[:, :, 4], r=[caq], w=[hf])
        k.dve("tensor_scalar", cak[:, :, 1], hf[:, :], -1.0, None, ALU.mult, r=[hf], w=[cak])
        k.dve("tensor_tensor", r1[:, :], r1[:, :], hf[:, :], ALU.subtract, r=[r1, hf], w=[r1])
        k.dve("tensor_copy", caq[:, :, 5], r1[:, :], r=[r1], w=[caq])
        k.dve("tensor_scalar", cak[:, :, 2], caq[:, :, 5], -1.0, None, ALU.mult, r=[caq], w=[cak])
        transposes(k, c, [caq[:, :, :].rearrange("p h i -> p (h i)")], caq, caqT,
                   lambda i0, cnt: caqT[:, :].rearrange("p (a b) -> p a b", a=1), BF16, k.ps[7])
        transposes(k, c, [cak[:, :, :].rearrange("p h i -> p (h i)")], cak, cakT,
                   lambda i0, cnt: cakT[:, :].rearrange("p (a b) -> p a b", a=1), BF16, k.ps[7])
        k.dma("sp", d["CAQ"][:, tok], caqT[:, :], r=[caqT], sem=caqT)
        k.dma("sp", d["CAK"][:, tok], cakT[:, :], r=[cakT], sem=cakT)
    k.end_stage()

    k.begin_stage()
    sets = []
    for s in range(2):
        st = dict(q=k.sb("aq%d" % s, [P, SEQ], BF16), k=k.sb("ak%d" % s, [P, SEQ], BF16),
                  cq=k.sb("acq%d" % s, [6, SEQ], BF16), ck=k.sb("ack%d" % s, [6, SEQ], BF16),
                  v=k.sb("av%d" % s, [P, NKT, 132], BF16))
        k.dve("memset", st["v"][:, :, 128:129], 1.0, w=[st["v"]])
        sets.append(st)

    def load_head(h, s):
        st = sets[s]
        k.dma("sp", st["q"][:, :], d["QT"][h, :, :], r=[d["QT"]], w=[st["q"]], sem=st["q"])
        k.dma("sp", st["k"][:, :], d["KT"][h, :, :], r=[d["KT"]], w=[st["k"]], sem=st["k"])
        k.dma("sp", st["cq"][:, :], d["CAQ"][h * 6:(h + 1) * 6, :], r=[d["CAQ"]], w=[st["cq"]], sem=st["cq"])
        k.dma("sp", st["ck"][:, :], d["CAK"][h * 6:(h + 1) * 6, :], r=[d["CAK"]], w=[st["ck"]], sem=st["ck"])
        k.dma("sp", st["v"][:, :, 0:128], d["V"][h, :, :].rearrange("(n p) d -> p n d", p=P), r=[d["V"]], w=[st["v"]], sem=st["v"])
        pairs = [
            (st["k"], (lambda kt, b=st["k"]: b[:, kt * P:(kt + 1) * P]), st["q"], (lambda a, e, b=st["q"]: b[:, a:e])),
            (st["ck"], (lambda kt, b=st["ck"]: b[:, kt * P:(kt + 1) * P]), st["cq"], (lambda a, e, b=st["cq"]: b[:, a:e])),
        ]
        return pairs, st["v"]

    attention(k, c, load_head, None, d["OTX"], gate_d=d["SG"])
    k.end_stage()
def rep128(v):
    v = np.ascontiguousarray(np.asarray(v, dtype=np.float32).reshape(1, -1))
    return np.ascontiguousarray(np.broadcast_to(v, (P, v.shape[1])))


def new_nc():
    return bass.Bass("TRN2", target_bir_lowering=False)


def build_A():
    nc = new_nc()
    k = K(nc)
    d = {}
    def ext(name, shape, dt):
        d[name] = k.dram(name, shape, dt, kind="ExternalInput")
    ext("x", [4096, 2048], F32); ext("pos", [4096, 1], I32)
    ext("g_mix", [P, 2048], F32); ext("g_ql", [P, 512], F32); ext("g_kvl", [P, 512], F32)
    ext("g_qk", [P, 384], F32); ext("freqs", [P, 32], F32)
    ext("w_in", [2048, 1088], F32); ext("w_uq", [512, 1536], F32); ext("w_ukv", [512, 2048], F32)
    d["KRT"] = k.dram("KRT", [64, 4096], BF16)
    d["QT"] = k.dram("QT", [8, 128, 4096], BF16)
    d["QRT"] = k.dram("QRT", [8, 64, 4096], BF16)
    d["KT"] = k.dram("KT", [8, 128, 4096], BF16)
    d["V"] = k.dram("V", [8, 4096, 128], BF16)
    d["OTX"] = k.dram("OTX", [2, 8, 128, 2048], BF16, kind="ExternalOutput")
    k.begin_stage()
    c = make_consts(k)
    k.end_stage()
    stage_A(k, c, d)
    k.finish()
    return nc


def inputs_A(inp, core):
    b, j = core // 2, core % 2
    half = 32
    freqs = (10000.0 ** (-np.arange(half, dtype=np.float32) / half)).astype(np.float32)
    uq = inp["l0_mla_w_uq"].reshape(512, 16, 192)[:, 8 * j:8 * j + 8, :].reshape(512, 1536)
    ukv = inp["l0_mla_w_ukv"].reshape(512, 16, 256)[:, 8 * j:8 * j + 8, :].reshape(512, 2048)
    return {
        "x": np.ascontiguousarray(inp["x"][b]),
        "pos": np.ascontiguousarray(inp["positions"][b].reshape(4096, 1).astype(np.int32)),
        "g_mix": rep128(inp["l0_norm_mix"]), "g_ql": rep128(inp["l0_mla_q_lat_norm"]),
        "g_kvl": rep128(inp["l0_mla_kv_lat_norm"]), "g_qk": rep128(inp["l0_mla_qk_gain"].reshape(-1)),
        "freqs": rep128(freqs),
        "w_in": np.ascontiguousarray(inp["l0_mla_w_in"]), "w_uq": np.ascontiguousarray(uq), "w_ukv": np.ascontiguousarray(ukv),
    }


def build_B(layer, upto=4, dbg=False):
    last = (layer == 1)
    nc = new_nc()
    k = K(nc)
    d = {}
    def ext(name, shape, dt):
        d[name] = k.dram(name, shape, dt, kind="ExternalInput")
    ext("OTin", [16, 128, 2048], BF16); ext("xres", [2048, 2048], F32); ext("w_o", [2048, 2048], F32)
    ext("g_ffn", [P, 2048], F32); ext("w_r", [2048, 72], F32); ext("b_r", [P, 72], F32)
    if upto > 2:
        ext("w_gu", [64, 2048, 1024], F32); ext("w_dn", [64, 512, 2048], F32)
    if dbg:
        d["DBG"] = k.dram("DBG", [P, 128], I32, kind="ExternalOutput")
        d["DBGF"] = k.dram("DBGF", [P, 1312], F32, kind="ExternalOutput")
    if not last:
        ext("g_next", [P, 2048], F32)
        d["HN"] = k.dram("HN", [2048, 2048], BF16, kind="ExternalOutput")
    d["H1s"] = k.dram("H1s", [2048, 2048], F32)
    d["HN2"] = k.dram("HN2", [2048, 2048], BF16)
    d["XBUF"] = k.dram("XBUF", [NBLK * P, 2048], BF16)
    d["YBUF"] = k.dram("YBUF", [NBLK * P, 2048], F32)
    d["HOUT"] = k.dram("HOUT", [2048, 2048], F32, kind="ExternalOutput")
    k.begin_stage()
    c = make_consts(k)
    k.end_stage()
    stage_WO_MOE(k, c, d, last, upto)
    k.finish()
    return nc


def inputs_B(inp, core, layer, otin, xres):
    pre = "l%d_" % layer
    wo = inp["l0_mla_w_o"] if layer == 0 else inp["l1_fox_w_o"]
    m = {
        "OTin": (None if otin is None else np.ascontiguousarray(otin)), "xres": (None if xres is None else np.ascontiguousarray(xres)), "w_o": np.ascontiguousarray(wo),
        "g_ffn": rep128(inp[pre + "norm_ffn"]),
        "w_r": np.ascontiguousarray(np.concatenate([inp[pre + "router_group"], inp[pre + "router_expert"]], axis=1)),
        "b_r": rep128(np.concatenate([inp[pre + "router_group_bias"], inp[pre + "router_expert_bias"]])),
        "w_gu": np.ascontiguousarray(inp[pre + "w_gate_up"]), "w_dn": np.ascontiguousarray(inp[pre + "w_down"]),
    }
    if layer == 0:
        m["g_next"] = rep128(inp["l1_norm_mix"])
    return m


def build_C():
    nc = new_nc()
    k = K(nc)
    d = {}
    def ext(name, shape, dt):
        d[name] = k.dram(name, shape, dt, kind="ExternalInput")
    ext("HNf", [4096, 2048], BF16); ext("w_qk", [2048, 2048], F32); ext("w_vg", [2048, 2056], F32)
    ext("fbias", [P, 8], F32); ext("g_qk", [P, 256], F32)
    d["QT"] = k.dram("QT", [8, 128, 4096], BF16)
    d["KT"] = k.dram("KT", [8, 128, 4096], BF16)
    d["V"] = k.dram("V", [8, 4096, 128], BF16)
    d["SG"] = k.dram("SG", [4096, 1024], BF16)
    d["CAQ"] = k.dram("CAQ", [48, 4096], BF16)
    d["CAK"] = k.dram("CAK", [48, 4096], BF16)
    d["OTX"] = k.dram("OTX", [2, 8, 128, 2048], BF16, kind="ExternalOutput")
    k.begin_stage()
    c = make_consts(k)
    k.end_stage()
    stage_C(k, c, d)
    k.finish()
    return nc


def inputs_C(inp, core, hnf):
    j = core % 2
    w = inp["l1_fox_w_in"]
    o = 1024 * j
    w_qk = np.concatenate([w[:, o:o + 1024], w[:, 2048 + o:2048 + o + 1024]], axis=1)
    w_vg = np.concatenate([w[:, 4096 + o:4096 + o + 1024], w[:, 6160 + o:6160 + o + 1024], w[:, 6144 + 8 * j:6144 + 8 * j + 8]], axis=1)
    return {
        "HNf": (None if hnf is None else np.ascontiguousarray(hnf)), "w_qk": np.ascontiguousarray(w_qk), "w_vg": np.ascontiguousarray(w_vg),
        "fbias": rep128(inp["l1_fox_forget_bias"][8 * j:8 * j + 8]), "g_qk": rep128(inp["l1_fox_qk_gain"].reshape(-1)),
    }


def _run(nc, maps):
    return run_bass_kernel_spmd(nc, maps, core_ids=list(range(8))).results


def build_F():
    nc = new_nc()
    k = K(nc)
    def ext(name, shape, dt):
        return k.dram(name, shape, dt, kind="ExternalInput")
    def view(ap, name):
        return Buf(ap, name)
    x = ext("x", [4096, 2048], F32)
    base_A = dict(x=x, pos=ext("pos", [4096, 1], I32), g_mix=ext("g_mix", [P, 2048], F32), g_ql=ext("g_ql", [P, 512], F32),
                  g_kvl=ext("g_kvl", [P, 512], F32), g_qk=ext("g_qk", [P, 384], F32), freqs=ext("freqs", [P, 32], F32),
                  w_in=ext("w_in", [2048, 1088], F32))
    w_uq = ext("w_uq", [512, 3072], F32)
    w_ukv = ext("w_ukv", [512, 4096], F32)
    base_A["KRT"] = k.dram("KRT", [64, 4096], BF16)
    base_A["QT"] = k.dram("QT", [8, 128, 4096], BF16)
    base_A["QRT"] = k.dram("QRT", [8, 64, 4096], BF16)
    base_A["KT"] = k.dram("KT", [8, 128, 4096], BF16)
    base_A["V"] = k.dram("V", [8, 4096, 128], BF16)
    otf = k.dram("OTF", [16, 128, 4096], BF16)
    shared = dict(H1s=k.dram("H1s", [2048, 2048], F32), HN2=k.dram("HN2", [2048, 2048], BF16),
                  XBUF=k.dram("XBUF", [NBLK * P, 2048], BF16), YBUF=k.dram("YBUF", [NBLK * P, 2048], F32))
    h2 = k.dram("H2", [4096, 2048], F32)
    hnf = k.dram("HNF", [4096, 2048], BF16)
    out = k.dram("out", [2048, 2048], F32, kind="ExternalOutput")
    otin_own = k.dram("OTIN_OWN", [16, 128, 2048], BF16)
    xres_own = k.dram("XRES_OWN", [2048, 2048], F32)
    idxo_d = ext("idxo", [P, 16], I32)
    idxr_d = ext("idxr", [P, 16], I32)

    def moe_w(layer):
        p_ = "l%d_" % layer
        return dict(w_o=ext(p_ + "w_o", [2048, 2048], F32), g_ffn=ext(p_ + "g_ffn", [P, 2048], F32),
                    w_r=ext(p_ + "w_r", [2048, 72], F32), b_r=ext(p_ + "b_r", [P, 72], F32),
                    w_gu=ext(p_ + "w_gu", [64, 2048, 1024], F32), w_dn=ext(p_ + "w_dn", [64, 512, 2048], F32))
    mw0 = moe_w(0)
    mw1 = moe_w(1)
    g_next = ext("g_next", [P, 2048], F32)
    cw = [dict(w_qk=ext("w_qk%d" % hh, [2048, 2048], F32), w_vg=ext("w_vg%d" % hh, [2048, 2056], F32),
               fbias=ext("fbias%d" % hh, [P, 8], F32)) for hh in range(2)]
    l1_g_qk = ext("l1_g_qk", [P, 256], F32)
    sg = k.dram("SG", [4096, 1024], BF16)
    caq = k.dram("CAQ", [48, 4096], BF16)
    cak = k.dram("CAK", [48, 4096], BF16)

    k.begin_stage()
    c = make_consts(k)
    idxo = k.sb("idxo_sb", [P, 16], I32, persist=True)
    idxr = k.sb("idxr_sb", [P, 16], I32, persist=True)
    k.dma("sp", idxo[:, :], idxo_d[:, :], w=[idxo], sem=idxo)
    k.dma("sp", idxr[:, :], idxr_d[:, :], w=[idxr], sem=idxr)
    k.end_stage()

    def otx_view(hh):
        return view(otf.t[hh * 8:(hh + 1) * 8, :, :].rearrange("h d (a t) -> a h d t", a=2), "otxv")

    for hh in range(2):
        dA = dict(base_A)
        dA["w_uq"] = view(w_uq.t[:, hh * 1536:(hh + 1) * 1536], "w_uq_v")
        dA["w_ukv"] = view(w_ukv.t[:, hh * 2048:(hh + 1) * 2048], "w_ukv_v")
        dA["OTX"] = otx_view(hh)
        stage_A(k, c, dA)
    for th in range(2):
        ts = slice(th * 2048, (th + 1) * 2048)
        dB = dict(shared)
        dB.update(mw0)
        dB["OTin"] = view(otf.t[:, :, ts], "otin_v")
        dB["xres"] = view(x.t[ts, :], "xres_v")
        dB["g_next"] = g_next
        dB["HOUT"] = view(h2.t[ts, :], "h2_v")
        dB["HN"] = view(hnf.t[ts, :], "hn_v")
        stage_WO_MOE(k, c, dB, False)
    for hh in range(2):
        dC = dict(HNf=hnf, QT=base_A["QT"], KT=base_A["KT"], V=base_A["V"], SG=sg, CAQ=caq, CAK=cak, g_qk=l1_g_qk)
        dC.update(cw[hh])
        dC["OTX"] = otx_view(hh)
        stage_C(k, c, dC)
    IOA = bass.IndirectOffsetOnAxis
    k.begin_stage()
    tb_ = [k.sb("selb%d" % i, [P, 2048], BF16) for i in range(2)]
    tf_ = [k.sb("self%d" % i, [P, 2048], F32) for i in range(2)]
    otf2d = otf.t.rearrange("h d (a t) -> (h d a) t", a=2)
    for hh in range(16):
        t_ = tb_[hh % 2]
        k.idma(t_[:, :], None, otf2d, IOA(ap=idxo[:, hh:hh + 1], axis=0), r=[idxo], w=[t_], sem=t_)
        k.dma("sp", otin_own.t[hh, :, :], t_[:, :], r=[t_], sem=t_)
    for i in range(16):
        t_ = tf_[i % 2]
        k.idma(t_[:, :], None, h2.t[:, :], IOA(ap=idxr[:, i:i + 1], axis=0), r=[idxr], w=[t_], sem=t_)
        k.dma("sp", xres_own.t[i * P:(i + 1) * P, :], t_[:, :], r=[t_], sem=t_)
    k.end_stage()
    dD = dict(shared)
    dD.update(mw1)
    dD["OTin"] = otin_own
    dD["xres"] = xres_own
    dD["HOUT"] = out
    stage_WO_MOE(k, c, dD, True)
    k.finish()
    return nc


def inputs_F(inp, core):
    b, j = core // 2, core % 2
    half = 32
    freqs = (10000.0 ** (-np.arange(half, dtype=np.float32) / half)).astype(np.float32)
    m = {
        "x": np.ascontiguousarray(inp["x"][b]),
        "pos": np.ascontiguousarray(inp["positions"][b].reshape(4096, 1).astype(np.int32)),
        "g_mix": rep128(inp["l0_norm_mix"]), "g_ql": rep128(inp["l0_mla_q_lat_norm"]),
        "g_kvl": rep128(inp["l0_mla_kv_lat_norm"]), "g_qk": rep128(inp["l0_mla_qk_gain"].reshape(-1)),
        "freqs": rep128(freqs),
        "w_in": np.ascontiguousarray(inp["l0_mla_w_in"]), "w_uq": np.ascontiguousarray(inp["l0_mla_w_uq"]),
        "w_ukv": np.ascontiguousarray(inp["l0_mla_w_ukv"]),
        "g_next": rep128(inp["l1_norm_mix"]),
        "l1_g_qk": rep128(inp["l1_fox_qk_gain"].reshape(-1)),
    }
    m["idxo"] = np.ascontiguousarray(((np.arange(16)[None, :] * 128 + np.arange(128)[:, None]) * 2 + j).astype(np.int32))
    m["idxr"] = np.ascontiguousarray((j * 2048 + np.arange(16)[None, :] * 128 + np.arange(128)[:, None]).astype(np.int32))
    m["l0_w_o"] = np.ascontiguousarray(inp["l0_mla_w_o"])
    m["l0_g_ffn"] = rep128(inp["l0_norm_ffn"])
    m["l0_w_r"] = np.ascontiguousarray(np.concatenate([inp["l0_router_group"], inp["l0_router_expert"]], axis=1))
    m["l0_b_r"] = rep128(np.concatenate([inp["l0_router_group_bias"], inp["l0_router_expert_bias"]]))
    m["l0_w_gu"] = np.ascontiguousarray(inp["l0_w_gate_up"])
    m["l0_w_dn"] = np.ascontiguousarray(inp["l0_w_down"])
    m["l1_w_o"] = np.ascontiguousarray(inp["l1_fox_w_o"])
    m["l1_g_ffn"] = rep128(inp["l1_norm_ffn"])
    m["l1_w_r"] = np.ascontiguousarray(np.concatenate([inp["l1_router_group"], inp["l1_router_expert"]], axis=1))
    m["l1_b_r"] = rep128(np.concatenate([inp["l1_router_group_bias"], inp["l1_router_expert_bias"]]))
    m["l1_w_gu"] = np.ascontiguousarray(inp["l1_w_gate_up"])
    m["l1_w_dn"] = np.ascontiguousarray(inp["l1_w_down"])
    w = inp["l1_fox_w_in"]
    for hh in range(2):
        o = 1024 * hh
        m["w_qk%d" % hh] = np.ascontiguousarray(np.concatenate([w[:, o:o + 1024], w[:, 2048 + o:2048 + o + 1024]], axis=1))
        m["w_vg%d" % hh] = np.ascontiguousarray(np.concatenate(
            [w[:, 4096 + o:4096 + o + 1024], w[:, 6160 + o:6160 + o + 1024], w[:, 6144 + 8 * hh:6144 + 8 * hh + 8]], axis=1))
        m["fbias%d" % hh] = rep128(inp["l1_fox_forget_bias"][8 * hh:8 * hh + 8])
    return m


def kernel(**inputs):
    inp = {k_: np.asarray(v) for k_, v in inputs.items()}
    res = _run(build_F(), [inputs_F(inp, c_) for c_ in range(8)])
    out = np.empty((4, 4096, 2048), np.float32)
    for c_ in range(8):
        b, j = c_ // 2, c_ % 2
        out[b, j * 2048:(j + 1) * 2048] = np.asarray(res[c_]["out"])
    return out
```
